# Optimizing a Trainium2 kernel written in Bass

```python
import jax, jax.numpy as jnp
from jax import lax
import numpy as np

D_MODEL = 1024
BATCH = 16
SEQ = 2048
DEPTH = 2

GRID_W = 64
CTX_LEN = 256
D_A = D_MODEL
HEAD_A = 64
H_A = D_A // HEAD_A
R_W = 64
R_A = 64
R_G = 128
SHIFT_W = 3
GN_EPS = 64e-5
D_B = D_MODEL
CHUNK = 128
G_B = 8
C_B = D_B // G_B
N_EXP = 16
CAP_FACTOR = 2
D_FF = 2 * D_MODEL
N_RWKV = 3 * D_A + 2 * R_W + 2 * R_A + R_G
N_IN = N_RWKV + 2 * D_B + 2 * D_MODEL
ALPHA = (2 * DEPTH) ** 0.25
BETA = (8 * DEPTH) ** -0.25
LN_EPS = 1e-5

kernel_name = "hybrid_rwkv7_gmlp_ecmoe_diffusion_block"


def layer_norm(x, g=None, b=None, eps=LN_EPS):
    xf = x.astype(jnp.float32)
    mu = xf.mean(-1, keepdims=True)
    var = jnp.square(xf - mu).mean(-1, keepdims=True)
    y = (xf - mu) * lax.rsqrt(var + eps)
    if g is not None:
        y = y * g + b
    return y.astype(x.dtype)


def modulate(x, shift, scale):
    return layer_norm(x) * (1 + scale) + shift


def centred_shift(z, w):
    zp = jnp.pad(z, ((0, 0), (1, 1), (0, 0)))
    return zp[:, :-2] * w[0] + zp[:, 1:-1] * w[1] + zp[:, 2:] * w[2]


def rwkv_prep(zr, p):
    B, T, _ = zr.shape
    cuts = np.cumsum((D_A, D_A, D_A, R_W, R_W, R_A, R_A)).tolist()
    r, k, v, dwf, dwb, daf, dab, dg = jnp.split(zr, cuts, axis=-1)
    heads = lambda t: t.astype(jnp.float32).reshape(B, T, H_A, HEAD_A)

    def decay(dw, w0, w2):
        w = -jax.nn.softplus(-(w0 + jnp.tanh(dw) @ w2)) - 0.5
        return jnp.exp(-jnp.exp(heads(w)))

    d_f = decay(dwf, p["w0"][0], p["w2"][0])
    d_b = decay(dwb, p["w0"][1], p["w2"][1])
    a_f = jax.nn.sigmoid(p["a0"][0] + daf @ p["a2"][0])
    a_b = jax.nn.sigmoid(p["a0"][1] + dab @ p["a2"][1])
    kk = heads(k * p["k_k"])
    kk = kk * lax.rsqrt(jnp.maximum(jnp.sum(kk * kk, -1, keepdims=True), 1e-12))
    k_f = heads(k * (1 + (a_f - 1) * p["k_a"]))
    k_b = heads(k * (1 + (a_b - 1) * p["k_a"]))
    return heads(r), k_f, k_b, heads(v), d_f, d_b, kk, heads(a_f), heads(a_b), dg


def rwkv_scan(r, k, v, d, kk, a, s0, reverse):
    xs = tuple(jnp.moveaxis(t, 1, 0) for t in (r, k, v, d, kk, a))

    def step(S, inp):
        r_t, k_t, v_t, d_t, kk_t, a_t = inp
        s_kk = jnp.einsum('bhvk,bhk->bhv', S, kk_t)
        S_new = (S * d_t[:, :, None, :]
                 - s_kk[..., None] * (kk_t * a_t)[:, :, None, :]
                 + v_t[..., None] * k_t[:, :, None, :])
        y = jnp.einsum('bhvk,bhk->bhv', S if reverse else S_new, r_t)
        return S_new, y

    s_fin, ys = lax.scan(step, s0, xs, reverse=reverse)
    return jnp.moveaxis(ys, 0, 1), s_fin


def rwkv_readout(y, r, k, v, g, p):
    B, T = y.shape[:2]
    mu = y.mean(-1, keepdims=True)
    var = jnp.square(y - mu).mean(-1, keepdims=True)
    y = ((y - mu) * lax.rsqrt(var + GN_EPS)).reshape(B, T, D_A) * p["lnx_g"] + p["lnx_b"]
    bonus = (jnp.sum(r * k * p["r_k"], -1, keepdims=True) * v).reshape(B, T, D_A)
    return ((y + bonus) * g).astype(g.dtype)


def spatial_gating(z_sgu, p, n_chunks):
    B, T, _ = z_sgu.shape
    u, v = jnp.split(jax.nn.gelu(z_sgu, approximate=False), 2, axis=-1)
    v = layer_norm(v, p["sgu_ln_g"], p["sgu_ln_b"]).reshape(B, n_chunks, CHUNK, G_B, C_B)
    v = jnp.einsum('gpq,bnqgc->bnpgc', p["sgu_w"], v) + p["sgu_b"].T[:, :, None]
    return u * v.reshape(B, T, D_B)


def token_mixer(h, p, s0_f, s0_b, n_chunks):
    z = h @ p["w_in"]
    z_rwkv, z_sgu, z_gate = jnp.split(z, [N_RWKV, N_RWKV + 2 * D_B], axis=-1)
    r, k_f, k_b, v, d_f, d_b, kk, a_f, a_b, dg = rwkv_prep(centred_shift(z_rwkv, p["shift_conv"]), p)
    y_f, s_f = rwkv_scan(r, k_f, v, d_f, kk, a_f, s0_f, reverse=False)
    y_b, s_b = rwkv_scan(r, k_b, v, d_b, kk, a_b, s0_b, reverse=True)
    y_a = rwkv_readout(y_f + y_b, r, k_f, v, jax.nn.sigmoid(dg) @ p["g2"], p)
    y_s = spatial_gating(z_sgu, p, n_chunks)
    g_a, g_s = jnp.split(jax.nn.sigmoid(z_gate), 2, axis=-1)
    merged = g_a * (y_a @ p["w_branch_a"]) + g_s * (y_s @ p["w_branch_b"])
    return merged @ p["w_out"], s_f, s_b


def context_scan_states(h, p, s0):
    z_rwkv = centred_shift(h @ p["w_in"][:, :N_RWKV], p["shift_conv"])
    r, k_f, k_b, v, d_f, d_b, kk, a_f, a_b, _ = rwkv_prep(z_rwkv, p)
    _, s_f = rwkv_scan(r, k_f, v, d_f, kk, a_f, s0, reverse=False)
    _, s_b = rwkv_scan(r, k_b, v, d_b, kk, a_b, s0, reverse=True)
    return s_f, s_b


def expert_choice_ffn(h, p):
    B, T, D = h.shape
    cap = CAP_FACTOR * T // N_EXP
    aff = jax.nn.softmax((h @ p["router_w"]).astype(jnp.float32), axis=-1)
    gate, idx = lax.top_k(jnp.swapaxes(aff, 1, 2), cap)
    xe = jax.vmap(lambda hb, ib: hb[ib])(h, idx)
    hid = jax.nn.silu(jnp.einsum('becd,edf->becf', xe, p["exp_w1"])) * \
        jnp.einsum('becd,edf->becf', xe, p["exp_w3"])
    ye = jnp.einsum('becf,efd->becd', hid, p["exp_w2"]) * gate[..., None].astype(h.dtype)
    return jax.vmap(lambda yb, ib: jax.ops.segment_sum(
        yb.reshape(-1, D), ib.reshape(-1), num_segments=T))(ye, idx)


def setup_inputs(seed: int = 0) -> dict:
    key = jax.random.key(seed)
    ks = iter(jax.random.split(key, 40))
    L, D = DEPTH, D_MODEL
    nrm = lambda shape, scale: scale * jax.random.normal(next(ks), shape, jnp.float32)
    return {
        "x": nrm((BATCH, SEQ, D), 1.0),
        "c": nrm((BATCH, D), 1.0),
        "ctx": nrm((BATCH, CTX_LEN, D), 1.0),
        "c_ctx": nrm((D,), 1.0),
        "ada_w": nrm((L, D, 6 * D), D ** -0.5),
        "ada_b": nrm((L, 6 * D), 0.02),
        "w_in": nrm((L, D, N_IN), D ** -0.5),
        "shift_conv": jnp.array([0.25, 0.5, 0.25], jnp.float32)[None, :, None] + nrm((L, SHIFT_W, N_RWKV), 0.1),
        "w0": jax.random.uniform(next(ks), (L, 2, D_A), jnp.float32, -6.0, -1.0),
        "w2": nrm((L, 2, R_W, D_A), 0.5 * R_W ** -0.5),
        "a0": nrm((L, 2, D_A), 0.1),
        "a2": nrm((L, 2, R_A, D_A), 0.5 * R_A ** -0.5),
        "g2": nrm((L, R_G, D_A), R_G ** -0.5),
        "k_k": 0.85 + nrm((L, D_A), 0.05),
        "k_a": 1.0 + nrm((L, D_A), 0.05),
        "r_k": nrm((L, H_A, HEAD_A), 0.1),
        "lnx_g": 1.0 + nrm((L, D_A), 0.02),
        "lnx_b": nrm((L, D_A), 0.02),
        "sgu_ln_g": 1.0 + nrm((L, D_B), 0.02),
        "sgu_ln_b": nrm((L, D_B), 0.02),
        "sgu_w": nrm((L, G_B, CHUNK, CHUNK), 0.5 * CHUNK ** -0.5),
        "sgu_b": 1.0 + nrm((L, G_B, CHUNK), 0.1),
        "w_branch_a": nrm((L, D_A, D), D_A ** -0.5),
        "w_branch_b": nrm((L, D_B, D), D_B ** -0.5),
        "w_out": nrm((L, D, D), BETA * D ** -0.5),
        "ln1_g": 1.0 + nrm((L, D), 0.02),
        "ln1_b": nrm((L, D), 0.02),
        "router_w": nrm((L, D, N_EXP), D ** -0.5),
        "exp_w1": nrm((L, N_EXP, D, D_FF), D ** -0.5),
        "exp_w3": nrm((L, N_EXP, D, D_FF), D ** -0.5),
        "exp_w2": nrm((L, N_EXP, D_FF, D), BETA * D_FF ** -0.5),
        "ln2_g": 1.0 + nrm((L, D), 0.02),
        "ln2_b": nrm((L, D), 0.02),
    }


def reference(x, c, ctx, c_ctx, ada_w, ada_b, w_in, shift_conv, w0, w2, a0, a2, g2, k_k, k_a, r_k,
              lnx_g, lnx_b, sgu_ln_g, sgu_ln_b, sgu_w, sgu_b, w_branch_a, w_branch_b, w_out,
              ln1_g, ln1_b, router_w, exp_w1, exp_w3, exp_w2, ln2_g, ln2_b):
    B, T, _ = x.shape
    rows = T // GRID_W
    lat_chunks = rows * GRID_W // CHUNK
    ctx_chunks = ctx.shape[1] // CHUNK
    s0 = jnp.zeros((B, H_A, HEAD_A, HEAD_A), jnp.float32)
    for l in range(DEPTH):
        p = dict(w_in=w_in[l], shift_conv=shift_conv[l], w0=w0[l], w2=w2[l], a0=a0[l], a2=a2[l],
                 g2=g2[l], k_k=k_k[l], k_a=k_a[l], r_k=r_k[l], lnx_g=lnx_g[l], lnx_b=lnx_b[l],
                 sgu_ln_g=sgu_ln_g[l], sgu_ln_b=sgu_ln_b[l], sgu_w=sgu_w[l], sgu_b=sgu_b[l],
                 w_branch_a=w_branch_a[l], w_branch_b=w_branch_b[l], w_out=w_out[l],
                 router_w=router_w[l], exp_w1=exp_w1[l], exp_w3=exp_w3[l], exp_w2=exp_w2[l])
        mod_x = (jax.nn.silu(c) @ ada_w[l] + ada_b[l])[:, None, :]
        mod_c = (jax.nn.silu(c_ctx) @ ada_w[l] + ada_b[l])[None, None, :]
        sh1, sc1, gt1, sh2, sc2, gt2 = jnp.split(mod_x, 6, axis=-1)
        csh1, csc1, cgt1, csh2, csc2, cgt2 = jnp.split(mod_c, 6, axis=-1)

        hc = modulate(ctx, csh1, csc1)
        if l == DEPTH - 1:
            s_f, s_b = context_scan_states(hc, p, s0)
        else:
            yc, s_f, s_b = token_mixer(hc, p, s0, s0, ctx_chunks)
            ctx = layer_norm(ALPHA * ctx + cgt1 * yc, ln1_g[l], ln1_b[l])
            ctx = layer_norm(ALPHA * ctx + cgt2 * expert_choice_ffn(modulate(ctx, csh2, csc2), p),
                             ln2_g[l], ln2_b[l])

        yx, _, _ = token_mixer(modulate(x, sh1, sc1), p, s_f, s_b, lat_chunks)
        x = layer_norm(ALPHA * x + gt1 * yx, ln1_g[l], ln1_b[l])
        x = layer_norm(ALPHA * x + gt2 * expert_choice_ffn(modulate(x, sh2, sc2), p),
                       ln2_g[l], ln2_b[l])
    return x
```

```python
from contextlib import ExitStack
import numpy as np
import concourse.bass as bass
import concourse.mybir as mybir
from concourse.bass_utils import run_bass_kernel_spmd

F32 = mybir.dt.float32
F32R = mybir.dt.float32r
BF16 = mybir.dt.bfloat16
I32 = mybir.dt.int32
U32 = mybir.dt.uint32
AF = mybir.ActivationFunctionType
ALU = mybir.AluOpType
AX = mybir.AxisListType

D = 1024
DEPTH = 2
NCORE = 8
BL = 2
N_RW = 3456
N_IN = 7552
H_A = 16
NEXP = 16
DFF = 2048
ALPHA = (2 * DEPTH) ** 0.25
LN_EPS = 1e-5
GN_EPS = 64e-5

ENGS = ("pe", "act", "dve", "pool", "sp")
EPOCH = 60000
DMA_RING = 8


class Prog:
    def __init__(self, nc, es, nsem=90):
        self.nc = nc
        self.free_sems = [es.enter_context(nc.semaphore(f"s{i}")) for i in range(nsem)]
        self.ops = {e: [] for e in ENGS}
        self.psem = {e: self.free_sems.pop() for e in ENGS}
        self.pcnt = {e: 0 for e in ENGS}
        self.dsem = {e: [self.free_sems.pop() for _ in range(DMA_RING)] for e in ("sp", "act", "pool")}
        self.dcnt = {e: 0 for e in ("sp", "act", "pool")}
        self.last_write = {}
        self.readers = {}
        self.waited = {e: {} for e in ENGS}
        self.ninst = 0

    def _deps(self, eng, reads, writes):
        toks = []
        for k in reads:
            t = self.last_write.get(k)
            if t is not None:
                toks.append(t)
        for k in writes:
            t = self.last_write.get(k)
            if t is not None:
                toks.append(t)
            toks.extend(self.readers.get(k, ()))
        if eng == "pe":
            ps = self.psem["pe"]
            toks = [t for t in toks if t[0] is not ps]
        return toks

    def _commit(self, tok, reads, writes):
        for k in reads:
            self.readers.setdefault(k, []).append(tok)
        for k in writes:
            self.last_write[k] = tok
            self.readers[k] = []

    def _waits(self, eng, toks):
        best = {}
        for (s, v) in toks:
            key = id(s)
            if key not in best or best[key][1] < v:
                best[key] = (s, v)
        out = []
        w = self.waited[eng]
        for key, (s, v) in best.items():
            if w.get(key, 0) >= v:
                continue
            w[key] = v
            out.append((s, v))
        return out

    def op(self, eng, fn, reads=(), writes=()):
        if self.pcnt[eng] >= EPOCH:
            self.psem[eng] = self.free_sems.pop()
            self.pcnt[eng] = 0
        toks = self._deps(eng, reads, writes)
        waits = self._waits(eng, toks)
        self.pcnt[eng] += 1
        tok = (self.psem[eng], self.pcnt[eng])
        self.ops[eng].append((fn, waits, (tok[0], 1)))
        self._commit(tok, reads, writes)
        return tok

    def dma(self, q, fn, reads=(), writes=()):
        i = self.dcnt[q]
        self.dcnt[q] += 1
        sem = self.dsem[q][i % DMA_RING]
        toks = self._deps(q, reads, writes)
        if i >= DMA_RING:
            toks.append((sem, 16 * (i // DMA_RING)))
        waits = self._waits(q, toks)
        tok = (sem, 16 * (i // DMA_RING + 1))
        self.ops[q].append((fn, waits, (sem, 16)))
        self._commit(tok, reads, writes)
        return tok

    def end_phase(self):
        toks = []
        for e in ENGS:
            if self.pcnt[e] > 0:
                toks.append((self.psem[e], self.pcnt[e]))
        for q in self.dcnt:
            n = self.dcnt[q]
            for j in range(min(n, DMA_RING)):
                last = ((n - 1 - j) // DMA_RING) * DMA_RING + j
                toks.append((self.dsem[q][j], 16 * (last // DMA_RING + 1)))
        self.ops["sp"].append((None, self._waits("sp", toks), None))
        nc = self.nc
        ops = self.ops
        me = self

        def run(engobj, lst):
            for fn, waits, inc in lst:
                for (s, v) in waits:
                    engobj.wait_ge(s, v)
                if fn is None:
                    continue
                ins = fn(engobj)
                ins.then_inc(inc[0], inc[1])
                me.ninst += 1

        with nc.Block() as block:
            @block.sync
            def _(e):
                run(e, ops["sp"])

            @block.scalar
            def _(e):
                run(e, ops["act"])

            @block.vector
            def _(e):
                run(e, ops["dve"])

            @block.gpsimd
            def _(e):
                run(e, ops["pool"])

            @block.tensor
            def _(e):
                run(e, ops["pe"])
        self.ops = {e: [] for e in ENGS}
        self.last_write = {}
        self.readers = {}
        for q in self.dcnt:
            if self.dcnt[q] // DMA_RING > 2500:
                self.dsem[q] = [self.free_sems.pop() for _ in range(DMA_RING)]
                self.dcnt[q] = 0


def round_robin(gens, width):
    gens = list(gens)
    active = []
    while gens or active:
        while gens and len(active) < width:
            active.append(gens.pop(0))
        for g in list(active):
            try:
                next(g)
            except StopIteration:
                active.remove(g)


class Rot:
    def __init__(self, tiles, name):
        self.tiles = tiles
        self.name = name
        self.i = -1

    def next(self):
        self.i += 1
        j = self.i % len(self.tiles)
        return self.tiles[j], f"{self.name}{j}"


class KB:
    def __init__(self, TC, TL, dbg=()):
        self.TC, self.TL = TC, TL
        self.TB = TC + TL
        self.dbg = set(dbg)
        self.nc = bass.Bass("TRN2", target_bir_lowering=False)
        self.es = ExitStack()
        self.P = Prog(self.nc, self.es)
        self.inputs = {}
        self.drams = {}
        nc = self.nc
        self.ident = self.es.enter_context(nc.sbuf_tensor("ident", [128, 128], F32))
        self.bo = self.es.enter_context(nc.sbuf_tensor("bo", [128, 128], F32))
        self.blocks = []
        for b in range(BL):
            self.blocks.append((b, 0, 0, TC))
            for t0 in range(0, TL, 512):
                self.blocks.append((b, 1, TC + t0, min(512, TL - t0)))

    def inp(self, name, shape):
        if name not in self.inputs:
            self.inputs[name] = self.nc.dram_tensor(name, list(shape), F32, kind="ExternalInput").ap()
        return self.inputs[name]

    def dram(self, name, shape, dt=F32):
        if name not in self.drams:
            kind = "ExternalOutput" if name in self.dbg else "Internal"
            self.drams[name] = self.nc.dram_tensor(name, list(shape), dt, kind=kind).ap()
        return self.drams[name]

    def uname(self, name):
        self.uid = getattr(self, "uid", 0) + 1
        return f"{name}_u{self.uid}"

    def sb(self, ph, name, shape, dt=F32):
        return ph.enter_context(self.nc.sbuf_tensor(self.uname(name), list(shape), dt))

    def rot(self, ph, name, shape, n, dt=F32, psum=False):
        if psum:
            tiles = [ph.enter_context(self.nc.psum_tensor(self.uname(f"{name}{i}"), list(shape), dt)) for i in range(n)]
        else:
            tiles = [ph.enter_context(self.nc.sbuf_tensor(self.uname(f"{name}{i}"), list(shape), dt)) for i in range(n)]
        return Rot(tiles, name)

    def dma(self, out, in_, R, W, q="sp", **kw):
        self.P.dma(q, lambda e: e.dma_start(out=out, in_=in_, **kw), reads=R, writes=W)

    def mm(self, out, lhsT, rhs, start, stop, R, W):
        self.P.op("pe", lambda e: e.matmul(out, lhsT=lhsT, rhs=rhs, start=start, stop=stop), reads=R, writes=W)

    def tr(self, out, in_, R, W, ident=None):
        idn = self.ident[0:in_.shape[0], 0:in_.shape[0]] if ident is None else ident
        self.P.op("pe", lambda e: e.transpose(out, in_, idn), reads=list(R) + ["ident"], writes=W)

    def act(self, out, in_, func, R, W, bias=None, scale=1.0):
        kw = {}
        if bias is not None:
            kw["bias"] = bias
        self.P.op("act", lambda e: e.activation(out=out, in_=in_, func=func, scale=scale, **kw), reads=R, writes=W)

    def ts(self, out, in0, s1, s2, op0, op1, R, W, eng="dve"):
        self.P.op(eng, lambda e: e.tensor_scalar(out=out, in0=in0, scalar1=s1, scalar2=s2, op0=op0, op1=op1) if op1 is not None
                  else e.tensor_scalar(out=out, in0=in0, scalar1=s1, scalar2=None, op0=op0), reads=R, writes=W)

    def tt(self, out, in0, in1, op, R, W, eng="dve"):
        self.P.op(eng, lambda e: e.tensor_tensor(out=out, in0=in0, in1=in1, op=op), reads=R, writes=W)

    def stt(self, out, in0, scalar, in1, op0, op1, R, W):
        self.P.op("dve", lambda e: e.scalar_tensor_tensor(out=out, in0=in0, scalar=scalar, in1=in1, op0=op0, op1=op1), reads=R, writes=W)

    def cp(self, out, in_, R, W, eng="dve"):
        if eng == "act":
            self.P.op("act", lambda e: e.copy(out=out, in_=in_), reads=R, writes=W)
        else:
            self.P.op(eng, lambda e: e.tensor_copy(out=out, in_=in_), reads=R, writes=W)

    def memset(self, ap, val, W, eng="dve"):
        self.P.op(eng, lambda e: e.memset(ap, val), reads=(), writes=W)

    def phase_consts(self):
        nc, P = self.nc, self.P
        with ExitStack() as ph:
            ones = self.sb(ph, "ones_i", [128, 128])
            self.memset(ones[:], 1.0, ["ones_i"])
            self.memset(self.ident[:], 0.0, ["ident"])
            self.memset(self.bo[:], 0.0, ["bo"])
            P.op("pool", lambda e: e.affine_select(out=self.ident[:], in_=ones[:], pattern=[[-1, 128]], compare_op=ALU.is_equal,
                                                   fill=0.0, base=0, channel_multiplier=1),
                 reads=["ones_i", "ident"], writes=["ident"])
            self.memset(self.bo[0:64, 0:64], 1.0, ["bo"])
            self.memset(self.bo[64:128, 64:128], 1.0, ["bo"])
            P.end_phase()

    def phase_init(self):
        nc, P = self.nc, self.P
        TC, TL, TB = self.TC, self.TL, self.TB
        x = self.inp("x", [BL, TL, D])
        ctx = self.inp("ctx", [BL, TC, D])
        xres = self.dram("xres", [BL, TB, D])
        self.phase_consts()
        for b in range(BL):
            self.dma(xres[b, 0:TC, :], ctx[b], [], [])
            self.dma(xres[b, TC:TB, :], x[b], [], [])
        P.end_phase()

    def phase_mod(self, l):
        nc, P = self.nc, self.P
        c = self.inp("c", [BL, D])
        cc = self.inp("c_ctx", [1, D])
        ada_w = self.inp("ada_w", [DEPTH, D, 6 * D])
        ada_b = self.inp("ada_b", [DEPTH, 6 * D])
        modrow = self.dram(f"modrow{l}", [3, 6 * D])
        with ExitStack() as ph, nc.allow_non_contiguous_dma(reason="small param transposes"):
            cT = self.sb(ph, "cT", [128, 8, 3])
            scT = self.sb(ph, "scT", [128, 8, 3])
            abt = self.sb(ph, "abt", [3, 6 * D])
            mrow = self.sb(ph, "mrow", [3, 6 * D])
            wr = self.rot(ph, "adaw", [128, 8, 512], 3)
            pr = self.rot(ph, "pmod", [3, 512], 2, psum=True)
            self.dma(cT[:, :, 0], cc[0].rearrange("(c p) -> p c", p=128), [], ["cT"])
            for b in range(BL):
                self.dma(cT[:, :, 1 + b], c[b].rearrange("(c p) -> p c", p=128), [], ["cT"])
            self.dma(abt[:], ada_b[l].partition_broadcast(3), [], ["abt"])
            self.act(scT[:], cT[:], AF.Silu, ["cT"], ["scT"])
            for nb in range(12):
                w, wk = wr.next()
                self.dma(w[:], ada_w[l, :, nb * 512:(nb + 1) * 512].rearrange("(c p) n -> p c n", p=128), [], [wk])
                pt, pk = pr.next()
                for dc in range(8):
                    self.mm(pt[:], scT[:, dc, :], w[:, dc, :], dc == 0, dc == 7, ["scT", wk], [pk])
                self.tt(mrow[:, nb * 512:(nb + 1) * 512], pt[:], abt[:, nb * 512:(nb + 1) * 512], ALU.add, [pk, "abt"], ["mrow"])
            self.dma(modrow[:, :], mrow[:], ["mrow"], [])
            P.end_phase()

    def ln_tile(self, xt, xk, xn, xnk, st, stk, eps=LN_EPS):
        P = self.P
        for h in range(2):
            P.op("dve", lambda e, h=h: e.bn_stats(out=st[:, 6 * h:6 * h + 6], in_=xt[:, 512 * h:512 * h + 512]), reads=[xk], writes=[stk])
        P.op("dve", lambda e: e.bn_aggr(out=st[:, 12:14], in_=st[:, 0:12]), reads=[stk], writes=[stk])
        self.ts(st[:, 14:15], st[:, 13:14], eps, None, ALU.add, None, [stk], [stk])
        self.act(st[:, 14:15], st[:, 14:15], AF.Sqrt, [stk], [stk])
        P.op("dve", lambda e: e.reciprocal(out=st[:, 14:15], in_=st[:, 14:15]), reads=[stk], writes=[stk])
        self.ts(xn, xt[:], st[:, 12:13], st[:, 14:15], ALU.subtract, ALU.mult, [xk, stk], [xnk])

    def load_modfm(self, ph, l):
        modrow = self.dram(f"modrow{l}", [3, 6 * D])
        modfm = self.sb(ph, "modfm", [128, 3, 6, 8])
        for j in range(3):
            self.dma(modfm[:, j], modrow[j].rearrange("(s c p) -> p s c", p=128, c=8), [], ["modfm"])
        return modfm

    def phase_proj(self, l):
        nc, P = self.nc, self.P
        TC, TL, TB = self.TC, self.TL, self.TB
        w_in = self.inp("w_in", [DEPTH, D, N_IN])
        xres = self.dram("xres", [BL, TB, D])
        ZR = self.dram("ZR", [27, 128, BL, TB + 2])
        UT = self.dram("UT", [8, 128, BL, TB])
        GT = self.dram("GT", [16, 128, BL, TB])
        VS = self.dram("VS", [BL, TB, D])
        cblocks = [(i * 512, 512) for i in range(6)] + [(3072, 384)] + [(3456 + i * 512, 512) for i in range(8)]
        with ExitStack() as ph, nc.allow_non_contiguous_dma(reason="small param transposes"):
            modfm = self.load_modfm(ph, l)
            ops1 = self.sb(ph, "ops1", [128, 3, 8])
            self.ts(ops1[:], modfm[:, :, 1, :], 1.0, None, ALU.add, None, ["modfm"], ["ops1"])
            xr = self.rot(ph, "xt", [128, D], 2)
            xnb = self.sb(ph, "xnb", [128, 4, D])
            st = self.rot(ph, "st", [128, 16], 2)
            hT = self.sb(ph, "hT", [128, 8, BL * TB], BF16)
            wr = self.rot(ph, "wblk", [128, 8, 512], 2, dt=BF16)
            wst = self.rot(ph, "wstg", [128, 8, 512], 3)
            ptr = self.rot(ph, "ptr", [128, 512], 2, psum=True)
            pmm = self.rot(ph, "pmm", [128, 512], 4, psum=True)
            stg = self.rot(ph, "stg", [128, 512], 6)
            for (b, kind, t0, L) in self.blocks:
                j = 0 if kind == 0 else 1 + b
                nt = L // 128
                off = b * TB + t0
                for i in range(nt):
                    xt, xk = xr.next()
                    s_, sk = st.next()
                    self.dma(xt[:], xres[b, t0 + i * 128:t0 + (i + 1) * 128, :], [], [xk])
                    self.ln_tile(xt, xk, xnb[:, i, :], f"xnb{i}", s_, sk)
                for dc in range(8):
                    pt, pk = ptr.next()
                    for i in range(nt):
                        self.tr(pt[:, i * 128:(i + 1) * 128], xnb[:, i, dc * 128:(dc + 1) * 128], [f"xnb{i}"], [pk])
                    self.act(hT[:, dc, off:off + L], pt[:, 0:L], AF.Identity, [pk, "ops1", "modfm"], ["hT"],
                             bias=modfm[:, j, 0, dc:dc + 1], scale=ops1[:, j, dc:dc + 1])
            for (c0, cw) in cblocks:
                w, wk = wr.next()
                ws, wsk = wst.next()
                self.dma(ws[:, :, 0:cw], w_in[l, :, c0:c0 + cw].rearrange("(c p) n -> p c n", p=128), [], [wsk])
                self.cp(w[:, :, 0:cw], ws[:, :, 0:cw], [wsk], [wk], eng="pool")
                for (b, kind, t0, L) in self.blocks:
                    nt = L // 128
                    off = b * TB + t0
                    if 4480 <= c0 < 5504:
                        for i in range(nt):
                            pm, pmk = pmm.next()
                            for dc in range(8):
                                self.mm(pm[:, 0:cw], hT[:, dc, off + i * 128:off + (i + 1) * 128], w[:, dc, 0:cw],
                                        dc == 0, dc == 7, ["hT", wk], [pmk])
                            sg, sgk = stg.next()
                            self.act(sg[:, 0:cw], pm[:, 0:cw], AF.Gelu, [pmk], [sgk])
                            self.dma(VS[b, t0 + i * 128:t0 + (i + 1) * 128, c0 - 4480:c0 - 4480 + cw], sg[:, 0:cw], [sgk], [])
                        continue
                    for cc in range(cw // 128):
                        col = c0 + cc * 128
                        pm, pmk = pmm.next()
                        for dc in range(8):
                            self.mm(pm[:, 0:L], w[:, dc, cc * 128:(cc + 1) * 128], hT[:, dc, off:off + L],
                                    dc == 0, dc == 7, ["hT", wk], [pmk])
                        sg, sgk = stg.next()
                        if col < N_RW:
                            self.cp(sg[:, 0:L], pm[:, 0:L], [pmk], [sgk])
                            self.dma(ZR[col // 128, :, b, 1 + t0:1 + t0 + L], sg[:, 0:L], [sgk], [])
                        elif col < 4480:
                            self.act(sg[:, 0:L], pm[:, 0:L], AF.Gelu, [pmk], [sgk])
                            self.dma(UT[(col - N_RW) // 128, :, b, t0:t0 + L], sg[:, 0:L], [sgk], [])
                        else:
                            self.act(sg[:, 0:L], pm[:, 0:L], AF.Sigmoid, [pmk], [sgk])
                            self.dma(GT[(col - 5504) // 128, :, b, t0:t0 + L], sg[:, 0:L], [sgk], [])
            P.end_phase()

    def phase_prep(self, l):
        nc, P = self.nc, self.P
        TC, TL, TB = self.TC, self.TL, self.TB
        shc = self.inp("shift_conv", [DEPTH, 3, N_RW])
        w0 = self.inp("w0", [DEPTH, 2, D]); w2 = self.inp("w2", [DEPTH, 2, 64, D])
        a0 = self.inp("a0", [DEPTH, 2, D]); a2 = self.inp("a2", [DEPTH, 2, 64, D])
        g2 = self.inp("g2", [DEPTH, 128, D])
        k_k = self.inp("k_k", [DEPTH, D]); k_a = self.inp("k_a", [DEPTH, D]); r_k = self.inp("r_k", [DEPTH, H_A, 64])
        ZR = self.dram("ZR", [27, 128, BL, TB + 2])
        SC = self.dram("SC", [9, 16, 128, TB])
        BON = self.dram("BON", [8, 128, BL, TB])
        GG = self.dram("GG", [8, 128, BL, TB])
        TM = self.dram("TM", [5, BL, TB, D], BF16)
        GE = self.dram("GE", [2, 16, 128, TB // 16])
        RL = self.dram("RL", [16, 128, TB // 16])
        with ExitStack() as ph, nc.allow_non_contiguous_dma(reason="small param transposes"):
            ptm = self.rot(ph, "ptm", [128, 512], 2, psum=True)
            stm = self.rot(ph, "stm", [128, 512], 3, dt=BF16)

            def to_tm(op, src, srck, b, p, t0, L):
                pt, pk = ptm.next()
                nt = L // 128
                for i in range(nt):
                    self.tr(pt[:, i * 128:(i + 1) * 128], src[:, i * 128:(i + 1) * 128], [srck], [pk])
                st_, stk_ = stm.next()
                self.cp(st_[:, 0:L], pt[:, 0:L], [pk], [stk_], eng="act")
                self.dma(TM[op, b, t0:t0 + L, p * 128:(p + 1) * 128].rearrange("(i t) c -> t i c", t=128),
                         st_[:, 0:L].rearrange("t (i c) -> t i c", c=128), [stk_], [])
            scv = self.sb(ph, "scv", [128, 3, 27])
            w0n = self.sb(ph, "w0n", [128, 2, 8]); a0t = self.sb(ph, "a0t", [128, 2, 8])
            kkw = self.sb(ph, "kkw", [128, 8]); kaw = self.sb(ph, "kaw", [128, 8]); rkw = self.sb(ph, "rkw", [128, 8])
            w2t = self.sb(ph, "w2t", [128, D]); a2t = self.sb(ph, "a2t", [128, D]); g2t = self.sb(ph, "g2t", [128, D])
            self.dma(scv[:], shc[l].rearrange("j (c p) -> p j c", p=128), [], ["scv"])
            self.dma(w0n[:], w0[l].rearrange("j (c p) -> p j c", p=128), [], ["w0n"])
            self.ts(w0n[:], w0n[:], -1.0, None, ALU.mult, None, ["w0n"], ["w0n"])
            self.dma(a0t[:], a0[l].rearrange("j (c p) -> p j c", p=128), [], ["a0t"])
            self.dma(kkw[:], k_k[l].rearrange("(c p) -> p c", p=128), [], ["kkw"])
            self.dma(kaw[:], k_a[l].rearrange("(c p) -> p c", p=128), [], ["kaw"])
            self.dma(rkw[:], r_k[l].rearrange("h k -> (h k)").rearrange("(c p) -> p c", p=128), [], ["rkw"])
            for j in range(2):
                self.dma(w2t[64 * j:64 * j + 64, :].bitcast(F32R), w2[l, j], [], ["w2t"], q="pool")
                self.dma(a2t[64 * j:64 * j + 64, :].bitcast(F32R), a2[l, j], [], ["a2t"], q="pool")
            self.dma(g2t[:].bitcast(F32R), g2[l], [], ["g2t"], q="pool")
            pools = {}

            ONE = {"zdw", "zda", "zdg", "dw", "da", "dg", "tdw", "dar", "sgg", "ge0", "ge1", "rsf"}

            def T(name, w=512):
                if name not in pools:
                    pools[name] = self.rot(ph, "p_" + name, [128, w], 1 if name in ONE else 2)
                return pools[name].next()
            psr = self.rot(ph, "pps", [128, 512], 5, psum=True)
            zer5 = self.sb(ph, "zer5", [128, 512])
            self.memset(zer5[:], 0.0, ["zer5"])

            def load_shift(c, b, t0, L, lz, rz, name, f32r=False):
                zt, zk = T("z" + name, 514)
                self.dma(zt[:, 0:L + 2], ZR[c, :, b, t0:t0 + L + 2], [], [zk])
                if lz:
                    self.memset(zt[:, 0:1], 0.0, [zk])
                if rz:
                    self.memset(zt[:, L + 1:L + 2], 0.0, [zk])
                o, ok = T(name)
                self.ts(o[:, 0:L], zt[:, 1:L + 1], scv[:, 1, c:c + 1], None, ALU.mult, None, [zk, "scv"], [ok])
                self.stt(o[:, 0:L], zt[:, 0:L], scv[:, 0, c:c + 1], o[:, 0:L], ALU.mult, ALU.add, [zk, "scv", ok], [ok])
                self.stt(o[:, 0:L], zt[:, 2:L + 2], scv[:, 2, c:c + 1], o[:, 0:L], ALU.mult, ALU.add, [zk, "scv", ok], [ok])
                return o, ok

            for (b, kind, t0, L) in self.blocks:
                lz = (kind == 0) or (t0 == TC)
                rz = (kind == 0) or (t0 + L == TB)
                dw, dwk = load_shift(24, b, t0, L, lz, rz, "dw")
                da, dak = load_shift(25, b, t0, L, lz, rz, "da")
                dg, dgk = load_shift(26, b, t0, L, lz, rz, "dg")
                tdw, tdwk = T("tdw"); dar, dark = T("dar"); sgg, sggk = T("sgg")
                self.act(tdw[:, 0:L].bitcast(F32R), dw[:, 0:L], AF.Tanh, [dwk], [tdwk])
                self.cp(dar[:, 0:L].bitcast(F32R), da[:, 0:L], [dak], [dark], eng="pool")
                self.act(sgg[:, 0:L].bitcast(F32R), dg[:, 0:L], AF.Sigmoid, [dgk], [sggk])
                def pair_gen(p, b=b, t0=t0, L=L, lz=lz, rz=rz, tdw=tdw, tdwk=tdwk, dar=dar, dark=dark, sgg=sgg, sggk=sggk):
                    g = b * 8 + p
                    cs = slice(p * 128, (p + 1) * 128)
                    r, rk_ = load_shift(p, b, t0, L, lz, rz, "r")
                    k, kk_ = load_shift(8 + p, b, t0, L, lz, rz, "k")
                    v, vk_ = load_shift(16 + p, b, t0, L, lz, rz, "v")
                    self.dma(RL[g, :, t0 // 16:(t0 + L) // 16], r[:, 0:L].rearrange("p (n j) -> p n j", j=16)[:, :, 15], [rk_], [])
                    to_tm(4, v, vk_, b, p, t0, L)
                    kkr, kkrk = T("kkr"); sq, sqk = T("sq")
                    self.ts(kkr[:, 0:L], k[:, 0:L], kkw[:, p:p + 1], None, ALU.mult, None, [kk_, "kkw"], [kkrk])
                    self.act(sq[:, 0:L], kkr[:, 0:L], AF.Square, [kkrk], [sqk])
                    yield
                    ps, psk = psr.next()
                    self.mm(ps[:, 0:L], self.bo[:], sq[:, 0:L], True, True, ["bo", sqk], [psk])
                    rn, rnk = T("rn")
                    yield
                    self.ts(rn[:, 0:L], ps[:, 0:L], 1e-12, None, ALU.max, None, [psk], [rnk])
                    yield
                    self.act(rn[:, 0:L], rn[:, 0:L], AF.Ln, [rnk], [rnk])
                    self.act(rn[:, 0:L], rn[:, 0:L], AF.Exp, [rnk], [rnk], scale=-0.5)
                    yield
                    kap, kapk = T("kap")
                    self.tt(kap[:, 0:L], kkr[:, 0:L], rn[:, 0:L], ALU.mult, [kkrk, rnk], [kapk])
                    kA, kAk = T("kA")
                    self.ts(kA[:, 0:L], k[:, 0:L], kaw[:, p:p + 1], None, ALU.mult, None, [kk_, "kaw"], [kAk])
                    kf = None
                    for dr in range(2):
                        rows = slice(64 * dr, 64 * dr + 64)
                        ps, psk = psr.next()
                        self.mm(ps[:, 0:L], w2t[rows, cs].bitcast(F32R), tdw[rows, 0:L].bitcast(F32R), True, True, ["w2t", tdwk], [psk])
                        yield
                        e1, e1k = T(f"e1{dr}")
                        self.act(e1[:, 0:L], ps[:, 0:L], AF.Exp, [psk, "w0n"], [e1k], bias=w0n[:, dr, p:p + 1], scale=-1.0)
                        self.ts(e1[:, 0:L], e1[:, 0:L], 1.0, None, ALU.add, None, [e1k], [e1k])
                        P.op("dve", lambda e, o=e1[:, 0:L]: e.reciprocal(out=o, in_=o), reads=[e1k], writes=[e1k])
                        yield
                        nb = L // 16
                        ld, ldk = T(f"ld{dr}")
                        self.ts(ld[:, 0:L], e1[:, 0:L], -float(np.exp(-0.5)), None, ALU.mult, None, [e1k], [ldk])
                        cs_, csk = T(f"cs{dr}")
                        P.op("dve", lambda e, o=cs_[:, 0:L], i0=ld[:, 0:L], i1=zer5[:, 0:L]: e.tensor_tensor_scan(
                            out=o, data0=i0, data1=i1, initial=0.0, op0=ALU.add, op1=ALU.add), reads=[ldk, "zer5"], writes=[csk])
                        cl, clk = T(f"cl{dr}")
                        c3 = cs_[:, 0:L].rearrange("p (n j) -> p n j", j=16)
                        l3 = cl[:, 0:L].rearrange("p (n j) -> p n j", j=16)
                        self.cp(l3[:, 0, :], c3[:, 0, :], [csk], [clk])
                        if nb > 1:
                            self.tt(l3[:, 1:nb, :], c3[:, 1:nb, :], c3[:, 0:nb - 1, 15:16].broadcast_to([128, nb - 1, 16]), ALU.subtract, [csk], [clk])
                        gx, gxk = T(f"gx{dr}"); gi, gik = T(f"gi{dr}")
                        gev, gevk = T(f"ge{dr}")
                        if dr == 0:
                            self.tt(gx[:, 0:L], cl[:, 0:L], ld[:, 0:L], ALU.subtract, [clk, ldk], [gxk])
                            self.act(gx[:, 0:L], gx[:, 0:L], AF.Exp, [gxk], [gxk])
                            self.act(gi[:, 0:L], cl[:, 0:L], AF.Exp, [clk], [gik], scale=-1.0)
                            rs_, rsk_ = T("rsf")
                            self.act(rs_[:, 0:L], cl[:, 0:L], AF.Exp, [clk], [rsk_])
                            self.cp(gev[:, 0:nb], rs_[:, 0:L].rearrange("p (n j) -> p n j", j=16)[:, :, 15], [rsk_], [gevk])
                        else:
                            g3 = gx[:, 0:L].rearrange("p (n j) -> p n j", j=16)
                            self.tt(g3, l3[:, :, 15:16].broadcast_to([128, nb, 16]), l3, ALU.subtract, [clk], [gxk])
                            self.tt(gi[:, 0:L], gx[:, 0:L], ld[:, 0:L], ALU.add, [gxk, ldk], [gik])
                            self.act(gx[:, 0:L], gx[:, 0:L], AF.Exp, [gxk], [gxk])
                            self.act(gi[:, 0:L], gi[:, 0:L], AF.Exp, [gik], [gik], scale=-1.0)
                            rs_, rsk_ = gx, gxk
                            self.act(gev[:, 0:nb], l3[:, :, 15], AF.Exp, [clk], [gevk])
                        self.dma(GE[dr, g, :, t0 // 16:(t0 + L) // 16], gev[:, 0:nb], [gevk], [])
                        kt_, ktk_ = T(f"kt{dr}"); rt_, rtk_ = T(f"rt{dr}")
                        self.tt(kt_[:, 0:L], kap[:, 0:L], gx[:, 0:L], ALU.mult, [kapk, gxk], [ktk_], eng="pool")
                        self.tt(rt_[:, 0:L], r[:, 0:L], rs_[:, 0:L], ALU.mult, [rk_, rsk_], [rtk_], eng="pool")
                        self.dma(SC[2 * dr, g, :, t0:t0 + L], kt_[:, 0:L], [ktk_], [])
                        self.dma(SC[2 * dr + 1, g, :, t0:t0 + L], rt_[:, 0:L], [rtk_], [])
                        yield
                        ps, psk = psr.next()
                        self.mm(ps[:, 0:L], a2t[rows, cs].bitcast(F32R), dar[rows, 0:L].bitcast(F32R), True, True, ["a2t", dark], [psk])
                        yield
                        aa, aak = T(f"aa{dr}")
                        self.act(aa[:, 0:L], ps[:, 0:L], AF.Sigmoid, [psk, "a0t"], [aak], bias=a0t[:, dr, p:p + 1])
                        yield
                        kd, kdk = T(f"kd{dr}")
                        self.stt(kd[:, 0:L], aa[:, 0:L], -1.0, kA[:, 0:L], ALU.add, ALU.mult, [aak, kAk], [kdk])
                        self.tt(kd[:, 0:L], kd[:, 0:L], k[:, 0:L], ALU.add, [kdk, kk_], [kdk])
                        kdsc, kdsck = T(f"kdsc{dr}")
                        self.tt(kdsc[:, 0:L], kd[:, 0:L], gi[:, 0:L], ALU.mult, [kdk, gik], [kdsck], eng="pool")
                        to_tm(1 + 2 * dr, kdsc, kdsck, b, p, t0, L)
                        na, nak = T(f"na{dr}")
                        self.stt(na[:, 0:L], kap[:, 0:L], -1.0, aa[:, 0:L], ALU.mult, ALU.mult, [kapk, aak], [nak])
                        self.tt(na[:, 0:L], na[:, 0:L], gi[:, 0:L], ALU.mult, [nak, gik], [nak])
                        to_tm(2 * dr, na, nak, b, p, t0, L)
                        if dr == 0:
                            kf, kfk = kd, kdk
                    yield
                    t1, t1k = T("t1")
                    self.stt(t1[:, 0:L], r[:, 0:L], rkw[:, p:p + 1], kf[:, 0:L], ALU.mult, ALU.mult, [rk_, "rkw", kfk], [t1k])
                    ps, psk = psr.next()
                    self.mm(ps[:, 0:L], self.bo[:], t1[:, 0:L], True, True, ["bo", t1k], [psk])
                    yield
                    bn, bnk = T("bn")
                    self.tt(bn[:, 0:L], ps[:, 0:L], v[:, 0:L], ALU.mult, [psk, vk_], [bnk])
                    self.dma(BON[p, :, b, t0:t0 + L], bn[:, 0:L], [bnk], [])
                    ps, psk = psr.next()
                    self.mm(ps[:, 0:L], g2t[:, cs].bitcast(F32R), sgg[:, 0:L].bitcast(F32R), True, True, ["g2t", sggk], [psk])
                    gt, gtk = T("gt")
                    self.cp(gt[:, 0:L], ps[:, 0:L], [psk], [gtk], eng="act")
                    self.dma(GG[p, :, b, t0:t0 + L], gt[:, 0:L], [gtk], [])
                round_robin([pair_gen(p) for p in range(8)], 2)
            P.end_phase()

    def phase_scan(self, l, TBs=16, max_steps=None, skip=()):
        nc, P = self.nc, self.P
        BF = mybir.dt.bfloat16
        TC, TL, TB = self.TC, self.TL, self.TB
        SC = self.dram("SC", [9, 16, 128, TB])
        TM = self.dram("TM", [5, BL, TB, D], BF16)
        YD = self.dram("YD", [2, 2, TB, D])
        GE = self.dram("GE", [2, 16, 128, TB // 16]); RL = self.dram("RL", [16, 128, TB // 16])
        assert TC % TBs == 0 and TL % TBs == 0 and TBs == 16
        NBK = TB // 16
        with ExitStack() as ph:
            mask = self.sb(ph, "mask64", [64, 16, 64])
            m16 = self.sb(ph, "m16", [64, 16])
            sel0 = self.sb(ph, "sel0", [96, 32])
            sel = self.sb(ph, "sel", [96, 32], BF)
            mask96 = self.sb(ph, "mask96", [128, 16, 64])
            self.tt(m16[:], self.ident[0:64, 0:16], self.ident[0:64, 16:32], ALU.add, ["ident"], ["m16"])
            self.tt(m16[:], m16[:], self.ident[0:64, 32:48], ALU.add, ["ident", "m16"], ["m16"])
            self.tt(m16[:], m16[:], self.ident[0:64, 48:64], ALU.add, ["ident", "m16"], ["m16"])
            self.cp(mask[:], m16[:].unsqueeze(2).broadcast_to([64, 16, 64]), ["m16"], ["mask"])
            m16b = self.sb(ph, "m16b", [128, 16])
            self.tt(m16b[64:128], self.ident[64:128, 64:80], self.ident[64:128, 80:96], ALU.add, ["ident"], ["m16b"])
            self.tt(m16b[64:128], m16b[64:128], self.ident[64:128, 96:112], ALU.add, ["ident", "m16b"], ["m16b"])
            self.tt(m16b[64:128], m16b[64:128], self.ident[64:128, 112:128], ALU.add, ["ident", "m16b"], ["m16b"])
            self.cp(mask96[64:128], m16b[64:128].unsqueeze(2).broadcast_to([64, 16, 64]), ["m16b"], ["mask96"])
            self.memset(sel0[:], 0.0, ["sel0"])
            for m in range(2):
                P.op("dve", lambda e, m=m: e.tensor_reduce(out=sel0[64:96, m:m + 1], in_=self.ident[64:96, 64 + 16 * m:80 + 16 * m],
                                                          axis=AX.X, op=ALU.add), reads=["ident", "sel0"], writes=["sel0"])
            self.cp(sel[:], sel0[:], ["sel0"], ["sel"])
            get = self.sb(ph, "get", [128, 2, 16, NBK]); rlt = self.sb(ph, "rlt", [128, 16, NBK])
            for d in range(2):
                self.dma(get[:, d], GE[d].rearrange("g p n -> p g n"), [], ["get"])
            self.dma(rlt[:], RL.rearrange("g p n -> p g n"), [], ["rlt"])
            dirs = []
            for d in range(2):
                t = {}
                t["S"] = [self.sb(ph, f"S{d}{i}", [128, 16, 64]) for i in range(1)]
                t["Sb"] = self.sb(ph, f"Sb{d}", [128, 16, 64], BF)
                t["kst"] = self.rot(ph, f"kst{d}", [128, 16, TBs], 2)
                t["rst"] = self.rot(ph, f"rst{d}", [128, 16, TBs], 2)
                t["L1"] = [self.sb(ph, f"L1{d}{i}", [128, TBs, 128], BF) for i in range(2)]
                t["L2"] = [self.sb(ph, f"L2{d}{i}", [128, TBs, 128], BF) for i in range(2)]
                t["Vs"] = self.rot(ph, f"Vs{d}", [32, TBs, 64], 2, dt=BF)
                t["R1"] = self.rot(ph, f"R1{d}", [128, 16, 64], 2, dt=BF)
                for tl_ in t["R1"].tiles:
                    self.memset(tl_[:], 0.0, [], eng="pool")
                t["yb"] = self.rot(ph, f"yb{d}", [2, 1, D], 3)
                t["P12"] = ph.enter_context(nc.psum_tensor(self.uname(f"P12{d}"), [128, 1024], F32))
                t["Py"] = ph.enter_context(nc.psum_tensor(self.uname(f"Py{d}"), [32, 1024], F32))
                t["L1x"] = self.sb(ph, f"L1x{d}", [128, 128], BF)
                for i in range(2):
                    self.memset(t["L1"][i][:], 0.0, [f"L1{d}{i}"], eng="pool")
                    self.memset(t["L2"][i][:], 0.0, [f"L2{d}{i}"], eng="pool")
                self.memset(t["S"][0][:], 0.0, [f"S{d}0"])
                self.memset(t["Sb"][:], 0.0, [f"Sb{d}"])
                self.memset(t["L1x"][:], 0.0, [f"L1x{d}"])
                t["cur"] = 0
                t["blk"] = -1
                dirs.append(t)

            def load_block(d, tb0):
                t = dirs[d]
                t["blk"] += 1
                i = t["blk"] % 2
                kst, kk = t["kst"].next(); rst, rk = t["rst"].next()
                self.dma(kst[:], SC[2 * d, :, :, tb0:tb0 + TBs].rearrange("g p t -> p g t"), [], [kk])
                self.dma(rst[:], SC[2 * d + 1, :, :, tb0:tb0 + TBs].rearrange("g p t -> p g t"), [], [rk])
                L1, L1k = t["L1"][i], f"L1{d}{i}"
                L2, L2k = t["L2"][i], f"L2{d}{i}"
                Vs, Vsk = t["Vs"].next()
                for m in range(2):
                    rows = slice(64 * m, 64 * m + 64)
                    self.cp(L1[rows, :, 96 + 16 * m:112 + 16 * m], kst[rows].rearrange("p g t -> p t g"), [kk], [L1k], eng="pool")
                    rc = slice(64 + 16 * m, 80 + 16 * m)
                    if d == 0:
                        if tb0 > 0:
                            self.cp(L1[rows, 0, rc], rlt[rows, :, tb0 // 16 - 1], ["rlt"], [L1k], eng="pool")
                        self.cp(L1[rows, 1:TBs, rc], rst[rows, :, 0:TBs - 1].rearrange("p g t -> p t g"), [rk], [L1k], eng="pool")
                    else:
                        self.cp(L1[rows, :, rc], rst[rows].rearrange("p g t -> p t g"), [rk], [L1k], eng="pool")
                    for b in range(BL):
                        r0 = 16 * m + 8 * b
                        cs = slice(64 * m, 64 * m + 64)
                        for wi, op in ((3, 2 * d), (0, 2 * d + 1)):
                            self.dma(L2[32 * wi + r0:32 * wi + r0 + 8, :, cs],
                                     TM[op, b, tb0:tb0 + TBs, :].rearrange("t (pp c) -> pp t c", c=128)[:, :, cs], [], [L2k])
                        self.dma(Vs[r0:r0 + 8, :, :],
                                 TM[4, b, tb0:tb0 + TBs, :].rearrange("t (pp c) -> pp t c", c=128)[:, :, cs], [], [Vsk])
                return dict(L1=L1, L1k=L1k, L2=L2, L2k=L2k, Vs=Vs, Vsk=Vsk, tb0=tb0)

            def stage_vx(d, blk, tc):
                t = dirs[d]
                R1, R1k = t["R1"].next()
                self.tt(R1[0:32], blk["Vs"][:, tc, :].unsqueeze(1).broadcast_to([32, 16, 64]), mask[0:32],
                        ALU.mult, [blk["Vsk"], "mask"], [R1k + "v"], eng="pool")
                return R1, R1k

            def stage_m1(d, L1ap, L1keys, R1=None, R1k=None):
                t = dirs[d]
                Sb, Sbk = t["Sb"], f"Sb{d}"
                P12, Pk = t["P12"], f"P12{d}"
                for h in range(2):
                    self.mm(P12[:, 512 * h:512 * h + 512], L1ap, Sb[:, 8 * h:8 * h + 8, :], True, True, L1keys + [Sbk], [Pk])
                if R1 is None:
                    R1, R1k = t["R1"].next()
                self.tt(R1[64:128], P12[64:128, :].rearrange("p (g v) -> p g v", v=64), mask96[64:128], ALU.mult, [Pk, "mask96"], [R1k + "s"])
                return R1, R1k

            def stage_upd(d, blk, tc, R1, R1k, last):
                t = dirs[d]
                S, Sk = t["S"][0], f"S{d}0"
                P12, Pk = t["P12"], f"P12{d}"
                for h in range(2):
                    cs = slice(512 * h, 512 * h + 512)
                    self.mm(P12[:, cs], blk["L2"][:, tc, :], R1[:, 8 * h:8 * h + 8, :], True, True, [blk["L2k"], R1k + "s", R1k + "v"], [Pk])
                self.tt(S[:], S[:], P12[:, :].rearrange("p (g v) -> p g v", v=64), ALU.add, [Sk, Pk], [Sk])
                if last:
                    bi_ = blk["tb0"] // 16
                    self.tt(S[:], S[:], get[:, d, :, bi_:bi_ + 1].broadcast_to([128, 16, 64]), ALU.mult, [Sk, "get"], [Sk])
                self.cp(t["Sb"][:], S[:], [Sk], [f"Sb{d}"], eng="act")

            def stage_y(d, R1, R1k, ytime):
                t = dirs[d]
                if ytime < 0:
                    return
                Py, Pyk = t["Py"], f"Py{d}"
                for h in range(2):
                    self.mm(Py[:, 512 * h:512 * h + 512], sel[64:96, :], R1[64:96, 8 * h:8 * h + 8, :], True, True, ["sel", R1k + "s"], [Pyk])
                yb, ybk = t["yb"].next()
                self.cp(yb[:, 0, :], Py[0:2, :], [Pyk], [ybk], eng="act")
                self.dma(YD[d, :, ytime, :], yb[:, 0, :], [ybk], [])

            fw_blocks = list(range(0, TB, TBs))
            bw_blocks = list(range(TC - TBs, -1, -TBs)) + list(range(TB - TBs, TC - 1, -TBs))
            nblk = len(fw_blocks)
            if max_steps is not None:
                nblk = max_steps // TBs
            def dir_gen(d):
                blist = fw_blocks if d == 0 else bw_blocks
                for bi in range(nblk):
                    blk = load_block(d, blist[bi])
                    for j in range(TBs):
                        tc = j if d == 0 else TBs - 1 - j
                        R1, R1k = stage_vx(d, blk, tc)
                        R1, R1k = stage_m1(d, blk["L1"][:, tc, :], [blk["L1k"]], R1, R1k)
                        yield
                        stage_upd(d, blk, tc, R1, R1k, j == TBs - 1)
                        yield
                        tt_ = blk["tb0"] + tc
                        stage_y(d, R1, R1k, tt_ - 1 if d == 0 else tt_)
                        yield
            g0, g1 = dir_gen(0), dir_gen(1)
            next(g0)
            round_robin([g1, g0], 2)
            t = dirs[0]
            for m in range(2):
                rows = slice(64 * m, 64 * m + 64)
                self.cp(t["L1x"][rows, 64 + 16 * m:80 + 16 * m], rlt[rows, :, fw_blocks[nblk - 1] // 16], ["rlt"], ["L1x0"], eng="pool")
            last_t = fw_blocks[nblk - 1] + TBs - 1
            R1, R1k = stage_m1(0, t["L1x"][:], ["L1x0"])
            stage_y(0, R1, R1k, last_t)
            if "SFIN" in self.dbg:
                SF = self.dram("SFIN", [2, 128, 1024])
                for d in range(2):
                    t = dirs[d]
                    self.dma(SF[d], t["S"][0][:].rearrange("p g v -> p (g v)"), [f"S{d}0"], [])
            P.end_phase()

    def phase_readout(self, l):
        nc, P = self.nc, self.P
        TC, TL, TB = self.TC, self.TL, self.TB
        lnx_g = self.inp("lnx_g", [DEPTH, D]); lnx_b = self.inp("lnx_b", [DEPTH, D])
        YD = self.dram("YD", [2, 2, TB, D])
        BON = self.dram("BON", [8, 128, BL, TB]); GG = self.dram("GG", [8, 128, BL, TB])
        YA = self.dram("YA", [8, 128, BL, TB])
        with ExitStack() as ph, nc.allow_non_contiguous_dma(reason="small param transposes"):
            lg = self.sb(ph, "lg", [128, 8]); lb = self.sb(ph, "lb", [128, 8])
            self.dma(lg[:], lnx_g[l].rearrange("(c p) -> p c", p=128), [], ["lg"])
            self.dma(lb[:], lnx_b[l].rearrange("(c p) -> p c", p=128), [], ["lb"])
            yt = self.rot(ph, "yt", [128, 2, 2, 64], 6)
            ys = self.rot(ph, "ysum", [128, 128], 4)
            pT = self.rot(ph, "pT", [128, 512], 3, psum=True)
            pS = self.rot(ph, "pS", [128, 512], 5, psum=True)
            pools = {}

            def T(name):
                if name not in pools:
                    pools[name] = self.rot(ph, "f_" + name, [128, 512], 3)
                return pools[name].next()
            def ro_gen(b, t0, L, p):
                nt = L // 128
                if True:
                    g = b * 8 + p
                    pt, pk = pT.next()
                    for i in range(nt):
                        y2, y2k = yt.next()
                        for d in range(2):
                            self.dma(y2[:, d], YD[d, :, t0 + i * 128:t0 + (i + 1) * 128, g * 64:(g + 1) * 64].rearrange("m t v -> t m v"), [], [y2k])
                        ysm, ysk = ys.next()
                        self.tt(ysm[:].rearrange("t (m v) -> t m v", v=64), y2[:, 0], y2[:, 1], ALU.add, [y2k], [ysk])
                        self.tr(pt[:, i * 128:(i + 1) * 128], ysm[:], [ysk], [pk])
                    yield
                    bn, bnk = T("bn"); gg, ggk = T("gg")
                    self.dma(bn[:, 0:L], BON[p, :, b, t0:t0 + L], [], [bnk])
                    self.dma(gg[:, 0:L], GG[p, :, b, t0:t0 + L], [], [ggk])
                    y, yk = T("y")
                    self.cp(y[:, 0:L], pt[:, 0:L], [pk], [yk], eng="act")
                    yield
                    pm, pmk = pS.next()
                    self.mm(pm[:, 0:L], self.bo[:], y[:, 0:L], True, True, ["bo", yk], [pmk])
                    yield
                    cen, cenk = T("cen")
                    self.stt(cen[:, 0:L], pm[:, 0:L], -1.0 / 64, y[:, 0:L], ALU.mult, ALU.add, [pmk, yk], [cenk])
                    yield
                    sq, sqk = T("sq")
                    self.act(sq[:, 0:L], cen[:, 0:L], AF.Square, [cenk], [sqk])
                    yield
                    pv, pvk = pS.next()
                    self.mm(pv[:, 0:L], self.bo[:], sq[:, 0:L], True, True, ["bo", sqk], [pvk])
                    yield
                    rs, rsk = T("rs")
                    self.ts(rs[:, 0:L], pv[:, 0:L], 1.0 / 64, GN_EPS, ALU.mult, ALU.add, [pvk], [rsk])
                    yield
                    self.act(rs[:, 0:L], rs[:, 0:L], AF.Sqrt, [rsk], [rsk])
                    yield
                    P.op("dve", lambda e, o=rs[:, 0:L]: e.reciprocal(out=o, in_=o), reads=[rsk], writes=[rsk])
                    self.tt(cen[:, 0:L], cen[:, 0:L], rs[:, 0:L], ALU.mult, [cenk, rsk], [cenk])
                    self.ts(cen[:, 0:L], cen[:, 0:L], lg[:, p:p + 1], lb[:, p:p + 1], ALU.mult, ALU.add, [cenk, "lg", "lb"], [cenk])
                    self.tt(cen[:, 0:L], cen[:, 0:L], bn[:, 0:L], ALU.add, [cenk, bnk], [cenk])
                    self.tt(cen[:, 0:L], cen[:, 0:L], gg[:, 0:L], ALU.mult, [cenk, ggk], [cenk])
                    self.dma(YA[p, :, b, t0:t0 + L], cen[:, 0:L], [cenk], [])
            round_robin([ro_gen(b, t0, L, p) for (b, kind, t0, L) in self.blocks for p in range(8)], 3)
            P.end_phase()

    def phase_sgu(self, l):
        nc, P = self.nc, self.P
        TC, TL, TB = self.TC, self.TL, self.TB
        sg_g = self.inp("sgu_ln_g", [DEPTH, D]); sg_b = self.inp("sgu_ln_b", [DEPTH, D])
        sg_w = self.inp("sgu_w", [DEPTH, 8, 128, 128]); sg_bias = self.inp("sgu_b", [DEPTH, 8, 128])
        VS = self.dram("VS", [BL, TB, D]); UT = self.dram("UT", [8, 128, BL, TB])
        YS = self.dram("YS", [8, 128, BL, TB])
        with ExitStack() as ph:
            grow = self.sb(ph, "grow", [128, D]); brow = self.sb(ph, "brow", [128, D])
            sgb = self.sb(ph, "sgb", [128, 8, 128])
            wsT = self.sb(ph, "wsT", [128, 8, 128], BF16)
            wld = self.rot(ph, "wld", [128, 128], 2)
            self.dma(grow[:], sg_g[l].partition_broadcast(128), [], ["grow"])
            self.dma(brow[:], sg_b[l].partition_broadcast(128), [], ["brow"])
            self.dma(sgb[:].rearrange("p g q -> p (g q)"), sg_bias[l].rearrange("g q -> (g q)").partition_broadcast(128), [], ["sgb"])
            pw = self.rot(ph, "pw", [128, 128], 2, psum=True)
            for g in range(8):
                w, wk = wld.next()
                self.dma(w[:], sg_w[l, g], [], [wk])
                pt, pk = pw.next()
                self.tr(pt[:], w[:], [wk], [pk])
                self.cp(wsT[:, g, :], pt[:], [pk], ["wsT"])
            vt = self.rot(ph, "vt", [128, D], 2)
            vn = self.rot(ph, "vn", [128, D], 2)
            vr = self.rot(ph, "vr", [128, D], 2, dt=BF16)
            st = self.rot(ph, "st", [128, 16], 2)
            ut = self.rot(ph, "ut", [128, 8, 128], 2)
            yo = self.rot(ph, "yo", [128, 8, 128], 2)
            pg = self.rot(ph, "pg", [128, 8, 128], 2, psum=True)
            for b in range(BL):
                for t0 in range(0, TB, 128):
                    v, vk = vt.next(); n, nk = vn.next(); s_, sk = st.next()
                    self.dma(v[:], VS[b, t0:t0 + 128, :], [], [vk])
                    u, uk = ut.next()
                    self.dma(u[:], UT[:, :, b, t0:t0 + 128].rearrange("g c p -> c g p"), [], [uk])
                    self.ln_tile(v, vk, n[:], nk, s_, sk)
                    self.tt(n[:], n[:], grow[:], ALU.mult, [nk, "grow"], [nk])
                    nr, nrk = vr.next()
                    self.tt(nr[:], n[:], brow[:], ALU.add, [nk, "brow"], [nrk])
                    pp, ppk = pg.next()
                    for g in range(8):
                        self.mm(pp[:, g, :], nr[:, g * 128:(g + 1) * 128], wsT[:, g, :], True, True, [nrk, "wsT"], [ppk])
                    o, ok = yo.next()
                    self.tt(o[:], pp[:], sgb[:], ALU.add, [ppk, "sgb"], [ok])
                    self.tt(o[:], o[:], u[:], ALU.mult, [ok, uk], [ok])
                    self.dma(YS[:, :, b, t0:t0 + 128].rearrange("g c p -> c g p"), o[:], [ok], [])
            P.end_phase()

    def phase_merge(self, l):
        nc, P = self.nc, self.P
        TC, TL, TB = self.TC, self.TL, self.TB
        wa = self.inp("w_branch_a", [DEPTH, D, D]); wb = self.inp("w_branch_b", [DEPTH, D, D]); wo = self.inp("w_out", [DEPTH, D, D])
        l1g = self.inp("ln1_g", [DEPTH, D]); l1b = self.inp("ln1_b", [DEPTH, D])
        modrow = self.dram(f"modrow{l}", [3, 6 * D])
        xres = self.dram("xres", [BL, TB, D])
        YA = self.dram("YA", [8, 128, BL, TB]); YS = self.dram("YS", [8, 128, BL, TB]); GT = self.dram("GT", [16, 128, BL, TB])
        with ExitStack() as ph:
            WO = self.sb(ph, "WO", [128, 8, D], BF16)
            for dc in range(8):
                self.dma(WO[:, dc, :], wo[l, dc * 128:(dc + 1) * 128, :], [], ["WO"], q="pool")
            wab = self.rot(ph, "wab", [128, 2, 8, 128], 3, dt=BF16)
            grow = self.sb(ph, "grow", [128, D]); brow = self.sb(ph, "brow", [128, D])
            self.dma(grow[:], l1g[l].partition_broadcast(128), [], ["grow"])
            self.dma(brow[:], l1b[l].partition_broadcast(128), [], ["brow"])
            gtr = self.sb(ph, "gtr", [128, 3, D])
            for j in range(3):
                self.dma(gtr[:, j, :], modrow[j, 2 * D:3 * D].partition_broadcast(128), [], ["gtr"])
            yaT = [self.sb(ph, f"yaT{ic}", [128, 512], BF16) for ic in range(8)]
            ysT = [self.sb(ph, f"ysT{ic}", [128, 512], BF16) for ic in range(8)]
            gin = self.rot(ph, "gin", [128, 512], 4)
            mT = self.sb(ph, "mT", [128, 8, 512], BF16)
            tmp = self.rot(ph, "mtmp", [128, 512], 2)
            pA = self.rot(ph, "pA", [128, 512], 2, psum=True)
            pB = self.rot(ph, "pB", [128, 512], 2, psum=True)
            pO = self.rot(ph, "pO", [128, D], 2, psum=True)
            xt = self.rot(ph, "xt", [128, D], 2); xo = self.rot(ph, "xo", [128, D], 2)
            st = self.rot(ph, "st", [128, 16], 2)
            for (b, kind, t0, L) in self.blocks:
                j = 0 if kind == 0 else 1 + b
                nt = L // 128
                ins_a = []; ins_s = []
                for ic in range(8):
                    a_, ak = yaT[ic], f"yaT{ic}"; s__, sk_ = ysT[ic], f"ysT{ic}"
                    self.dma(a_[:, 0:L], YA[ic, :, b, t0:t0 + L], [], [ak], q="pool")
                    self.dma(s__[:, 0:L], YS[ic, :, b, t0:t0 + L], [], [sk_], q="pool")
                    ins_a.append((a_, ak)); ins_s.append((s__, sk_))
                for oc in range(8):
                    ga, gak = gin.next(); gs, gsk = gin.next()
                    self.dma(ga[:, 0:L], GT[oc, :, b, t0:t0 + L], [], [gak])
                    self.dma(gs[:, 0:L], GT[8 + oc, :, b, t0:t0 + L], [], [gsk])
                    pa, pak = pA.next(); pb, pbk = pB.next()
                    W2_, w2k = wab.next()
                    self.dma(W2_[:, 0], wa[l, :, oc * 128:(oc + 1) * 128].rearrange("(c p) n -> p c n", p=128), [], [w2k], q="pool")
                    self.dma(W2_[:, 1], wb[l, :, oc * 128:(oc + 1) * 128].rearrange("(c p) n -> p c n", p=128), [], [w2k], q="pool")
                    for ic in range(8):
                        self.mm(pa[:, 0:L], W2_[:, 0, ic, :], ins_a[ic][0][:, 0:L],
                                ic == 0, ic == 7, [w2k, ins_a[ic][1]], [pak])
                    for ic in range(8):
                        self.mm(pb[:, 0:L], W2_[:, 1, ic, :], ins_s[ic][0][:, 0:L],
                                ic == 0, ic == 7, [w2k, ins_s[ic][1]], [pbk])
                    tm, tmk = tmp.next()
                    self.tt(tm[:, 0:L], pa[:, 0:L], ga[:, 0:L], ALU.mult, [pak, gak], [tmk])
                    self.tt(gs[:, 0:L], pb[:, 0:L], gs[:, 0:L], ALU.mult, [pbk, gsk], [gsk])
                    self.tt(mT[:, oc, 0:L], tm[:, 0:L], gs[:, 0:L], ALU.add, [tmk, gsk], [f"mT{oc}"])
                mk = [f"mT{oc}" for oc in range(8)]
                for i in range(nt):
                    po, pok = pO.next()
                    for h in range(2):
                        for ic in range(8):
                            self.mm(po[:, 512 * h:512 * h + 512], mT[:, ic, i * 128:(i + 1) * 128],
                                    WO[:, ic, 512 * h:512 * h + 512], ic == 0, ic == 7, mk + ["WO"], [pok])
                    x, xk = xt.next(); o, ok = xo.next(); s_, sk = st.next()
                    rows = slice(t0 + i * 128, t0 + (i + 1) * 128)
                    self.dma(x[:], xres[b, rows, :], [], [xk])
                    self.tt(o[:], po[:], gtr[:, j, :], ALU.mult, [pok, "gtr"], [ok])
                    self.stt(o[:], x[:], ALPHA, o[:], ALU.mult, ALU.add, [xk, ok], [ok])
                    self.ln_tile(o, ok, x[:], xk, s_, sk)
                    self.tt(x[:], x[:], grow[:], ALU.mult, [xk, "grow"], [xk])
                    self.tt(x[:], x[:], brow[:], ALU.add, [xk, "brow"], [xk])
                    self.dma(xres[b, rows, :], x[:], [xk], [])
            P.end_phase()

    def phase_moe_a(self, l):
        nc, P = self.nc, self.P
        TC, TL, TB = self.TC, self.TL, self.TB
        NTB = TB // 128
        rw_in = self.inp("router_w", [DEPTH, D, NEXP])
        modrow = self.dram(f"modrow{l}", [3, 6 * D])
        xres = self.dram("xres", [BL, TB, D])
        HMD = self.dram("HMD", [BL * TB, D + 16])
        MOED = self.dram("MOED", [BL * TB, D])
        IDXD = self.dram("IDXD", [128, NEXP * 5], I32)
        capL, capC = 2 * TL // NEXP, 2 * TC // NEXP
        assert capL == 256 and capC == 32
        with ExitStack() as ph, nc.allow_non_contiguous_dma(reason="small param transposes"):
            shr = self.sb(ph, "shr", [128, 3, D]); scr = self.sb(ph, "scr", [128, 3, D])
            for j in range(3):
                self.dma(shr[:, j, :], modrow[j, 3 * D:4 * D].partition_broadcast(128), [], ["shr"])
                self.dma(scr[:, j, :], modrow[j, 4 * D:5 * D].partition_broadcast(128), [], ["scr"])
            self.ts(scr[:], scr[:], 1.0, None, ALU.add, None, ["scr"], ["scr"])
            rw = self.sb(ph, "rw", [128, 8, NEXP])
            self.dma(rw[:], rw_in[l].rearrange("(c p) e -> p c e", p=128), [], ["rw"])
            zt = self.sb(ph, "zt", [128, D])
            self.memset(zt[:], 0.0, ["zt"])
            for i in range(BL * NTB):
                self.dma(MOED[i * 128:(i + 1) * 128, :], zt[:], ["zt"], [])
            xt = self.rot(ph, "xt", [128, D], 2); st = self.rot(ph, "st", [128, 16], 2)
            hm = self.rot(ph, "hm", [128, D + 16], 2)
            hmT = self.rot(ph, "hmT", [128, 8, 128], 2)
            sm = self.rot(ph, "sm", [128, 4], 2)
            ex = self.rot(ph, "ex", [128, NEXP], 2)
            pT = self.rot(ph, "pT", [128, 8, 128], 1, psum=True)
            pr = self.rot(ph, "pr", [128, 512], 2, psum=True)
            pa = self.rot(ph, "pa", [128, 512], 1, psum=True)
            aftL = [self.sb(ph, f"aftL{b}", [NEXP, TL]) for b in range(BL)]
            aftC = [self.sb(ph, f"aftC{b}", [NEXP, TC]) for b in range(BL)]
            def tile_gen(b, ti):
                if True:
                    t0 = ti * 128
                    j = 0 if t0 < TC else 1 + b
                    x, xk = xt.next(); s_, sk = st.next(); h, hk = hm.next()
                    self.dma(x[:], xres[b, t0:t0 + 128, :], [], [xk])
                    self.ln_tile(x, xk, h[:, 0:D], hk, s_, sk)
                    yield
                    self.tt(h[:, 0:D], h[:, 0:D], scr[:, j, :], ALU.mult, [hk, "scr"], [hk])
                    self.tt(h[:, 0:D], h[:, 0:D], shr[:, j, :], ALU.add, [hk, "shr"], [hk])
                    yield
                    pt, pk = pT.next()
                    for dc in range(8):
                        self.tr(pt[:, dc, :], h[:, dc * 128:(dc + 1) * 128], [hk], [pk])
                    yield
                    hT, hTk = hmT.next()
                    self.cp(hT[:], pt[:], [pk], [hTk], eng="act")
                    yield
                    pq, pqk = pr.next()
                    for dc in range(8):
                        self.mm(pq[:, 0:NEXP], hT[:, dc, :], rw[:, dc, :], dc == 0, dc == 7, [hTk, "rw"], [pqk])
                    yield
                    m_, mk_ = sm.next(); e_, ek_ = ex.next()
                    P.op("dve", lambda e, o=m_[:, 0:1], i_=pq[:, 0:NEXP]: e.tensor_reduce(out=o, in_=i_, axis=AX.X, op=ALU.max), reads=[pqk], writes=[mk_])
                    self.ts(m_[:, 1:2], m_[:, 0:1], -1.0, None, ALU.mult, None, [mk_], [mk_])
                    yield
                    P.op("act", lambda e, o=e_[:], i_=pq[:, 0:NEXP], bb=m_[:, 1:2], ac=m_[:, 2:3]: e.activation(out=o, in_=i_, func=AF.Exp, bias=bb, scale=1.0, accum_out=ac),
                         reads=[pqk, mk_], writes=[ek_, mk_])
                    P.op("dve", lambda e, o=m_[:, 3:4], i_=m_[:, 2:3]: e.reciprocal(out=o, in_=i_), reads=[mk_], writes=[mk_])
                    self.ts(h[:, D:D + NEXP], e_[:], m_[:, 3:4], None, ALU.mult, None, [ek_, mk_], [hk])
                    yield
                    self.dma(HMD[b * TB + t0:b * TB + t0 + 128, :], h[:], [hk], [])
                    pp, ppk = pa.next()
                    self.tr(pp[0:NEXP, 0:128], h[:, D:D + NEXP], [hk], [ppk])
                    if t0 < TC:
                        self.cp(aftC[b][:, t0:t0 + 128], pp[0:NEXP, 0:128], [ppk], [f"aftC{b}"], eng="act")
                    else:
                        self.cp(aftL[b][:, t0 - TC:t0 - TC + 128], pp[0:NEXP, 0:128], [ppk], [f"aftL{b}"], eng="act")
            round_robin([tile_gen(b, ti) for b in range(BL) for ti in range(NTB)], 1)
            postm = self.sb(ph, "postm", [128, BL * NTB, NEXP])
            work = self.sb(ph, "work", [NEXP, TL]); cum = self.sb(ph, "cum", [NEXP, TL]); zer = self.sb(ph, "zer", [NEXP, TL])
            m8 = self.sb(ph, "m8", [NEXP, 8])
            self.memset(zer[:], 0.0, ["zer"])
            pp2 = pa
            for b in range(BL):
                for (aft, ak, T_, cap, tbase) in ((aftC[b], f"aftC{b}", TC, capC, 0), (aftL[b], f"aftL{b}", TL, capL, TC)):
                    self.cp(work[:, 0:T_], aft[:, 0:T_], [ak], ["work"])
                    for r_ in range(cap // 8):
                        P.op("dve", lambda e, T_=T_: e.max(out=m8[:], in_=work[:, 0:T_]), reads=["work"], writes=["m8"])
                        if r_ < cap // 8 - 1:
                            P.op("dve", lambda e, T_=T_: e.match_replace(out=work[:, 0:T_], in_to_replace=m8[:], in_values=work[:, 0:T_], imm_value=-1e30),
                                 reads=["work", "m8"], writes=["work"])
                    self.ts(work[:, 0:T_], aft[:, 0:T_], m8[:, 7:8], None, ALU.is_ge, None, [ak, "m8"], ["work"])
                    P.op("dve", lambda e, T_=T_: e.tensor_tensor_scan(out=cum[:, 0:T_], data0=work[:, 0:T_], data1=zer[:, 0:T_], initial=0.0,
                                                                      op0=ALU.add, op1=ALU.add), reads=["work", "zer"], writes=["cum"])
                    self.tt(cum[:, 0:T_], cum[:, 0:T_], work[:, 0:T_], ALU.mult, ["cum", "work"], ["cum"])
                    self.ts(cum[:, 0:T_], cum[:, 0:T_], -1.0, None, ALU.add, None, ["cum"], ["cum"])
                    for i in range(T_ // 128):
                        gt = b * NTB + (tbase + i * 128) // 128
                        pq, pqk = pp2.next()
                        self.tr(pq[:, 256:256 + NEXP], cum[:, i * 128:(i + 1) * 128], ["cum"], [pqk])
                        self.cp(postm[:, gt, :], pq[:, 256:256 + NEXP], [pqk], ["postm"], eng="act")
            tgi = self.sb(ph, "tgi", [128, BL * NTB], I32); tg = self.sb(ph, "tg", [128, BL * NTB])
            iri = self.sb(ph, "iri", [128, 256], I32); ir = self.sb(ph, "ir", [128, 256])
            P.op("pool", lambda e: e.iota(tgi[:], pattern=[[128, BL * NTB]], base=0, channel_multiplier=1), reads=(), writes=["tgi"])
            P.op("pool", lambda e: e.iota(iri[:], pattern=[[1, 256]], base=0, channel_multiplier=0), reads=(), writes=["iri"])
            self.cp(tg[:], tgi[:], ["tgi"], ["tg"]); self.cp(ir[:], iri[:], ["iri"], ["ir"])
            idx = self.sb(ph, "idx", [128, NEXP, 5], I32)
            self.memset(idx[:], 0, ["idx"])
            Pm = self.rot(ph, "Pm", [128, 256], 3)
            Pmc = [self.sb(ph, f"Pmc{b}", [128, 2, 64]) for b in range(BL)]
            for b in range(BL):
                self.memset(Pmc[b][:], 0.0, [f"Pmc{b}"])
            pix = self.rot(ph, "pix", [128, 512], 3, psum=True)
            ntl = TL // 128
            for e_i in range(NEXP):
                for b in range(BL):
                    p0, p0k = pix.next(); p1, p1k = pix.next()
                    for i in range(ntl):
                        gt = b * NTB + TC // 128 + i
                        pm, pmk = Pm.next()
                        self.ts(pm[:], ir[:], postm[:, gt, e_i:e_i + 1], None, ALU.is_equal, None, ["ir", "postm"], [pmk])
                        self.mm(p0[:, 0:1], pm[:, 0:128], tg[:, gt:gt + 1], i == 0, i == ntl - 1, [pmk, "tg"], [p0k])
                        self.mm(p1[:, 0:1], pm[:, 128:256], tg[:, gt:gt + 1], i == 0, i == ntl - 1, [pmk, "tg"], [p1k])
                    self.cp(idx[:, e_i, 2 * b:2 * b + 1], p0[:, 0:1], [p0k], ["idx"])
                    self.cp(idx[:, e_i, 2 * b + 1:2 * b + 2], p1[:, 0:1], [p1k], ["idx"])
                pc, pck = pix.next()
                n_ = 0
                for b in range(BL):
                    for i in range(TC // 128):
                        gt = b * NTB + i
                        pmc = Pmc[b]; pmck = f"Pmc{b}"
                        self.ts(pmc[:, i, 32 * b:32 * b + 32], ir[:, 0:32], postm[:, gt, e_i:e_i + 1], None, ALU.is_equal, None, ["ir", "postm"], [pmck])
                        self.mm(pc[0:64, 0:1], pmc[:, i, :], tg[:, gt:gt + 1], n_ == 0, n_ == BL * (TC // 128) - 1, [pmck, "tg"], [pck])
                        n_ += 1
                self.cp(idx[0:64, e_i, 4:5], pc[0:64, 0:1], [pck], ["idx"])
            self.dma(IDXD[:, :], idx[:].rearrange("p e k -> p (e k)"), ["idx"], [])
            P.end_phase()

    def phase_moe_b(self, l):
        nc, P = self.nc, self.P
        TC, TL, TB = self.TC, self.TL, self.TB
        w1 = self.inp("exp_w1", [DEPTH, NEXP, D, DFF]); w3 = self.inp("exp_w3", [DEPTH, NEXP, D, DFF]); w2 = self.inp("exp_w2", [DEPTH, NEXP, DFF, D])
        HMD = self.dram("HMD", [BL * TB, D + 16]); MOED = self.dram("MOED", [BL * TB, D]); IDXD = self.dram("IDXD", [128, NEXP * 5], I32)
        NS = 640
        blks = [(0, 128), (128, 128), (256, 128), (384, 128), (512, 64)]
        with ExitStack() as ph:
            idx = self.sb(ph, "idx", [128, NEXP, 5], I32)
            self.dma(idx[:].rearrange("p e k -> p (e k)"), IDXD[:, :], [], ["idx"])
            xe = self.rot(ph, "xe", [128, D + 16], 3)
            gates = self.rot(ph, "gates", [128, 8], 2)
            xeTr = self.rot(ph, "xeT", [128, 8, NS], 2, dt=BF16); hidTr = self.rot(ph, "hidT", [128, 16, NS], 2, dt=BF16)
            for tl_ in xeTr.tiles:
                self.memset(tl_[:], 0.0, [])
            stg = self.rot(ph, "wstg", [128, 8, 512], 3)
            wr = self.rot(ph, "wr", [128, 8, 512], 4, dt=BF16)
            sl = self.rot(ph, "sl", [128, 512], 2)
            pg = self.rot(ph, "pg", [128, 8, 128], 1, psum=True)
            pmm = self.rot(ph, "pmm", [128, 512], 6, psum=True)

            def load_w(src_ap):
                s_, sk_ = stg.next(); w_, wk_ = wr.next()
                self.dma(s_[:], src_ap, [], [sk_])
                self.cp(w_[:], s_[:], [sk_], [wk_], eng="pool")
                return w_, wk_
            def load_w2(src_ap):
                s_, sk_ = stg.next(); w_, wk_ = wr.next()
                self.dma(s_[:, 0:4, :], src_ap, [], [sk_])
                self.cp(w_[:, 0:4, :], s_[:, 0:4, :], [sk_], [wk_], eng="pool")
                return w_, wk_
            for e_i in range(NEXP):
                gt_, gk_ = gates.next()
                xeT, xeTk = xeTr.next(); hidT, hidTk = hidTr.next()
                for bi, (c0, n) in enumerate(blks):
                    x_, xk_ = xe.next()
                    P.dma("pool", lambda e, x_=x_, n=n, bi=bi, e_i=e_i: e.indirect_dma_start(
                        out=x_[0:n, :], out_offset=None, in_=HMD[:, :],
                        in_offset=bass.IndirectOffsetOnAxis(ap=idx[0:n, e_i, bi:bi + 1], axis=0)), reads=["idx"], writes=[xk_])
                    self.cp(gt_[0:n, bi:bi + 1], x_[0:n, D + e_i:D + e_i + 1], [xk_], [gk_], eng="act")
                    pt, pk = pg.next()
                    for dc in range(8):
                        self.tr(pt[:, dc, 0:n], x_[0:n, dc * 128:(dc + 1) * 128], [xk_], [pk])
                    self.cp(xeT[:, :, c0:c0 + n], pt[:, :, 0:n], [pk], [xeTk])
                for fb in range(4):
                    W1, W1k = load_w(w1[l, e_i, :, fb * 512:(fb + 1) * 512].rearrange("(c p) n -> p c n", p=128))
                    W3, W3k = load_w(w3[l, e_i, :, fb * 512:(fb + 1) * 512].rearrange("(c p) n -> p c n", p=128))
                    for fc in range(4):
                        fcc = fb * 4 + fc
                        for (n0, nn) in ((0, 512), (512, 128)):
                            p1, p1k = pmm.next(); p3, p3k = pmm.next()
                            for dc in range(8):
                                self.mm(p1[:, 0:nn], W1[:, dc, fc * 128:(fc + 1) * 128], xeT[:, dc, n0:n0 + nn],
                                        dc == 0, dc == 7, [W1k, xeTk], [p1k])
                            for dc in range(8):
                                self.mm(p3[:, 0:nn], W3[:, dc, fc * 128:(fc + 1) * 128], xeT[:, dc, n0:n0 + nn],
                                        dc == 0, dc == 7, [W3k, xeTk], [p3k])
                            s_, sk_ = sl.next()
                            self.act(s_[:, 0:nn], p1[:, 0:nn], AF.Silu, [p1k], [sk_])
                            self.tt(hidT[:, fcc, n0:n0 + nn], s_[:, 0:nn], p3[:, 0:nn], ALU.mult, [sk_, p3k], [hidTk])
                yts = {}
                for half in range(2):
                    accs = [pmm.next() for _ in blks]
                    for pc in range(4):
                        W2, W2k = load_w2(w2[l, e_i, pc * 512:(pc + 1) * 512, half * 512:(half + 1) * 512].rearrange("(c p) n -> p c n", p=128))
                        for bi, (c0, n) in enumerate(blks):
                            po, pok = accs[bi]
                            for fc in range(4):
                                fcc = pc * 4 + fc
                                self.mm(po[:, :], hidT[:, fcc, c0:c0 + 128], W2[:, fc, :],
                                        fcc == 0, fcc == 15, [hidTk, W2k], [pok])
                    for bi, (c0, n) in enumerate(blks):
                        po, pok = accs[bi]
                        if bi not in yts:
                            yts[bi] = self.yes_tile(ph, bi)
                        yt_, ytk_ = yts[bi]
                        self.ts(yt_[0:n, half * 512:(half + 1) * 512], po[0:n, :], gt_[0:n, bi:bi + 1], None, ALU.mult, None, [pok, gk_], [ytk_])
                for bi, (c0, n) in enumerate(blks):
                    yt_, ytk_ = yts[bi]
                    P.dma("pool", lambda e, yt_=yt_, n=n, bi=bi, e_i=e_i: e.indirect_dma_start(
                        out=MOED[:, :], out_offset=bass.IndirectOffsetOnAxis(ap=idx[0:n, e_i, bi:bi + 1], axis=0),
                        in_=yt_[0:n, :], in_offset=None, compute_op=ALU.add), reads=["idx", ytk_], writes=["MOED"])
            P.end_phase()

    def yes_tile(self, ph, bi):
        if not hasattr(self, "_yes") or self._yes_ph is not ph:
            self._yes = [self.rot(ph, f"yesb{k}", [128, D], 1) for k in range(5)]
            self._yes_ph = ph
        return self._yes[bi].next()

    def phase_ln2(self, l, last):
        nc, P = self.nc, self.P
        TC, TL, TB = self.TC, self.TL, self.TB
        l2g = self.inp("ln2_g", [DEPTH, D]); l2b = self.inp("ln2_b", [DEPTH, D])
        modrow = self.dram(f"modrow{l}", [3, 6 * D])
        xres = self.dram("xres", [BL, TB, D]); MOED = self.dram("MOED", [BL * TB, D])
        if last:
            if "out" not in self.drams:
                self.drams["out"] = nc.dram_tensor("out", [BL, TL, D], F32, kind="ExternalOutput").ap()
            outp = self.drams["out"]
        with ExitStack() as ph:
            grow = self.sb(ph, "grow", [128, D]); brow = self.sb(ph, "brow", [128, D])
            self.dma(grow[:], l2g[l].partition_broadcast(128), [], ["grow"])
            self.dma(brow[:], l2b[l].partition_broadcast(128), [], ["brow"])
            gtr = self.sb(ph, "gtr", [128, 3, D])
            for j in range(3):
                self.dma(gtr[:, j, :], modrow[j, 5 * D:6 * D].partition_broadcast(128), [], ["gtr"])
            xt = self.rot(ph, "xt", [128, D], 3); mt = self.rot(ph, "mt", [128, D], 3); st = self.rot(ph, "st", [128, 16], 2)
            for b in range(BL):
                for t0 in range(0, TB, 128):
                    if last and t0 < TC:
                        continue
                    j = 0 if t0 < TC else 1 + b
                    x, xk = xt.next(); m, mk = mt.next(); s_, sk = st.next()
                    self.dma(x[:], xres[b, t0:t0 + 128, :], [], [xk])
                    self.dma(m[:], MOED[b * TB + t0:b * TB + t0 + 128, :], [], [mk])
                    self.tt(m[:], m[:], gtr[:, j, :], ALU.mult, [mk, "gtr"], [mk])
                    self.stt(m[:], x[:], ALPHA, m[:], ALU.mult, ALU.add, [xk, mk], [mk])
                    self.ln_tile(m, mk, x[:], xk, s_, sk)
                    self.tt(x[:], x[:], grow[:], ALU.mult, [xk, "grow"], [xk])
                    self.tt(x[:], x[:], brow[:], ALU.add, [xk, "brow"], [xk])
                    if last:
                        self.dma(outp[b, t0 - TC:t0 - TC + 128, :], x[:], [xk], [])
                    else:
                        self.dma(xres[b, t0:t0 + 128, :], x[:], [xk], [])
            P.end_phase()

    def build_all(self):
        self.phase_init()
        for l in range(DEPTH):
            self.phase_mod(l)
            self.phase_proj(l)
            self.phase_prep(l)
            self.phase_scan(l)
            self.phase_readout(l)
            self.phase_sgu(l)
            self.phase_merge(l)
            self.phase_moe_a(l)
            self.phase_moe_b(l)
            self.phase_ln2(l, l == DEPTH - 1)
        return self.finish()

    def finish(self):
        self.es.close()
        return self.nc


_CACHE = {}

WEIGHTS = ["ada_w", "ada_b", "w_in", "shift_conv", "w0", "w2", "a0", "a2", "g2", "k_k", "k_a", "r_k", "lnx_g", "lnx_b",
           "sgu_ln_g", "sgu_ln_b", "sgu_w", "sgu_b", "w_branch_a", "w_branch_b", "w_out", "ln1_g", "ln1_b", "router_w",
           "exp_w1", "exp_w3", "exp_w2", "ln2_g", "ln2_b"]


def kernel(**inputs):
    x = np.asarray(inputs["x"], dtype=np.float32)
    B, TL, _ = x.shape
    TC = inputs["ctx"].shape[1]
    if "nc" not in _CACHE:
        kb = KB(TC, TL)
        _CACHE["nc"] = kb.build_all()
        _CACHE["names"] = list(kb.inputs.keys())
    nc = _CACHE["nc"]
    names = _CACHE["names"]
    shared = {k: np.ascontiguousarray(np.asarray(inputs[k], dtype=np.float32)) for k in WEIGHTS if k in names}
    cctx = np.ascontiguousarray(np.asarray(inputs["c_ctx"], dtype=np.float32)[None, :])
    in_maps = []
    for c in range(NCORE):
        m = dict(shared)
        m["x"] = np.ascontiguousarray(x[BL * c:BL * c + BL])
        m["ctx"] = np.ascontiguousarray(np.asarray(inputs["ctx"], dtype=np.float32)[BL * c:BL * c + BL])
        m["c"] = np.ascontiguousarray(np.asarray(inputs["c"], dtype=np.float32)[BL * c:BL * c + BL])
        m["c_ctx"] = cctx
        in_maps.append({k: m[k] for k in names})
    res = run_bass_kernel_spmd(nc, in_maps, core_ids=list(range(NCORE)))
    return np.concatenate([res.results[c]["out"] for c in range(NCORE)], axis=0).astype(np.float32)
```

```python
from contextlib import ExitStack
import numpy as np
import concourse.bass as bass
import concourse.mybir as mybir
from concourse.bass_utils import run_bass_kernel_spmd

F32 = mybir.dt.float32
F32R = mybir.dt.float32r
BF16 = mybir.dt.bfloat16
I32 = mybir.dt.int32
U32 = mybir.dt.uint32
AF = mybir.ActivationFunctionType
ALU = mybir.AluOpType
AX = mybir.AxisListType

D = 1024
DEPTH = 2
NCORE = 8
BL = 2
N_RW = 3456
N_IN = 7552
H_A = 16
NEXP = 16
DFF = 2048
ALPHA = (2 * DEPTH) ** 0.25
LN_EPS = 1e-5
GN_EPS = 64e-5

ENGS = ("pe", "act", "dve", "pool", "sp")
EPOCH = 60000
DMA_RING = 8


class Prog:
    def __init__(self, nc, es, nsem=90):
        self.nc = nc
        self.free_sems = [es.enter_context(nc.semaphore(f"s{i}")) for i in range(nsem)]
        self.ops = {e: [] for e in ENGS}
        self.psem = {e: self.free_sems.pop() for e in ENGS}
        self.pcnt = {e: 0 for e in ENGS}
        self.dsem = {e: [self.free_sems.pop() for _ in range(DMA_RING)] for e in ("sp", "act", "pool")}
        self.dcnt = {e: 0 for e in ("sp", "act", "pool")}
        self.last_write = {}
        self.readers = {}
        self.waited = {e: {} for e in ENGS}
        self.ninst = 0

    def _deps(self, eng, reads, writes):
        toks = []
        for k in reads:
            t = self.last_write.get(k)
            if t is not None:
                toks.append(t)
        for k in writes:
            t = self.last_write.get(k)
            if t is not None:
                toks.append(t)
            toks.extend(self.readers.get(k, ()))
        if eng == "pe":
            ps = self.psem["pe"]
            toks = [t for t in toks if t[0] is not ps]
        return toks

    def _commit(self, tok, reads, writes):
        for k in reads:
            self.readers.setdefault(k, []).append(tok)
        for k in writes:
            self.last_write[k] = tok
            self.readers[k] = []

    def _waits(self, eng, toks):
        best = {}
        for (s, v) in toks:
            key = id(s)
            if key not in best or best[key][1] < v:
                best[key] = (s, v)
        out = []
        w = self.waited[eng]
        for key, (s, v) in best.items():
            if w.get(key, 0) >= v:
                continue
            w[key] = v
            out.append((s, v))
        return out

    def op(self, eng, fn, reads=(), writes=()):
        if self.pcnt[eng] >= EPOCH:
            self.psem[eng] = self.free_sems.pop()
            self.pcnt[eng] = 0
        toks = self._deps(eng, reads, writes)
        waits = self._waits(eng, toks)
        self.pcnt[eng] += 1
        tok = (self.psem[eng], self.pcnt[eng])
        self.ops[eng].append((fn, waits, (tok[0], 1)))
        self._commit(tok, reads, writes)
        return tok

    def dma(self, q, fn, reads=(), writes=()):
        i = self.dcnt[q]
        self.dcnt[q] += 1
        sem = self.dsem[q][i % DMA_RING]
        toks = self._deps(q, reads, writes)
        if i >= DMA_RING:
            toks.append((sem, 16 * (i // DMA_RING)))
        waits = self._waits(q, toks)
        tok = (sem, 16 * (i // DMA_RING + 1))
        self.ops[q].append((fn, waits, (sem, 16)))
        self._commit(tok, reads, writes)
        return tok

    def end_phase(self):
        toks = []
        for e in ENGS:
            if self.pcnt[e] > 0:
                toks.append((self.psem[e], self.pcnt[e]))
        for q in self.dcnt:
            n = self.dcnt[q]
            for j in range(min(n, DMA_RING)):
                last = ((n - 1 - j) // DMA_RING) * DMA_RING + j
                toks.append((self.dsem[q][j], 16 * (last // DMA_RING + 1)))
        self.ops["sp"].append((None, self._waits("sp", toks), None))
        nc = self.nc
        ops = self.ops
        me = self

        def run(engobj, lst):
            for fn, waits, inc in lst:
                for (s, v) in waits:
                    engobj.wait_ge(s, v)
                if fn is None:
                    continue
                ins = fn(engobj)
                ins.then_inc(inc[0], inc[1])
                me.ninst += 1

        with nc.Block() as block:
            @block.sync
            def _(e):
                run(e, ops["sp"])

            @block.scalar
            def _(e):
                run(e, ops["act"])

            @block.vector
            def _(e):
                run(e, ops["dve"])

            @block.gpsimd
            def _(e):
                run(e, ops["pool"])

            @block.tensor
            def _(e):
                run(e, ops["pe"])
        self.ops = {e: [] for e in ENGS}
        self.last_write = {}
        self.readers = {}
        for q in self.dcnt:
            if self.dcnt[q] // DMA_RING > 2500:
                self.dsem[q] = [self.free_sems.pop() for _ in range(DMA_RING)]
                self.dcnt[q] = 0


def round_robin(gens, width):
    gens = list(gens)
    active = []
    while gens or active:
        while gens and len(active) < width:
            active.append(gens.pop(0))
        for g in list(active):
            try:
                next(g)
            except StopIteration:
                active.remove(g)


class Rot:
    def __init__(self, tiles, name):
        self.tiles = tiles
        self.name = name
        self.i = -1

    def next(self):
        self.i += 1
        j = self.i % len(self.tiles)
        return self.tiles[j], f"{self.name}{j}"


class KB:
    def __init__(self, TC, TL, dbg=()):
        self.TC, self.TL = TC, TL
        self.TB = TC + TL
        self.dbg = set(dbg)
        self.nc = bass.Bass("TRN2", target_bir_lowering=False)
        self.es = ExitStack()
        self.P = Prog(self.nc, self.es)
        self.inputs = {}
        self.drams = {}
        nc = self.nc
        self.ident = self.es.enter_context(nc.sbuf_tensor("ident", [128, 128], F32))
        self.bo = self.es.enter_context(nc.sbuf_tensor("bo", [128, 128], F32))
        self.blocks = []
        for b in range(BL):
            self.blocks.append((b, 0, 0, TC))
            for t0 in range(0, TL, 512):
                self.blocks.append((b, 1, TC + t0, min(512, TL - t0)))

    def inp(self, name, shape):
        if name not in self.inputs:
            self.inputs[name] = self.nc.dram_tensor(name, list(shape), F32, kind="ExternalInput").ap()
        return self.inputs[name]

    def dram(self, name, shape, dt=F32):
        if name not in self.drams:
            kind = "ExternalOutput" if name in self.dbg else "Internal"
            self.drams[name] = self.nc.dram_tensor(name, list(shape), dt, kind=kind).ap()
        return self.drams[name]

    def uname(self, name):
        self.uid = getattr(self, "uid", 0) + 1
        return f"{name}_u{self.uid}"

    def sb(self, ph, name, shape, dt=F32):
        return ph.enter_context(self.nc.sbuf_tensor(self.uname(name), list(shape), dt))

    def rot(self, ph, name, shape, n, dt=F32, psum=False):
        if psum:
            tiles = [ph.enter_context(self.nc.psum_tensor(self.uname(f"{name}{i}"), list(shape), dt)) for i in range(n)]
        else:
            tiles = [ph.enter_context(self.nc.sbuf_tensor(self.uname(f"{name}{i}"), list(shape), dt)) for i in range(n)]
        return Rot(tiles, name)

    def dma(self, out, in_, R, W, q="sp", **kw):
        self.P.dma(q, lambda e: e.dma_start(out=out, in_=in_, **kw), reads=R, writes=W)

    def mm(self, out, lhsT, rhs, start, stop, R, W):
        self.P.op("pe", lambda e: e.matmul(out, lhsT=lhsT, rhs=rhs, start=start, stop=stop), reads=R, writes=W)

    def tr(self, out, in_, R, W, ident=None):
        idn = self.ident[0:in_.shape[0], 0:in_.shape[0]] if ident is None else ident
        self.P.op("pe", lambda e: e.transpose(out, in_, idn), reads=list(R) + ["ident"], writes=W)

    def act(self, out, in_, func, R, W, bias=None, scale=1.0):
        kw = {}
        if bias is not None:
            kw["bias"] = bias
        self.P.op("act", lambda e: e.activation(out=out, in_=in_, func=func, scale=scale, **kw), reads=R, writes=W)

    def ts(self, out, in0, s1, s2, op0, op1, R, W, eng="dve"):
        self.P.op(eng, lambda e: e.tensor_scalar(out=out, in0=in0, scalar1=s1, scalar2=s2, op0=op0, op1=op1) if op1 is not None
                  else e.tensor_scalar(out=out, in0=in0, scalar1=s1, scalar2=None, op0=op0), reads=R, writes=W)

    def tt(self, out, in0, in1, op, R, W, eng="dve"):
        self.P.op(eng, lambda e: e.tensor_tensor(out=out, in0=in0, in1=in1, op=op), reads=R, writes=W)

    def stt(self, out, in0, scalar, in1, op0, op1, R, W):
        self.P.op("dve", lambda e: e.scalar_tensor_tensor(out=out, in0=in0, scalar=scalar, in1=in1, op0=op0, op1=op1), reads=R, writes=W)

    def cp(self, out, in_, R, W, eng="dve"):
        if eng == "act":
            self.P.op("act", lambda e: e.copy(out=out, in_=in_), reads=R, writes=W)
        else:
            self.P.op(eng, lambda e: e.tensor_copy(out=out, in_=in_), reads=R, writes=W)

    def memset(self, ap, val, W, eng="dve"):
        self.P.op(eng, lambda e: e.memset(ap, val), reads=(), writes=W)

    def phase_consts(self):
        nc, P = self.nc, self.P
        with ExitStack() as ph:
            ones = self.sb(ph, "ones_i", [128, 128])
            self.memset(ones[:], 1.0, ["ones_i"])
            self.memset(self.ident[:], 0.0, ["ident"])
            self.memset(self.bo[:], 0.0, ["bo"])
            P.op("pool", lambda e: e.affine_select(out=self.ident[:], in_=ones[:], pattern=[[-1, 128]], compare_op=ALU.is_equal,
                                                   fill=0.0, base=0, channel_multiplier=1),
                 reads=["ones_i", "ident"], writes=["ident"])
            self.memset(self.bo[0:64, 0:64], 1.0, ["bo"])
            self.memset(self.bo[64:128, 64:128], 1.0, ["bo"])
            P.end_phase()

    def phase_init(self):
        nc, P = self.nc, self.P
        TC, TL, TB = self.TC, self.TL, self.TB
        x = self.inp("x", [BL, TL, D])
        ctx = self.inp("ctx", [BL, TC, D])
        xres = self.dram("xres", [BL, TB, D])
        self.phase_consts()
        for b in range(BL):
            self.dma(xres[b, 0:TC, :], ctx[b], [], [])
            self.dma(xres[b, TC:TB, :], x[b], [], [])
        P.end_phase()

    def phase_mod(self, l):
        nc, P = self.nc, self.P
        c = self.inp("c", [BL, D])
        cc = self.inp("c_ctx", [1, D])
        ada_w = self.inp("ada_w", [DEPTH, D, 6 * D])
        ada_b = self.inp("ada_b", [DEPTH, 6 * D])
        modrow = self.dram(f"modrow{l}", [3, 6 * D])
        with ExitStack() as ph, nc.allow_non_contiguous_dma(reason="small param transposes"):
            cT = self.sb(ph, "cT", [128, 8, 3])
            scT = self.sb(ph, "scT", [128, 8, 3])
            abt = self.sb(ph, "abt", [3, 6 * D])
            mrow = self.sb(ph, "mrow", [3, 6 * D])
            wr = self.rot(ph, "adaw", [128, 8, 512], 3)
            pr = self.rot(ph, "pmod", [3, 512], 2, psum=True)
            self.dma(cT[:, :, 0], cc[0].rearrange("(c p) -> p c", p=128), [], ["cT"])
            for b in range(BL):
                self.dma(cT[:, :, 1 + b], c[b].rearrange("(c p) -> p c", p=128), [], ["cT"])
            self.dma(abt[:], ada_b[l].partition_broadcast(3), [], ["abt"])
            self.act(scT[:], cT[:], AF.Silu, ["cT"], ["scT"])
            for nb in range(12):
                w, wk = wr.next()
                self.dma(w[:], ada_w[l, :, nb * 512:(nb + 1) * 512].rearrange("(c p) n -> p c n", p=128), [], [wk])
                pt, pk = pr.next()
                for dc in range(8):
                    self.mm(pt[:], scT[:, dc, :], w[:, dc, :], dc == 0, dc == 7, ["scT", wk], [pk])
                self.tt(mrow[:, nb * 512:(nb + 1) * 512], pt[:], abt[:, nb * 512:(nb + 1) * 512], ALU.add, [pk, "abt"], ["mrow"])
            self.dma(modrow[:, :], mrow[:], ["mrow"], [])
            P.end_phase()

    def ln_tile(self, xt, xk, xn, xnk, st, stk, eps=LN_EPS):
        P = self.P
        for h in range(2):
            P.op("dve", lambda e, h=h: e.bn_stats(out=st[:, 6 * h:6 * h + 6], in_=xt[:, 512 * h:512 * h + 512]), reads=[xk], writes=[stk])
        P.op("dve", lambda e: e.bn_aggr(out=st[:, 12:14], in_=st[:, 0:12]), reads=[stk], writes=[stk])
        self.ts(st[:, 14:15], st[:, 13:14], eps, None, ALU.add, None, [stk], [stk])
        self.act(st[:, 14:15], st[:, 14:15], AF.Sqrt, [stk], [stk])
        P.op("dve", lambda e: e.reciprocal(out=st[:, 14:15], in_=st[:, 14:15]), reads=[stk], writes=[stk])
        self.ts(xn, xt[:], st[:, 12:13], st[:, 14:15], ALU.subtract, ALU.mult, [xk, stk], [xnk])

    def load_modfm(self, ph, l):
        modrow = self.dram(f"modrow{l}", [3, 6 * D])
        modfm = self.sb(ph, "modfm", [128, 3, 6, 8])
        for j in range(3):
            self.dma(modfm[:, j], modrow[j].rearrange("(s c p) -> p s c", p=128, c=8), [], ["modfm"])
        return modfm

    def phase_proj(self, l):
        nc, P = self.nc, self.P
        TC, TL, TB = self.TC, self.TL, self.TB
        w_in = self.inp("w_in", [DEPTH, D, N_IN])
        xres = self.dram("xres", [BL, TB, D])
        ZR = self.dram("ZR", [27, 128, BL, TB + 2])
        UT = self.dram("UT", [8, 128, BL, TB])
        GT = self.dram("GT", [16, 128, BL, TB])
        VS = self.dram("VS", [BL, TB, D])
        cblocks = [(i * 512, 512) for i in range(6)] + [(3072, 384)] + [(3456 + i * 512, 512) for i in range(8)]
        with ExitStack() as ph, nc.allow_non_contiguous_dma(reason="small param transposes"):
            modfm = self.load_modfm(ph, l)
            ops1 = self.sb(ph, "ops1", [128, 3, 8])
            self.ts(ops1[:], modfm[:, :, 1, :], 1.0, None, ALU.add, None, ["modfm"], ["ops1"])
            xr = self.rot(ph, "xt", [128, D], 2)
            xnb = self.sb(ph, "xnb", [128, 4, D])
            st = self.rot(ph, "st", [128, 16], 2)
            hT = self.sb(ph, "hT", [128, 8, BL * TB], BF16)
            wr = self.rot(ph, "wblk", [128, 8, 512], 2, dt=BF16)
            wst = self.rot(ph, "wstg", [128, 8, 512], 3)
            ptr = self.rot(ph, "ptr", [128, 512], 2, psum=True)
            pmm = self.rot(ph, "pmm", [128, 512], 4, psum=True)
            stg = self.rot(ph, "stg", [128, 512], 6)
            for (b, kind, t0, L) in self.blocks:
                j = 0 if kind == 0 else 1 + b
                nt = L // 128
                off = b * TB + t0
                for i in range(nt):
                    xt, xk = xr.next()
                    s_, sk = st.next()
                    self.dma(xt[:], xres[b, t0 + i * 128:t0 + (i + 1) * 128, :], [], [xk])
                    self.ln_tile(xt, xk, xnb[:, i, :], f"xnb{i}", s_, sk)
                for dc in range(8):
                    pt, pk = ptr.next()
                    for i in range(nt):
                        self.tr(pt[:, i * 128:(i + 1) * 128], xnb[:, i, dc * 128:(dc + 1) * 128], [f"xnb{i}"], [pk])
                    self.act(hT[:, dc, off:off + L], pt[:, 0:L], AF.Identity, [pk, "ops1", "modfm"], ["hT"],
                             bias=modfm[:, j, 0, dc:dc + 1], scale=ops1[:, j, dc:dc + 1])
            for (c0, cw) in cblocks:
                w, wk = wr.next()
                ws, wsk = wst.next()
                self.dma(ws[:, :, 0:cw], w_in[l, :, c0:c0 + cw].rearrange("(c p) n -> p c n", p=128), [], [wsk])
                self.cp(w[:, :, 0:cw], ws[:, :, 0:cw], [wsk], [wk], eng="pool")
                for (b, kind, t0, L) in self.blocks:
                    nt = L // 128
                    off = b * TB + t0
                    if 4480 <= c0 < 5504:
                        for i in range(nt):
                            pm, pmk = pmm.next()
                            for dc in range(8):
                                self.mm(pm[:, 0:cw], hT[:, dc, off + i * 128:off + (i + 1) * 128], w[:, dc, 0:cw],
                                        dc == 0, dc == 7, ["hT", wk], [pmk])
                            sg, sgk = stg.next()
                            self.act(sg[:, 0:cw], pm[:, 0:cw], AF.Gelu, [pmk], [sgk])
                            self.dma(VS[b, t0 + i * 128:t0 + (i + 1) * 128, c0 - 4480:c0 - 4480 + cw], sg[:, 0:cw], [sgk], [])
                        continue
                    for cc in range(cw // 128):
                        col = c0 + cc * 128
                        pm, pmk = pmm.next()
                        for dc in range(8):
                            self.mm(pm[:, 0:L], w[:, dc, cc * 128:(cc + 1) * 128], hT[:, dc, off:off + L],
                                    dc == 0, dc == 7, ["hT", wk], [pmk])
                        sg, sgk = stg.next()
                        if col < N_RW:
                            self.cp(sg[:, 0:L], pm[:, 0:L], [pmk], [sgk])
                            self.dma(ZR[col // 128, :, b, 1 + t0:1 + t0 + L], sg[:, 0:L], [sgk], [])
                        elif col < 4480:
                            self.act(sg[:, 0:L], pm[:, 0:L], AF.Gelu, [pmk], [sgk])
                            self.dma(UT[(col - N_RW) // 128, :, b, t0:t0 + L], sg[:, 0:L], [sgk], [])
                        else:
                            self.act(sg[:, 0:L], pm[:, 0:L], AF.Sigmoid, [pmk], [sgk])
                            self.dma(GT[(col - 5504) // 128, :, b, t0:t0 + L], sg[:, 0:L], [sgk], [])
            P.end_phase()

    def phase_prep(self, l):
        nc, P = self.nc, self.P
        TC, TL, TB = self.TC, self.TL, self.TB
        shc = self.inp("shift_conv", [DEPTH, 3, N_RW])
        w0 = self.inp("w0", [DEPTH, 2, D]); w2 = self.inp("w2", [DEPTH, 2, 64, D])
        a0 = self.inp("a0", [DEPTH, 2, D]); a2 = self.inp("a2", [DEPTH, 2, 64, D])
        g2 = self.inp("g2", [DEPTH, 128, D])
        k_k = self.inp("k_k", [DEPTH, D]); k_a = self.inp("k_a", [DEPTH, D]); r_k = self.inp("r_k", [DEPTH, H_A, 64])
        ZR = self.dram("ZR", [27, 128, BL, TB + 2])
        SC = self.dram("SC", [9, 16, 128, TB])
        BON = self.dram("BON", [8, 128, BL, TB])
        GG = self.dram("GG", [8, 128, BL, TB])
        TM = self.dram("TM", [5, BL, TB, D], BF16)
        GE = self.dram("GE", [2, 16, 128, TB // 16])
        RL = self.dram("RL", [16, 128, TB // 16])
        with ExitStack() as ph, nc.allow_non_contiguous_dma(reason="small param transposes"):
            ptm = self.rot(ph, "ptm", [128, 512], 2, psum=True)
            stm = self.rot(ph, "stm", [128, 512], 3, dt=BF16)

            def to_tm(op, src, srck, b, p, t0, L):
                pt, pk = ptm.next()
                nt = L // 128
                for i in range(nt):
                    self.tr(pt[:, i * 128:(i + 1) * 128], src[:, i * 128:(i + 1) * 128], [srck], [pk])
                st_, stk_ = stm.next()
                self.cp(st_[:, 0:L], pt[:, 0:L], [pk], [stk_], eng="act")
                self.dma(TM[op, b, t0:t0 + L, p * 128:(p + 1) * 128].rearrange("(i t) c -> t i c", t=128),
                         st_[:, 0:L].rearrange("t (i c) -> t i c", c=128), [stk_], [])
            scv = self.sb(ph, "scv", [128, 3, 27])
            w0n = self.sb(ph, "w0n", [128, 2, 8]); a0t = self.sb(ph, "a0t", [128, 2, 8])
            kkw = self.sb(ph, "kkw", [128, 8]); kaw = self.sb(ph, "kaw", [128, 8]); rkw = self.sb(ph, "rkw", [128, 8])
            w2t = self.sb(ph, "w2t", [128, D]); a2t = self.sb(ph, "a2t", [128, D]); g2t = self.sb(ph, "g2t", [128, D])
            self.dma(scv[:], shc[l].rearrange("j (c p) -> p j c", p=128), [], ["scv"])
            self.dma(w0n[:], w0[l].rearrange("j (c p) -> p j c", p=128), [], ["w0n"])
            self.ts(w0n[:], w0n[:], -1.0, None, ALU.mult, None, ["w0n"], ["w0n"])
            self.dma(a0t[:], a0[l].rearrange("j (c p) -> p j c", p=128), [], ["a0t"])
            self.dma(kkw[:], k_k[l].rearrange("(c p) -> p c", p=128), [], ["kkw"])
            self.dma(kaw[:], k_a[l].rearrange("(c p) -> p c", p=128), [], ["kaw"])
            self.dma(rkw[:], r_k[l].rearrange("h k -> (h k)").rearrange("(c p) -> p c", p=128), [], ["rkw"])
            for j in range(2):
                self.dma(w2t[64 * j:64 * j + 64, :].bitcast(F32R), w2[l, j], [], ["w2t"], q="pool")
                self.dma(a2t[64 * j:64 * j + 64, :].bitcast(F32R), a2[l, j], [], ["a2t"], q="pool")
            self.dma(g2t[:].bitcast(F32R), g2[l], [], ["g2t"], q="pool")
            pools = {}

            ONE = {"zdw", "zda", "zdg", "dw", "da", "dg", "tdw", "dar", "sgg", "ge0", "ge1", "rsf"}

            def T(name, w=512):
                if name not in pools:
                    pools[name] = self.rot(ph, "p_" + name, [128, w], 1 if name in ONE else 2)
                return pools[name].next()
            psr = self.rot(ph, "pps", [128, 512], 5, psum=True)
            zer5 = self.sb(ph, "zer5", [128, 512])
            self.memset(zer5[:], 0.0, ["zer5"])

            def load_shift(c, b, t0, L, lz, rz, name, f32r=False):
                zt, zk = T("z" + name, 514)
                self.dma(zt[:, 0:L + 2], ZR[c, :, b, t0:t0 + L + 2], [], [zk])
                if lz:
                    self.memset(zt[:, 0:1], 0.0, [zk])
                if rz:
                    self.memset(zt[:, L + 1:L + 2], 0.0, [zk])
                o, ok = T(name)
                self.ts(o[:, 0:L], zt[:, 1:L + 1], scv[:, 1, c:c + 1], None, ALU.mult, None, [zk, "scv"], [ok])
                self.stt(o[:, 0:L], zt[:, 0:L], scv[:, 0, c:c + 1], o[:, 0:L], ALU.mult, ALU.add, [zk, "scv", ok], [ok])
                self.stt(o[:, 0:L], zt[:, 2:L + 2], scv[:, 2, c:c + 1], o[:, 0:L], ALU.mult, ALU.add, [zk, "scv", ok], [ok])
                return o, ok

            for (b, kind, t0, L) in self.blocks:
                lz = (kind == 0) or (t0 == TC)
                rz = (kind == 0) or (t0 + L == TB)
                dw, dwk = load_shift(24, b, t0, L, lz, rz, "dw")
                da, dak = load_shift(25, b, t0, L, lz, rz, "da")
                dg, dgk = load_shift(26, b, t0, L, lz, rz, "dg")
                tdw, tdwk = T("tdw"); dar, dark = T("dar"); sgg, sggk = T("sgg")
                self.act(tdw[:, 0:L].bitcast(F32R), dw[:, 0:L], AF.Tanh, [dwk], [tdwk])
                self.cp(dar[:, 0:L].bitcast(F32R), da[:, 0:L], [dak], [dark], eng="pool")
                self.act(sgg[:, 0:L].bitcast(F32R), dg[:, 0:L], AF.Sigmoid, [dgk], [sggk])
                def pair_gen(p, b=b, t0=t0, L=L, lz=lz, rz=rz, tdw=tdw, tdwk=tdwk, dar=dar, dark=dark, sgg=sgg, sggk=sggk):
                    g = b * 8 + p
                    cs = slice(p * 128, (p + 1) * 128)
                    r, rk_ = load_shift(p, b, t0, L, lz, rz, "r")
                    k, kk_ = load_shift(8 + p, b, t0, L, lz, rz, "k")
                    v, vk_ = load_shift(16 + p, b, t0, L, lz, rz, "v")
                    self.dma(RL[g, :, t0 // 16:(t0 + L) // 16], r[:, 0:L].rearrange("p (n j) -> p n j", j=16)[:, :, 15], [rk_], [])
                    to_tm(4, v, vk_, b, p, t0, L)
                    kkr, kkrk = T("kkr"); sq, sqk = T("sq")
                    self.ts(kkr[:, 0:L], k[:, 0:L], kkw[:, p:p + 1], None, ALU.mult, None, [kk_, "kkw"], [kkrk])
                    self.act(sq[:, 0:L], kkr[:, 0:L], AF.Square, [kkrk], [sqk])
                    yield
                    ps, psk = psr.next()
                    self.mm(ps[:, 0:L], self.bo[:], sq[:, 0:L], True, True, ["bo", sqk], [psk])
                    rn, rnk = T("rn")
                    yield
                    self.ts(rn[:, 0:L], ps[:, 0:L], 1e-12, None, ALU.max, None, [psk], [rnk])
                    yield
                    self.act(rn[:, 0:L], rn[:, 0:L], AF.Ln, [rnk], [rnk])
                    self.act(rn[:, 0:L], rn[:, 0:L], AF.Exp, [rnk], [rnk], scale=-0.5)
                    yield
                    kap, kapk = T("kap")
                    self.tt(kap[:, 0:L], kkr[:, 0:L], rn[:, 0:L], ALU.mult, [kkrk, rnk], [kapk])
                    kA, kAk = T("kA")
                    self.ts(kA[:, 0:L], k[:, 0:L], kaw[:, p:p + 1], None, ALU.mult, None, [kk_, "kaw"], [kAk])
                    kf = None
                    for dr in range(2):
                        rows = slice(64 * dr, 64 * dr + 64)
                        ps, psk = psr.next()
                        self.mm(ps[:, 0:L], w2t[rows, cs].bitcast(F32R), tdw[rows, 0:L].bitcast(F32R), True, True, ["w2t", tdwk], [psk])
                        yield
                        e1, e1k = T(f"e1{dr}")
                        self.act(e1[:, 0:L], ps[:, 0:L], AF.Exp, [psk, "w0n"], [e1k], bias=w0n[:, dr, p:p + 1], scale=-1.0)
                        self.ts(e1[:, 0:L], e1[:, 0:L], 1.0, None, ALU.add, None, [e1k], [e1k])
                        P.op("dve", lambda e, o=e1[:, 0:L]: e.reciprocal(out=o, in_=o), reads=[e1k], writes=[e1k])
                        yield
                        nb = L // 16
                        ld, ldk = T(f"ld{dr}")
                        self.ts(ld[:, 0:L], e1[:, 0:L], -float(np.exp(-0.5)), None, ALU.mult, None, [e1k], [ldk])
                        cs_, csk = T(f"cs{dr}")
                        P.op("dve", lambda e, o=cs_[:, 0:L], i0=ld[:, 0:L], i1=zer5[:, 0:L]: e.tensor_tensor_scan(
                            out=o, data0=i0, data1=i1, initial=0.0, op0=ALU.add, op1=ALU.add), reads=[ldk, "zer5"], writes=[csk])
                        cl, clk = T(f"cl{dr}")
                        c3 = cs_[:, 0:L].rearrange("p (n j) -> p n j", j=16)
                        l3 = cl[:, 0:L].rearrange("p (n j) -> p n j", j=16)
                        self.cp(l3[:, 0, :], c3[:, 0, :], [csk], [clk])
                        if nb > 1:
                            self.tt(l3[:, 1:nb, :], c3[:, 1:nb, :], c3[:, 0:nb - 1, 15:16].broadcast_to([128, nb - 1, 16]), ALU.subtract, [csk], [clk])
                        gx, gxk = T(f"gx{dr}"); gi, gik = T(f"gi{dr}")
                        gev, gevk = T(f"ge{dr}")
                        if dr == 0:
                            self.tt(gx[:, 0:L], cl[:, 0:L], ld[:, 0:L], ALU.subtract, [clk, ldk], [gxk])
                            self.act(gx[:, 0:L], gx[:, 0:L], AF.Exp, [gxk], [gxk])
                            self.act(gi[:, 0:L], cl[:, 0:L], AF.Exp, [clk], [gik], scale=-1.0)
                            rs_, rsk_ = T("rsf")
                            self.act(rs_[:, 0:L], cl[:, 0:L], AF.Exp, [clk], [rsk_])
                            self.cp(gev[:, 0:nb], rs_[:, 0:L].rearrange("p (n j) -> p n j", j=16)[:, :, 15], [rsk_], [gevk])
                        else:
                            g3 = gx[:, 0:L].rearrange("p (n j) -> p n j", j=16)
                            self.tt(g3, l3[:, :, 15:16].broadcast_to([128, nb, 16]), l3, ALU.subtract, [clk], [gxk])
                            self.tt(gi[:, 0:L], gx[:, 0:L], ld[:, 0:L], ALU.add, [gxk, ldk], [gik])
                            self.act(gx[:, 0:L], gx[:, 0:L], AF.Exp, [gxk], [gxk])
                            self.act(gi[:, 0:L], gi[:, 0:L], AF.Exp, [gik], [gik], scale=-1.0)
                            rs_, rsk_ = gx, gxk
                            self.act(gev[:, 0:nb], l3[:, :, 15], AF.Exp, [clk], [gevk])
                        self.dma(GE[dr, g, :, t0 // 16:(t0 + L) // 16], gev[:, 0:nb], [gevk], [])
                        kt_, ktk_ = T(f"kt{dr}"); rt_, rtk_ = T(f"rt{dr}")
                        self.tt(kt_[:, 0:L], kap[:, 0:L], gx[:, 0:L], ALU.mult, [kapk, gxk], [ktk_], eng="pool")
                        self.tt(rt_[:, 0:L], r[:, 0:L], rs_[:, 0:L], ALU.mult, [rk_, rsk_], [rtk_], eng="pool")
                        self.dma(SC[2 * dr, g, :, t0:t0 + L], kt_[:, 0:L], [ktk_], [])
                        self.dma(SC[2 * dr + 1, g, :, t0:t0 + L], rt_[:, 0:L], [rtk_], [])
                        yield
                        ps, psk = psr.next()
                        self.mm(ps[:, 0:L], a2t[rows, cs].bitcast(F32R), dar[rows, 0:L].bitcast(F32R), True, True, ["a2t", dark], [psk])
                        yield
                        aa, aak = T(f"aa{dr}")
                        self.act(aa[:, 0:L], ps[:, 0:L], AF.Sigmoid, [psk, "a0t"], [aak], bias=a0t[:, dr, p:p + 1])
                        yield
                        kd, kdk = T(f"kd{dr}")
                        self.stt(kd[:, 0:L], aa[:, 0:L], -1.0, kA[:, 0:L], ALU.add, ALU.mult, [aak, kAk], [kdk])
                        self.tt(kd[:, 0:L], kd[:, 0:L], k[:, 0:L], ALU.add, [kdk, kk_], [kdk])
                        kdsc, kdsck = T(f"kdsc{dr}")
                        self.tt(kdsc[:, 0:L], kd[:, 0:L], gi[:, 0:L], ALU.mult, [kdk, gik], [kdsck], eng="pool")
                        to_tm(1 + 2 * dr, kdsc, kdsck, b, p, t0, L)
                        na, nak = T(f"na{dr}")
                        self.stt(na[:, 0:L], kap[:, 0:L], -1.0, aa[:, 0:L], ALU.mult, ALU.mult, [kapk, aak], [nak])
                        self.tt(na[:, 0:L], na[:, 0:L], gi[:, 0:L], ALU.mult, [nak, gik], [nak])
                        to_tm(2 * dr, na, nak, b, p, t0, L)
                        if dr == 0:
                            kf, kfk = kd, kdk
                    yield
                    t1, t1k = T("t1")
                    self.stt(t1[:, 0:L], r[:, 0:L], rkw[:, p:p + 1], kf[:, 0:L], ALU.mult, ALU.mult, [rk_, "rkw", kfk], [t1k])
                    ps, psk = psr.next()
                    self.mm(ps[:, 0:L], self.bo[:], t1[:, 0:L], True, True, ["bo", t1k], [psk])
                    yield
                    bn, bnk = T("bn")
                    self.tt(bn[:, 0:L], ps[:, 0:L], v[:, 0:L], ALU.mult, [psk, vk_], [bnk])
                    self.dma(BON[p, :, b, t0:t0 + L], bn[:, 0:L], [bnk], [])
                    ps, psk = psr.next()
                    self.mm(ps[:, 0:L], g2t[:, cs].bitcast(F32R), sgg[:, 0:L].bitcast(F32R), True, True, ["g2t", sggk], [psk])
                    gt, gtk = T("gt")
                    self.cp(gt[:, 0:L], ps[:, 0:L], [psk], [gtk], eng="act")
                    self.dma(GG[p, :, b, t0:t0 + L], gt[:, 0:L], [gtk], [])
                round_robin([pair_gen(p) for p in range(8)], 2)
            P.end_phase()

    def phase_scan(self, l, TBs=16, max_steps=None, skip=()):
        nc, P = self.nc, self.P
        BF = mybir.dt.bfloat16
        TC, TL, TB = self.TC, self.TL, self.TB
        SC = self.dram("SC", [9, 16, 128, TB])
        TM = self.dram("TM", [5, BL, TB, D], BF16)
        YD = self.dram("YD", [2, 2, TB, D])
        GE = self.dram("GE", [2, 16, 128, TB // 16]); RL = self.dram("RL", [16, 128, TB // 16])
        assert TC % TBs == 0 and TL % TBs == 0 and TBs == 16
        NBK = TB // 16
        with ExitStack() as ph:
            mask = self.sb(ph, "mask64", [64, 16, 64])
            m16 = self.sb(ph, "m16", [64, 16])
            sel0 = self.sb(ph, "sel0", [96, 32])
            sel = self.sb(ph, "sel", [96, 32], BF)
            mask96 = self.sb(ph, "mask96", [128, 16, 64])
            self.tt(m16[:], self.ident[0:64, 0:16], self.ident[0:64, 16:32], ALU.add, ["ident"], ["m16"])
            self.tt(m16[:], m16[:], self.ident[0:64, 32:48], ALU.add, ["ident", "m16"], ["m16"])
            self.tt(m16[:], m16[:], self.ident[0:64, 48:64], ALU.add, ["ident", "m16"], ["m16"])
            self.cp(mask[:], m16[:].unsqueeze(2).broadcast_to([64, 16, 64]), ["m16"], ["mask"])
            m16b = self.sb(ph, "m16b", [128, 16])
            self.tt(m16b[64:128], self.ident[64:128, 64:80], self.ident[64:128, 80:96], ALU.add, ["ident"], ["m16b"])
            self.tt(m16b[64:128], m16b[64:128], self.ident[64:128, 96:112], ALU.add, ["ident", "m16b"], ["m16b"])
            self.tt(m16b[64:128], m16b[64:128], self.ident[64:128, 112:128], ALU.add, ["ident", "m16b"], ["m16b"])
            self.cp(mask96[64:128], m16b[64:128].unsqueeze(2).broadcast_to([64, 16, 64]), ["m16b"], ["mask96"])
            self.memset(sel0[:], 0.0, ["sel0"])
            for m in range(2):
                P.op("dve", lambda e, m=m: e.tensor_reduce(out=sel0[64:96, m:m + 1], in_=self.ident[64:96, 64 + 16 * m:80 + 16 * m],
                                                          axis=AX.X, op=ALU.add), reads=["ident", "sel0"], writes=["sel0"])
            self.cp(sel[:], sel0[:], ["sel0"], ["sel"])
            get = self.sb(ph, "get", [128, 2, 16, NBK]); rlt = self.sb(ph, "rlt", [128, 16, NBK])
            for d in range(2):
                self.dma(get[:, d], GE[d].rearrange("g p n -> p g n"), [], ["get"])
            self.dma(rlt[:], RL.rearrange("g p n -> p g n"), [], ["rlt"])
            dirs = []
            for d in range(2):
                t = {}
                t["S"] = [self.sb(ph, f"S{d}{i}", [128, 16, 64]) for i in range(1)]
                t["Sb"] = self.sb(ph, f"Sb{d}", [128, 16, 64], BF)
                t["kst"] = self.rot(ph, f"kst{d}", [128, 16, TBs], 2)
                t["rst"] = self.rot(ph, f"rst{d}", [128, 16, TBs], 2)
                t["L1"] = [self.sb(ph, f"L1{d}{i}", [128, TBs, 128], BF) for i in range(2)]
                t["L2"] = [self.sb(ph, f"L2{d}{i}", [128, TBs, 128], BF) for i in range(2)]
                t["Vs"] = self.rot(ph, f"Vs{d}", [32, TBs, 64], 2, dt=BF)
                t["R1"] = self.rot(ph, f"R1{d}", [128, 16, 64], 2, dt=BF)
                for tl_ in t["R1"].tiles:
                    self.memset(tl_[:], 0.0, [], eng="pool")
                t["yb"] = self.rot(ph, f"yb{d}", [2, 1, D], 3)
                t["P12"] = ph.enter_context(nc.psum_tensor(self.uname(f"P12{d}"), [128, 1024], F32))
                t["Py"] = ph.enter_context(nc.psum_tensor(self.uname(f"Py{d}"), [32, 1024], F32))
                t["L1x"] = self.sb(ph, f"L1x{d}", [128, 128], BF)
                for i in range(2):
                    self.memset(t["L1"][i][:], 0.0, [f"L1{d}{i}"], eng="pool")
                    self.memset(t["L2"][i][:], 0.0, [f"L2{d}{i}"], eng="pool")
                self.memset(t["S"][0][:], 0.0, [f"S{d}0"])
                self.memset(t["Sb"][:], 0.0, [f"Sb{d}"])
                self.memset(t["L1x"][:], 0.0, [f"L1x{d}"])
                t["cur"] = 0
                t["blk"] = -1
                dirs.append(t)

            def load_dma(d, tb0):
                t = dirs[d]
                t["blk"] += 1
                i = t["blk"] % 2
                kst, kk = t["kst"].next(); rst, rk = t["rst"].next()
                self.dma(kst[:], SC[2 * d, :, :, tb0:tb0 + TBs].rearrange("g p t -> p g t"), [], [kk])
                self.dma(rst[:], SC[2 * d + 1, :, :, tb0:tb0 + TBs].rearrange("g p t -> p g t"), [], [rk])
                L1, L1k = t["L1"][i], f"L1{d}{i}"
                L2, L2k = t["L2"][i], f"L2{d}{i}"
                Vs, Vsk = t["Vs"].next()
                for m in range(2):
                    for b in range(BL):
                        r0 = 16 * m + 8 * b
                        cs = slice(64 * m, 64 * m + 64)
                        for wi, op in ((3, 2 * d), (0, 2 * d + 1)):
                            self.dma(L2[32 * wi + r0:32 * wi + r0 + 8, :, cs],
                                     TM[op, b, tb0:tb0 + TBs, :].rearrange("t (pp c) -> pp t c", c=128)[:, :, cs], [], [L2k])
                        self.dma(Vs[r0:r0 + 8, :, :],
                                 TM[4, b, tb0:tb0 + TBs, :].rearrange("t (pp c) -> pp t c", c=128)[:, :, cs], [], [Vsk])
                return dict(L1=L1, L1k=L1k, L2=L2, L2k=L2k, Vs=Vs, Vsk=Vsk, tb0=tb0, kst=kst, kk=kk, rst=rst, rk=rk)

            def load_fill(d, blk):
                L1, L1k, kst, kk, rst, rk, tb0 = blk["L1"], blk["L1k"], blk["kst"], blk["kk"], blk["rst"], blk["rk"], blk["tb0"]
                for m in range(2):
                    rows = slice(64 * m, 64 * m + 64)
                    self.cp(L1[rows, :, 96 + 16 * m:112 + 16 * m], kst[rows].rearrange("p g t -> p t g"), [kk], [L1k], eng="pool")
                    rc = slice(64 + 16 * m, 80 + 16 * m)
                    if d == 0:
                        if tb0 > 0:
                            self.cp(L1[rows, 0, rc], rlt[rows, :, tb0 // 16 - 1], ["rlt"], [L1k], eng="pool")
                        self.cp(L1[rows, 1:TBs, rc], rst[rows, :, 0:TBs - 1].rearrange("p g t -> p t g"), [rk], [L1k], eng="pool")
                    else:
                        self.cp(L1[rows, :, rc], rst[rows].rearrange("p g t -> p t g"), [rk], [L1k], eng="pool")
                return blk

            def stage_vx(d, blk, tc):
                t = dirs[d]
                R1, R1k = t["R1"].next()
                self.tt(R1[0:32], blk["Vs"][:, tc, :].unsqueeze(1).broadcast_to([32, 16, 64]), mask[0:32],
                        ALU.mult, [blk["Vsk"], "mask"], [R1k + "v"], eng="pool")
                return R1, R1k

            def stage_m1(d, L1ap, L1keys, R1=None, R1k=None):
                t = dirs[d]
                Sb, Sbk = t["Sb"], f"Sb{d}"
                P12, Pk = t["P12"], f"P12{d}"
                for h in range(2):
                    self.mm(P12[:, 512 * h:512 * h + 512], L1ap, Sb[:, 8 * h:8 * h + 8, :], True, True, L1keys + [Sbk], [Pk])
                if R1 is None:
                    R1, R1k = t["R1"].next()
                self.tt(R1[64:128], P12[64:128, :].rearrange("p (g v) -> p g v", v=64), mask96[64:128], ALU.mult, [Pk, "mask96"], [R1k + "s"])
                return R1, R1k

            def stage_upd(d, blk, tc, R1, R1k, last):
                t = dirs[d]
                S, Sk = t["S"][0], f"S{d}0"
                P12, Pk = t["P12"], f"P12{d}"
                for h in range(2):
                    cs = slice(512 * h, 512 * h + 512)
                    self.mm(P12[:, cs], blk["L2"][:, tc, :], R1[:, 8 * h:8 * h + 8, :], True, True, [blk["L2k"], R1k + "s", R1k + "v"], [Pk])
                self.tt(S[:], S[:], P12[:, :].rearrange("p (g v) -> p g v", v=64), ALU.add, [Sk, Pk], [Sk])
                if last:
                    bi_ = blk["tb0"] // 16
                    self.tt(S[:], S[:], get[:, d, :, bi_:bi_ + 1].broadcast_to([128, 16, 64]), ALU.mult, [Sk, "get"], [Sk])
                self.cp(t["Sb"][:], S[:], [Sk], [f"Sb{d}"], eng="act")

            def stage_y(d, R1, R1k, ytime):
                t = dirs[d]
                if ytime < 0:
                    return
                Py, Pyk = t["Py"], f"Py{d}"
                for h in range(2):
                    self.mm(Py[:, 512 * h:512 * h + 512], sel[64:96, :], R1[64:96, 8 * h:8 * h + 8, :], True, True, ["sel", R1k + "s"], [Pyk])
                yb, ybk = t["yb"].next()
                self.cp(yb[:, 0, :], Py[0:2, :], [Pyk], [ybk], eng="act")
                self.dma(YD[d, :, ytime, :], yb[:, 0, :], [ybk], [], q="act")

            fw_blocks = list(range(0, TB, TBs))
            bw_blocks = list(range(TC - TBs, -1, -TBs)) + list(range(TB - TBs, TC - 1, -TBs))
            nblk = len(fw_blocks)
            if max_steps is not None:
                nblk = max_steps // TBs
            def dir_gen(d):
                blist = fw_blocks if d == 0 else bw_blocks
                blk = load_fill(d, load_dma(d, blist[0]))
                for bi in range(nblk):
                    nxt = load_dma(d, blist[bi + 1]) if bi + 1 < nblk else None
                    for j in range(TBs):
                        if j == TBs // 2 and nxt is not None:
                            load_fill(d, nxt)
                        tc = j if d == 0 else TBs - 1 - j
                        R1, R1k = stage_vx(d, blk, tc)
                        R1, R1k = stage_m1(d, blk["L1"][:, tc, :], [blk["L1k"]], R1, R1k)
                        yield
                        stage_upd(d, blk, tc, R1, R1k, j == TBs - 1)
                        yield
                        tt_ = blk["tb0"] + tc
                        stage_y(d, R1, R1k, tt_ - 1 if d == 0 else tt_)
                        yield
                    blk = nxt
            g0, g1 = dir_gen(0), dir_gen(1)
            next(g0)
            round_robin([g1, g0], 2)
            t = dirs[0]
            for m in range(2):
                rows = slice(64 * m, 64 * m + 64)
                self.cp(t["L1x"][rows, 64 + 16 * m:80 + 16 * m], rlt[rows, :, fw_blocks[nblk - 1] // 16], ["rlt"], ["L1x0"], eng="pool")
            last_t = fw_blocks[nblk - 1] + TBs - 1
            R1, R1k = stage_m1(0, t["L1x"][:], ["L1x0"])
            stage_y(0, R1, R1k, last_t)
            if "SFIN" in self.dbg:
                SF = self.dram("SFIN", [2, 128, 1024])
                for d in range(2):
                    t = dirs[d]
                    self.dma(SF[d], t["S"][0][:].rearrange("p g v -> p (g v)"), [f"S{d}0"], [])
            P.end_phase()

    def phase_readout(self, l):
        nc, P = self.nc, self.P
        TC, TL, TB = self.TC, self.TL, self.TB
        lnx_g = self.inp("lnx_g", [DEPTH, D]); lnx_b = self.inp("lnx_b", [DEPTH, D])
        YD = self.dram("YD", [2, 2, TB, D])
        BON = self.dram("BON", [8, 128, BL, TB]); GG = self.dram("GG", [8, 128, BL, TB])
        YA = self.dram("YA", [8, 128, BL, TB])
        with ExitStack() as ph, nc.allow_non_contiguous_dma(reason="small param transposes"):
            lg = self.sb(ph, "lg", [128, 8]); lb = self.sb(ph, "lb", [128, 8])
            self.dma(lg[:], lnx_g[l].rearrange("(c p) -> p c", p=128), [], ["lg"])
            self.dma(lb[:], lnx_b[l].rearrange("(c p) -> p c", p=128), [], ["lb"])
            yt = self.rot(ph, "yt", [128, 2, 2, 64], 6)
            ys = self.rot(ph, "ysum", [128, 128], 4)
            pT = self.rot(ph, "pT", [128, 512], 3, psum=True)
            pS = self.rot(ph, "pS", [128, 512], 5, psum=True)
            pools = {}

            def T(name):
                if name not in pools:
                    pools[name] = self.rot(ph, "f_" + name, [128, 512], 3)
                return pools[name].next()
            def ro_gen(b, t0, L, p):
                nt = L // 128
                if True:
                    g = b * 8 + p
                    pt, pk = pT.next()
                    for i in range(nt):
                        y2, y2k = yt.next()
                        for d in range(2):
                            self.dma(y2[:, d], YD[d, :, t0 + i * 128:t0 + (i + 1) * 128, g * 64:(g + 1) * 64].rearrange("m t v -> t m v"), [], [y2k])
                        ysm, ysk = ys.next()
                        self.tt(ysm[:].rearrange("t (m v) -> t m v", v=64), y2[:, 0], y2[:, 1], ALU.add, [y2k], [ysk])
                        self.tr(pt[:, i * 128:(i + 1) * 128], ysm[:], [ysk], [pk])
                    yield
                    bn, bnk = T("bn"); gg, ggk = T("gg")
                    self.dma(bn[:, 0:L], BON[p, :, b, t0:t0 + L], [], [bnk])
                    self.dma(gg[:, 0:L], GG[p, :, b, t0:t0 + L], [], [ggk])
                    y, yk = T("y")
                    self.cp(y[:, 0:L], pt[:, 0:L], [pk], [yk], eng="act")
                    yield
                    pm, pmk = pS.next()
                    self.mm(pm[:, 0:L], self.bo[:], y[:, 0:L], True, True, ["bo", yk], [pmk])
                    yield
                    cen, cenk = T("cen")
                    self.stt(cen[:, 0:L], pm[:, 0:L], -1.0 / 64, y[:, 0:L], ALU.mult, ALU.add, [pmk, yk], [cenk])
                    yield
                    sq, sqk = T("sq")
                    self.act(sq[:, 0:L], cen[:, 0:L], AF.Square, [cenk], [sqk])
                    yield
                    pv, pvk = pS.next()
                    self.mm(pv[:, 0:L], self.bo[:], sq[:, 0:L], True, True, ["bo", sqk], [pvk])
                    yield
                    rs, rsk = T("rs")
                    self.ts(rs[:, 0:L], pv[:, 0:L], 1.0 / 64, GN_EPS, ALU.mult, ALU.add, [pvk], [rsk])
                    yield
                    self.act(rs[:, 0:L], rs[:, 0:L], AF.Sqrt, [rsk], [rsk])
                    yield
                    P.op("dve", lambda e, o=rs[:, 0:L]: e.reciprocal(out=o, in_=o), reads=[rsk], writes=[rsk])
                    self.tt(cen[:, 0:L], cen[:, 0:L], rs[:, 0:L], ALU.mult, [cenk, rsk], [cenk])
                    self.ts(cen[:, 0:L], cen[:, 0:L], lg[:, p:p + 1], lb[:, p:p + 1], ALU.mult, ALU.add, [cenk, "lg", "lb"], [cenk])
                    self.tt(cen[:, 0:L], cen[:, 0:L], bn[:, 0:L], ALU.add, [cenk, bnk], [cenk])
                    self.tt(cen[:, 0:L], cen[:, 0:L], gg[:, 0:L], ALU.mult, [cenk, ggk], [cenk])
                    self.dma(YA[p, :, b, t0:t0 + L], cen[:, 0:L], [cenk], [])
            round_robin([ro_gen(b, t0, L, p) for (b, kind, t0, L) in self.blocks for p in range(8)], 3)
            P.end_phase()

    def phase_sgu(self, l):
        nc, P = self.nc, self.P
        TC, TL, TB = self.TC, self.TL, self.TB
        sg_g = self.inp("sgu_ln_g", [DEPTH, D]); sg_b = self.inp("sgu_ln_b", [DEPTH, D])
        sg_w = self.inp("sgu_w", [DEPTH, 8, 128, 128]); sg_bias = self.inp("sgu_b", [DEPTH, 8, 128])
        VS = self.dram("VS", [BL, TB, D]); UT = self.dram("UT", [8, 128, BL, TB])
        YS = self.dram("YS", [8, 128, BL, TB])
        with ExitStack() as ph:
            grow = self.sb(ph, "grow", [128, D]); brow = self.sb(ph, "brow", [128, D])
            sgb = self.sb(ph, "sgb", [128, 8, 128])
            wsT = self.sb(ph, "wsT", [128, 8, 128], BF16)
            wld = self.rot(ph, "wld", [128, 128], 2)
            self.dma(grow[:], sg_g[l].partition_broadcast(128), [], ["grow"])
            self.dma(brow[:], sg_b[l].partition_broadcast(128), [], ["brow"])
            self.dma(sgb[:].rearrange("p g q -> p (g q)"), sg_bias[l].rearrange("g q -> (g q)").partition_broadcast(128), [], ["sgb"])
            pw = self.rot(ph, "pw", [128, 128], 2, psum=True)
            for g in range(8):
                w, wk = wld.next()
                self.dma(w[:], sg_w[l, g], [], [wk])
                pt, pk = pw.next()
                self.tr(pt[:], w[:], [wk], [pk])
                self.cp(wsT[:, g, :], pt[:], [pk], ["wsT"])
            vt = self.rot(ph, "vt", [128, D], 2)
            vn = self.rot(ph, "vn", [128, D], 2)
            vr = self.rot(ph, "vr", [128, D], 2, dt=BF16)
            st = self.rot(ph, "st", [128, 16], 2)
            ut = self.rot(ph, "ut", [128, 8, 128], 2)
            yo = self.rot(ph, "yo", [128, 8, 128], 2)
            pg = self.rot(ph, "pg", [128, 8, 128], 2, psum=True)
            for b in range(BL):
                for t0 in range(0, TB, 128):
                    v, vk = vt.next(); n, nk = vn.next(); s_, sk = st.next()
                    self.dma(v[:], VS[b, t0:t0 + 128, :], [], [vk])
                    u, uk = ut.next()
                    self.dma(u[:], UT[:, :, b, t0:t0 + 128].rearrange("g c p -> c g p"), [], [uk])
                    self.ln_tile(v, vk, n[:], nk, s_, sk)
                    self.tt(n[:], n[:], grow[:], ALU.mult, [nk, "grow"], [nk])
                    nr, nrk = vr.next()
                    self.tt(nr[:], n[:], brow[:], ALU.add, [nk, "brow"], [nrk])
                    pp, ppk = pg.next()
                    for g in range(8):
                        self.mm(pp[:, g, :], nr[:, g * 128:(g + 1) * 128], wsT[:, g, :], True, True, [nrk, "wsT"], [ppk])
                    o, ok = yo.next()
                    self.tt(o[:], pp[:], sgb[:], ALU.add, [ppk, "sgb"], [ok])
                    self.tt(o[:], o[:], u[:], ALU.mult, [ok, uk], [ok])
                    self.dma(YS[:, :, b, t0:t0 + 128].rearrange("g c p -> c g p"), o[:], [ok], [])
            P.end_phase()

    def phase_merge(self, l):
        nc, P = self.nc, self.P
        TC, TL, TB = self.TC, self.TL, self.TB
        wa = self.inp("w_branch_a", [DEPTH, D, D]); wb = self.inp("w_branch_b", [DEPTH, D, D]); wo = self.inp("w_out", [DEPTH, D, D])
        l1g = self.inp("ln1_g", [DEPTH, D]); l1b = self.inp("ln1_b", [DEPTH, D])
        modrow = self.dram(f"modrow{l}", [3, 6 * D])
        xres = self.dram("xres", [BL, TB, D])
        YA = self.dram("YA", [8, 128, BL, TB]); YS = self.dram("YS", [8, 128, BL, TB]); GT = self.dram("GT", [16, 128, BL, TB])
        with ExitStack() as ph:
            WO = self.sb(ph, "WO", [128, 8, D], BF16)
            for dc in range(8):
                self.dma(WO[:, dc, :], wo[l, dc * 128:(dc + 1) * 128, :], [], ["WO"], q="pool")
            wab = self.rot(ph, "wab", [128, 2, 8, 128], 3, dt=BF16)
            grow = self.sb(ph, "grow", [128, D]); brow = self.sb(ph, "brow", [128, D])
            self.dma(grow[:], l1g[l].partition_broadcast(128), [], ["grow"])
            self.dma(brow[:], l1b[l].partition_broadcast(128), [], ["brow"])
            gtr = self.sb(ph, "gtr", [128, 3, D])
            for j in range(3):
                self.dma(gtr[:, j, :], modrow[j, 2 * D:3 * D].partition_broadcast(128), [], ["gtr"])
            yaT = [self.sb(ph, f"yaT{ic}", [128, 512], BF16) for ic in range(8)]
            ysT = [self.sb(ph, f"ysT{ic}", [128, 512], BF16) for ic in range(8)]
            gin = self.rot(ph, "gin", [128, 512], 4)
            mT = self.sb(ph, "mT", [128, 8, 512], BF16)
            tmp = self.rot(ph, "mtmp", [128, 512], 2)
            pA = self.rot(ph, "pA", [128, 512], 2, psum=True)
            pB = self.rot(ph, "pB", [128, 512], 2, psum=True)
            pO = self.rot(ph, "pO", [128, D], 2, psum=True)
            xt = self.rot(ph, "xt", [128, D], 2); xo = self.rot(ph, "xo", [128, D], 2)
            st = self.rot(ph, "st", [128, 16], 2)
            for (b, kind, t0, L) in self.blocks:
                j = 0 if kind == 0 else 1 + b
                nt = L // 128
                ins_a = []; ins_s = []
                for ic in range(8):
                    a_, ak = yaT[ic], f"yaT{ic}"; s__, sk_ = ysT[ic], f"ysT{ic}"
                    self.dma(a_[:, 0:L], YA[ic, :, b, t0:t0 + L], [], [ak], q="pool")
                    self.dma(s__[:, 0:L], YS[ic, :, b, t0:t0 + L], [], [sk_], q="pool")
                    ins_a.append((a_, ak)); ins_s.append((s__, sk_))
                for oc in range(8):
                    ga, gak = gin.next(); gs, gsk = gin.next()
                    self.dma(ga[:, 0:L], GT[oc, :, b, t0:t0 + L], [], [gak])
                    self.dma(gs[:, 0:L], GT[8 + oc, :, b, t0:t0 + L], [], [gsk])
                    pa, pak = pA.next(); pb, pbk = pB.next()
                    W2_, w2k = wab.next()
                    self.dma(W2_[:, 0], wa[l, :, oc * 128:(oc + 1) * 128].rearrange("(c p) n -> p c n", p=128), [], [w2k], q="pool")
                    self.dma(W2_[:, 1], wb[l, :, oc * 128:(oc + 1) * 128].rearrange("(c p) n -> p c n", p=128), [], [w2k], q="pool")
                    for ic in range(8):
                        self.mm(pa[:, 0:L], W2_[:, 0, ic, :], ins_a[ic][0][:, 0:L],
                                ic == 0, ic == 7, [w2k, ins_a[ic][1]], [pak])
                    for ic in range(8):
                        self.mm(pb[:, 0:L], W2_[:, 1, ic, :], ins_s[ic][0][:, 0:L],
                                ic == 0, ic == 7, [w2k, ins_s[ic][1]], [pbk])
                    tm, tmk = tmp.next()
                    self.tt(tm[:, 0:L], pa[:, 0:L], ga[:, 0:L], ALU.mult, [pak, gak], [tmk])
                    self.tt(gs[:, 0:L], pb[:, 0:L], gs[:, 0:L], ALU.mult, [pbk, gsk], [gsk])
                    self.tt(mT[:, oc, 0:L], tm[:, 0:L], gs[:, 0:L], ALU.add, [tmk, gsk], [f"mT{oc}"])
                mk = [f"mT{oc}" for oc in range(8)]
                for i in range(nt):
                    po, pok = pO.next()
                    for h in range(2):
                        for ic in range(8):
                            self.mm(po[:, 512 * h:512 * h + 512], mT[:, ic, i * 128:(i + 1) * 128],
                                    WO[:, ic, 512 * h:512 * h + 512], ic == 0, ic == 7, mk + ["WO"], [pok])
                    x, xk = xt.next(); o, ok = xo.next(); s_, sk = st.next()
                    rows = slice(t0 + i * 128, t0 + (i + 1) * 128)
                    self.dma(x[:], xres[b, rows, :], [], [xk])
                    self.tt(o[:], po[:], gtr[:, j, :], ALU.mult, [pok, "gtr"], [ok])
                    self.stt(o[:], x[:], ALPHA, o[:], ALU.mult, ALU.add, [xk, ok], [ok])
                    self.ln_tile(o, ok, x[:], xk, s_, sk)
                    self.tt(x[:], x[:], grow[:], ALU.mult, [xk, "grow"], [xk])
                    self.tt(x[:], x[:], brow[:], ALU.add, [xk, "brow"], [xk])
                    self.dma(xres[b, rows, :], x[:], [xk], [])
            P.end_phase()

    def phase_moe_a(self, l):
        nc, P = self.nc, self.P
        TC, TL, TB = self.TC, self.TL, self.TB
        NTB = TB // 128
        rw_in = self.inp("router_w", [DEPTH, D, NEXP])
        modrow = self.dram(f"modrow{l}", [3, 6 * D])
        xres = self.dram("xres", [BL, TB, D])
        HMD = self.dram("HMD", [BL * TB, D + 16])
        MOED = self.dram("MOED", [BL * TB, D])
        IDXD = self.dram("IDXD", [128, NEXP * 5], I32)
        capL, capC = 2 * TL // NEXP, 2 * TC // NEXP
        assert capL == 256 and capC == 32
        with ExitStack() as ph, nc.allow_non_contiguous_dma(reason="small param transposes"):
            shr = self.sb(ph, "shr", [128, 3, D]); scr = self.sb(ph, "scr", [128, 3, D])
            for j in range(3):
                self.dma(shr[:, j, :], modrow[j, 3 * D:4 * D].partition_broadcast(128), [], ["shr"])
                self.dma(scr[:, j, :], modrow[j, 4 * D:5 * D].partition_broadcast(128), [], ["scr"])
            self.ts(scr[:], scr[:], 1.0, None, ALU.add, None, ["scr"], ["scr"])
            rw = self.sb(ph, "rw", [128, 8, NEXP])
            self.dma(rw[:], rw_in[l].rearrange("(c p) e -> p c e", p=128), [], ["rw"])
            zt = self.sb(ph, "zt", [128, D])
            self.memset(zt[:], 0.0, ["zt"])
            for i in range(BL * NTB):
                self.dma(MOED[i * 128:(i + 1) * 128, :], zt[:], ["zt"], [])
            xt = self.rot(ph, "xt", [128, D], 2); st = self.rot(ph, "st", [128, 16], 2)
            hm = self.rot(ph, "hm", [128, D + 16], 2)
            hmT = self.rot(ph, "hmT", [128, 8, 128], 2)
            sm = self.rot(ph, "sm", [128, 4], 2)
            ex = self.rot(ph, "ex", [128, NEXP], 2)
            pT = self.rot(ph, "pT", [128, 8, 128], 1, psum=True)
            pr = self.rot(ph, "pr", [128, 512], 2, psum=True)
            pa = self.rot(ph, "pa", [128, 512], 1, psum=True)
            aftL = [self.sb(ph, f"aftL{b}", [NEXP, TL]) for b in range(BL)]
            aftC = [self.sb(ph, f"aftC{b}", [NEXP, TC]) for b in range(BL)]
            def tile_gen(b, ti):
                if True:
                    t0 = ti * 128
                    j = 0 if t0 < TC else 1 + b
                    x, xk = xt.next(); s_, sk = st.next(); h, hk = hm.next()
                    self.dma(x[:], xres[b, t0:t0 + 128, :], [], [xk])
                    self.ln_tile(x, xk, h[:, 0:D], hk, s_, sk)
                    yield
                    self.tt(h[:, 0:D], h[:, 0:D], scr[:, j, :], ALU.mult, [hk, "scr"], [hk])
                    self.tt(h[:, 0:D], h[:, 0:D], shr[:, j, :], ALU.add, [hk, "shr"], [hk])
                    yield
                    pt, pk = pT.next()
                    for dc in range(8):
                        self.tr(pt[:, dc, :], h[:, dc * 128:(dc + 1) * 128], [hk], [pk])
                    yield
                    hT, hTk = hmT.next()
                    self.cp(hT[:], pt[:], [pk], [hTk], eng="act")
                    yield
                    pq, pqk = pr.next()
                    for dc in range(8):
                        self.mm(pq[:, 0:NEXP], hT[:, dc, :], rw[:, dc, :], dc == 0, dc == 7, [hTk, "rw"], [pqk])
                    yield
                    m_, mk_ = sm.next(); e_, ek_ = ex.next()
                    P.op("dve", lambda e, o=m_[:, 0:1], i_=pq[:, 0:NEXP]: e.tensor_reduce(out=o, in_=i_, axis=AX.X, op=ALU.max), reads=[pqk], writes=[mk_])
                    self.ts(m_[:, 1:2], m_[:, 0:1], -1.0, None, ALU.mult, None, [mk_], [mk_])
                    yield
                    P.op("act", lambda e, o=e_[:], i_=pq[:, 0:NEXP], bb=m_[:, 1:2], ac=m_[:, 2:3]: e.activation(out=o, in_=i_, func=AF.Exp, bias=bb, scale=1.0, accum_out=ac),
                         reads=[pqk, mk_], writes=[ek_, mk_])
                    P.op("dve", lambda e, o=m_[:, 3:4], i_=m_[:, 2:3]: e.reciprocal(out=o, in_=i_), reads=[mk_], writes=[mk_])
                    self.ts(h[:, D:D + NEXP], e_[:], m_[:, 3:4], None, ALU.mult, None, [ek_, mk_], [hk])
                    yield
                    self.dma(HMD[b * TB + t0:b * TB + t0 + 128, :], h[:], [hk], [])
                    pp, ppk = pa.next()
                    self.tr(pp[0:NEXP, 0:128], h[:, D:D + NEXP], [hk], [ppk])
                    if t0 < TC:
                        self.cp(aftC[b][:, t0:t0 + 128], pp[0:NEXP, 0:128], [ppk], [f"aftC{b}"], eng="act")
                    else:
                        self.cp(aftL[b][:, t0 - TC:t0 - TC + 128], pp[0:NEXP, 0:128], [ppk], [f"aftL{b}"], eng="act")
            round_robin([tile_gen(b, ti) for b in range(BL) for ti in range(NTB)], 1)
            postm = self.sb(ph, "postm", [128, BL * NTB, NEXP])
            work = self.sb(ph, "work", [NEXP, TL]); cum = self.sb(ph, "cum", [NEXP, TL]); zer = self.sb(ph, "zer", [NEXP, TL])
            m8 = self.sb(ph, "m8", [NEXP, 8])
            self.memset(zer[:], 0.0, ["zer"])
            pp2 = pa
            for b in range(BL):
                for (aft, ak, T_, cap, tbase) in ((aftC[b], f"aftC{b}", TC, capC, 0), (aftL[b], f"aftL{b}", TL, capL, TC)):
                    self.cp(work[:, 0:T_], aft[:, 0:T_], [ak], ["work"])
                    for r_ in range(cap // 8):
                        P.op("dve", lambda e, T_=T_: e.max(out=m8[:], in_=work[:, 0:T_]), reads=["work"], writes=["m8"])
                        if r_ < cap // 8 - 1:
                            P.op("dve", lambda e, T_=T_: e.match_replace(out=work[:, 0:T_], in_to_replace=m8[:], in_values=work[:, 0:T_], imm_value=-1e30),
                                 reads=["work", "m8"], writes=["work"])
                    self.ts(work[:, 0:T_], aft[:, 0:T_], m8[:, 7:8], None, ALU.is_ge, None, [ak, "m8"], ["work"])
                    P.op("dve", lambda e, T_=T_: e.tensor_tensor_scan(out=cum[:, 0:T_], data0=work[:, 0:T_], data1=zer[:, 0:T_], initial=0.0,
                                                                      op0=ALU.add, op1=ALU.add), reads=["work", "zer"], writes=["cum"])
                    self.tt(cum[:, 0:T_], cum[:, 0:T_], work[:, 0:T_], ALU.mult, ["cum", "work"], ["cum"])
                    self.ts(cum[:, 0:T_], cum[:, 0:T_], -1.0, None, ALU.add, None, ["cum"], ["cum"])
                    for i in range(T_ // 128):
                        gt = b * NTB + (tbase + i * 128) // 128
                        pq, pqk = pp2.next()
                        self.tr(pq[:, 256:256 + NEXP], cum[:, i * 128:(i + 1) * 128], ["cum"], [pqk])
                        self.cp(postm[:, gt, :], pq[:, 256:256 + NEXP], [pqk], ["postm"], eng="act")
            tgi = self.sb(ph, "tgi", [128, BL * NTB], I32); tg = self.sb(ph, "tg", [128, BL * NTB])
            iri = self.sb(ph, "iri", [128, 256], I32); ir = self.sb(ph, "ir", [128, 256])
            P.op("pool", lambda e: e.iota(tgi[:], pattern=[[128, BL * NTB]], base=0, channel_multiplier=1), reads=(), writes=["tgi"])
            P.op("pool", lambda e: e.iota(iri[:], pattern=[[1, 256]], base=0, channel_multiplier=0), reads=(), writes=["iri"])
            self.cp(tg[:], tgi[:], ["tgi"], ["tg"]); self.cp(ir[:], iri[:], ["iri"], ["ir"])
            idx = self.sb(ph, "idx", [128, NEXP, 5], I32)
            self.memset(idx[:], 0, ["idx"])
            Pm = self.rot(ph, "Pm", [128, 256], 3)
            Pmc = [self.sb(ph, f"Pmc{b}", [128, 2, 64]) for b in range(BL)]
            for b in range(BL):
                self.memset(Pmc[b][:], 0.0, [f"Pmc{b}"])
            pix = self.rot(ph, "pix", [128, 512], 3, psum=True)
            ntl = TL // 128
            for e_i in range(NEXP):
                for b in range(BL):
                    p0, p0k = pix.next(); p1, p1k = pix.next()
                    for i in range(ntl):
                        gt = b * NTB + TC // 128 + i
                        pm, pmk = Pm.next()
                        self.ts(pm[:], ir[:], postm[:, gt, e_i:e_i + 1], None, ALU.is_equal, None, ["ir", "postm"], [pmk])
                        self.mm(p0[:, 0:1], pm[:, 0:128], tg[:, gt:gt + 1], i == 0, i == ntl - 1, [pmk, "tg"], [p0k])
                        self.mm(p1[:, 0:1], pm[:, 128:256], tg[:, gt:gt + 1], i == 0, i == ntl - 1, [pmk, "tg"], [p1k])
                    self.cp(idx[:, e_i, 2 * b:2 * b + 1], p0[:, 0:1], [p0k], ["idx"])
                    self.cp(idx[:, e_i, 2 * b + 1:2 * b + 2], p1[:, 0:1], [p1k], ["idx"])
                pc, pck = pix.next()
                n_ = 0
                for b in range(BL):
                    for i in range(TC // 128):
                        gt = b * NTB + i
                        pmc = Pmc[b]; pmck = f"Pmc{b}"
                        self.ts(pmc[:, i, 32 * b:32 * b + 32], ir[:, 0:32], postm[:, gt, e_i:e_i + 1], None, ALU.is_equal, None, ["ir", "postm"], [pmck])
                        self.mm(pc[0:64, 0:1], pmc[:, i, :], tg[:, gt:gt + 1], n_ == 0, n_ == BL * (TC // 128) - 1, [pmck, "tg"], [pck])
                        n_ += 1
                self.cp(idx[0:64, e_i, 4:5], pc[0:64, 0:1], [pck], ["idx"])
            self.dma(IDXD[:, :], idx[:].rearrange("p e k -> p (e k)"), ["idx"], [])
            P.end_phase()

    def phase_moe_b(self, l):
        nc, P = self.nc, self.P
        TC, TL, TB = self.TC, self.TL, self.TB
        w1 = self.inp("exp_w1", [DEPTH, NEXP, D, DFF]); w3 = self.inp("exp_w3", [DEPTH, NEXP, D, DFF]); w2 = self.inp("exp_w2", [DEPTH, NEXP, DFF, D])
        HMD = self.dram("HMD", [BL * TB, D + 16]); MOED = self.dram("MOED", [BL * TB, D]); IDXD = self.dram("IDXD", [128, NEXP * 5], I32)
        NS = 640
        blks = [(0, 128), (128, 128), (256, 128), (384, 128), (512, 64)]
        with ExitStack() as ph:
            idx = self.sb(ph, "idx", [128, NEXP, 5], I32)
            self.dma(idx[:].rearrange("p e k -> p (e k)"), IDXD[:, :], [], ["idx"])
            xe = self.rot(ph, "xe", [128, D + 16], 3)
            gates = self.rot(ph, "gates", [128, 8], 2)
            xeTr = self.rot(ph, "xeT", [128, 8, NS], 2, dt=BF16); hidTr = self.rot(ph, "hidT", [128, 16, NS], 2, dt=BF16)
            for tl_ in xeTr.tiles:
                self.memset(tl_[:], 0.0, [])
            stg = self.rot(ph, "wstg", [128, 8, 512], 3)
            wr = self.rot(ph, "wr", [128, 8, 512], 4, dt=BF16)
            sl = self.rot(ph, "sl", [128, 512], 2)
            pg = self.rot(ph, "pg", [128, 8, 128], 1, psum=True)
            pmm = self.rot(ph, "pmm", [128, 512], 6, psum=True)

            def load_w(src_ap):
                s_, sk_ = stg.next(); w_, wk_ = wr.next()
                self.dma(s_[:], src_ap, [], [sk_])
                self.cp(w_[:], s_[:], [sk_], [wk_], eng="pool")
                return w_, wk_
            def load_w2(src_ap):
                s_, sk_ = stg.next(); w_, wk_ = wr.next()
                self.dma(s_[:, 0:4, :], src_ap, [], [sk_])
                self.cp(w_[:, 0:4, :], s_[:, 0:4, :], [sk_], [wk_], eng="pool")
                return w_, wk_
            for e_i in range(NEXP):
                gt_, gk_ = gates.next()
                xeT, xeTk = xeTr.next(); hidT, hidTk = hidTr.next()
                for bi, (c0, n) in enumerate(blks):
                    x_, xk_ = xe.next()
                    P.dma("pool", lambda e, x_=x_, n=n, bi=bi, e_i=e_i: e.indirect_dma_start(
                        out=x_[0:n, :], out_offset=None, in_=HMD[:, :],
                        in_offset=bass.IndirectOffsetOnAxis(ap=idx[0:n, e_i, bi:bi + 1], axis=0)), reads=["idx"], writes=[xk_])
                    self.cp(gt_[0:n, bi:bi + 1], x_[0:n, D + e_i:D + e_i + 1], [xk_], [gk_], eng="act")
                    pt, pk = pg.next()
                    for dc in range(8):
                        self.tr(pt[:, dc, 0:n], x_[0:n, dc * 128:(dc + 1) * 128], [xk_], [pk])
                    self.cp(xeT[:, :, c0:c0 + n], pt[:, :, 0:n], [pk], [xeTk])
                for fb in range(4):
                    W1, W1k = load_w(w1[l, e_i, :, fb * 512:(fb + 1) * 512].rearrange("(c p) n -> p c n", p=128))
                    W3, W3k = load_w(w3[l, e_i, :, fb * 512:(fb + 1) * 512].rearrange("(c p) n -> p c n", p=128))
                    for fc in range(4):
                        fcc = fb * 4 + fc
                        for (n0, nn) in ((0, 512), (512, 128)):
                            p1, p1k = pmm.next(); p3, p3k = pmm.next()
                            for dc in range(8):
                                self.mm(p1[:, 0:nn], W1[:, dc, fc * 128:(fc + 1) * 128], xeT[:, dc, n0:n0 + nn],
                                        dc == 0, dc == 7, [W1k, xeTk], [p1k])
                            for dc in range(8):
                                self.mm(p3[:, 0:nn], W3[:, dc, fc * 128:(fc + 1) * 128], xeT[:, dc, n0:n0 + nn],
                                        dc == 0, dc == 7, [W3k, xeTk], [p3k])
                            s_, sk_ = sl.next()
                            self.act(s_[:, 0:nn], p1[:, 0:nn], AF.Silu, [p1k], [sk_])
                            self.tt(hidT[:, fcc, n0:n0 + nn], s_[:, 0:nn], p3[:, 0:nn], ALU.mult, [sk_, p3k], [hidTk])
                yts = {}
                for half in range(2):
                    accs = [pmm.next() for _ in blks]
                    for pc in range(4):
                        W2, W2k = load_w2(w2[l, e_i, pc * 512:(pc + 1) * 512, half * 512:(half + 1) * 512].rearrange("(c p) n -> p c n", p=128))
                        for bi, (c0, n) in enumerate(blks):
                            po, pok = accs[bi]
                            for fc in range(4):
                                fcc = pc * 4 + fc
                                self.mm(po[:, :], hidT[:, fcc, c0:c0 + 128], W2[:, fc, :],
                                        fcc == 0, fcc == 15, [hidTk, W2k], [pok])
                    for bi, (c0, n) in enumerate(blks):
                        po, pok = accs[bi]
                        if bi not in yts:
                            yts[bi] = self.yes_tile(ph, bi)
                        yt_, ytk_ = yts[bi]
                        self.ts(yt_[0:n, half * 512:(half + 1) * 512], po[0:n, :], gt_[0:n, bi:bi + 1], None, ALU.mult, None, [pok, gk_], [ytk_])
                for bi, (c0, n) in enumerate(blks):
                    yt_, ytk_ = yts[bi]
                    P.dma("pool", lambda e, yt_=yt_, n=n, bi=bi, e_i=e_i: e.indirect_dma_start(
                        out=MOED[:, :], out_offset=bass.IndirectOffsetOnAxis(ap=idx[0:n, e_i, bi:bi + 1], axis=0),
                        in_=yt_[0:n, :], in_offset=None, compute_op=ALU.add), reads=["idx", ytk_], writes=["MOED"])
            P.end_phase()

    def yes_tile(self, ph, bi):
        if not hasattr(self, "_yes") or self._yes_ph is not ph:
            self._yes = [self.rot(ph, f"yesb{k}", [128, D], 1) for k in range(5)]
            self._yes_ph = ph
        return self._yes[bi].next()

    def phase_ln2(self, l, last):
        nc, P = self.nc, self.P
        TC, TL, TB = self.TC, self.TL, self.TB
        l2g = self.inp("ln2_g", [DEPTH, D]); l2b = self.inp("ln2_b", [DEPTH, D])
        modrow = self.dram(f"modrow{l}", [3, 6 * D])
        xres = self.dram("xres", [BL, TB, D]); MOED = self.dram("MOED", [BL * TB, D])
        if last:
            if "out" not in self.drams:
                self.drams["out"] = nc.dram_tensor("out", [BL, TL, D], F32, kind="ExternalOutput").ap()
            outp = self.drams["out"]
        with ExitStack() as ph:
            grow = self.sb(ph, "grow", [128, D]); brow = self.sb(ph, "brow", [128, D])
            self.dma(grow[:], l2g[l].partition_broadcast(128), [], ["grow"])
            self.dma(brow[:], l2b[l].partition_broadcast(128), [], ["brow"])
            gtr = self.sb(ph, "gtr", [128, 3, D])
            for j in range(3):
                self.dma(gtr[:, j, :], modrow[j, 5 * D:6 * D].partition_broadcast(128), [], ["gtr"])
            xt = self.rot(ph, "xt", [128, D], 3); mt = self.rot(ph, "mt", [128, D], 3); st = self.rot(ph, "st", [128, 16], 2)
            for b in range(BL):
                for t0 in range(0, TB, 128):
                    if last and t0 < TC:
                        continue
                    j = 0 if t0 < TC else 1 + b
                    x, xk = xt.next(); m, mk = mt.next(); s_, sk = st.next()
                    self.dma(x[:], xres[b, t0:t0 + 128, :], [], [xk])
                    self.dma(m[:], MOED[b * TB + t0:b * TB + t0 + 128, :], [], [mk])
                    self.tt(m[:], m[:], gtr[:, j, :], ALU.mult, [mk, "gtr"], [mk])
                    self.stt(m[:], x[:], ALPHA, m[:], ALU.mult, ALU.add, [xk, mk], [mk])
                    self.ln_tile(m, mk, x[:], xk, s_, sk)
                    self.tt(x[:], x[:], grow[:], ALU.mult, [xk, "grow"], [xk])
                    self.tt(x[:], x[:], brow[:], ALU.add, [xk, "brow"], [xk])
                    if last:
                        self.dma(outp[b, t0 - TC:t0 - TC + 128, :], x[:], [xk], [])
                    else:
                        self.dma(xres[b, t0:t0 + 128, :], x[:], [xk], [])
            P.end_phase()

    def build_all(self):
        self.phase_init()
        for l in range(DEPTH):
            self.phase_mod(l)
            self.phase_proj(l)
            self.phase_prep(l)
            self.phase_scan(l)
            self.phase_readout(l)
            self.phase_sgu(l)
            self.phase_merge(l)
            self.phase_moe_a(l)
            self.phase_moe_b(l)
            self.phase_ln2(l, l == DEPTH - 1)
        return self.finish()

    def finish(self):
        self.es.close()
        return self.nc


_CACHE = {}

WEIGHTS = ["ada_w", "ada_b", "w_in", "shift_conv", "w0", "w2", "a0", "a2", "g2", "k_k", "k_a", "r_k", "lnx_g", "lnx_b",
           "sgu_ln_g", "sgu_ln_b", "sgu_w", "sgu_b", "w_branch_a", "w_branch_b", "w_out", "ln1_g", "ln1_b", "router_w",
           "exp_w1", "exp_w3", "exp_w2", "ln2_g", "ln2_b"]


def kernel(**inputs):
    x = np.asarray(inputs["x"], dtype=np.float32)
    B, TL, _ = x.shape
    TC = inputs["ctx"].shape[1]
    if "nc" not in _CACHE:
        kb = KB(TC, TL)
        _CACHE["nc"] = kb.build_all()
        _CACHE["names"] = list(kb.inputs.keys())
    nc = _CACHE["nc"]
    names = _CACHE["names"]
    shared = {k: np.ascontiguousarray(np.asarray(inputs[k], dtype=np.float32)) for k in WEIGHTS if k in names}
    cctx = np.ascontiguousarray(np.asarray(inputs["c_ctx"], dtype=np.float32)[None, :])
    in_maps = []
    for c in range(NCORE):
        m = dict(shared)
        m["x"] = np.ascontiguousarray(x[BL * c:BL * c + BL])
        m["ctx"] = np.ascontiguousarray(np.asarray(inputs["ctx"], dtype=np.float32)[BL * c:BL * c + BL])
        m["c"] = np.ascontiguousarray(np.asarray(inputs["c"], dtype=np.float32)[BL * c:BL * c + BL])
        m["c_ctx"] = cctx
        in_maps.append({k: m[k] for k in names})
    res = run_bass_kernel_spmd(nc, in_maps, core_ids=list(range(NCORE)))
    return np.concatenate([res.results[c]["out"] for c in range(NCORE)], axis=0).astype(np.float32)
```

```python
from contextlib import ExitStack
import numpy as np
import concourse.bass as bass
import concourse.mybir as mybir
from concourse.bass_utils import run_bass_kernel_spmd

F32 = mybir.dt.float32
F32R = mybir.dt.float32r
BF16 = mybir.dt.bfloat16
I32 = mybir.dt.int32
U32 = mybir.dt.uint32
AF = mybir.ActivationFunctionType
ALU = mybir.AluOpType
AX = mybir.AxisListType

D = 1024
DEPTH = 2
NCORE = 8
BL = 2
N_RW = 3456
N_IN = 7552
H_A = 16
NEXP = 16
DFF = 2048
ALPHA = (2 * DEPTH) ** 0.25
LN_EPS = 1e-5
GN_EPS = 64e-5

ENGS = ("pe", "act", "dve", "pool", "sp")
EPOCH = 60000
DMA_RING = 8


class Prog:
    def __init__(self, nc, es, nsem=90):
        self.nc = nc
        self.free_sems = [es.enter_context(nc.semaphore(f"s{i}")) for i in range(nsem)]
        self.ops = {e: [] for e in ENGS}
        self.psem = {e: self.free_sems.pop() for e in ENGS}
        self.pcnt = {e: 0 for e in ENGS}
        self.dsem = {e: [self.free_sems.pop() for _ in range(DMA_RING)] for e in ("sp", "act", "pool")}
        self.dcnt = {e: 0 for e in ("sp", "act", "pool")}
        self.last_write = {}
        self.readers = {}
        self.waited = {e: {} for e in ENGS}
        self.ninst = 0

    def _deps(self, eng, reads, writes):
        toks = []
        for k in reads:
            t = self.last_write.get(k)
            if t is not None:
                toks.append(t)
        for k in writes:
            t = self.last_write.get(k)
            if t is not None:
                toks.append(t)
            toks.extend(self.readers.get(k, ()))
        if eng == "pe":
            ps = self.psem["pe"]
            toks = [t for t in toks if t[0] is not ps]
        return toks

    def _commit(self, tok, reads, writes):
        for k in reads:
            self.readers.setdefault(k, []).append(tok)
        for k in writes:
            self.last_write[k] = tok
            self.readers[k] = []

    def _waits(self, eng, toks):
        best = {}
        for (s, v) in toks:
            key = id(s)
            if key not in best or best[key][1] < v:
                best[key] = (s, v)
        out = []
        w = self.waited[eng]
        for key, (s, v) in best.items():
            if w.get(key, 0) >= v:
                continue
            w[key] = v
            out.append((s, v))
        return out

    def op(self, eng, fn, reads=(), writes=()):
        if self.pcnt[eng] >= EPOCH:
            self.psem[eng] = self.free_sems.pop()
            self.pcnt[eng] = 0
        toks = self._deps(eng, reads, writes)
        waits = self._waits(eng, toks)
        self.pcnt[eng] += 1
        tok = (self.psem[eng], self.pcnt[eng])
        self.ops[eng].append((fn, waits, (tok[0], 1)))
        self._commit(tok, reads, writes)
        return tok

    def dma(self, q, fn, reads=(), writes=()):
        i = self.dcnt[q]
        self.dcnt[q] += 1
        sem = self.dsem[q][i % DMA_RING]
        toks = self._deps(q, reads, writes)
        if i >= DMA_RING:
            toks.append((sem, 16 * (i // DMA_RING)))
        waits = self._waits(q, toks)
        tok = (sem, 16 * (i // DMA_RING + 1))
        self.ops[q].append((fn, waits, (sem, 16)))
        self._commit(tok, reads, writes)
        return tok

    def end_phase(self):
        toks = []
        for e in ENGS:
            if self.pcnt[e] > 0:
                toks.append((self.psem[e], self.pcnt[e]))
        for q in self.dcnt:
            n = self.dcnt[q]
            for j in range(min(n, DMA_RING)):
                last = ((n - 1 - j) // DMA_RING) * DMA_RING + j
                toks.append((self.dsem[q][j], 16 * (last // DMA_RING + 1)))
        self.ops["sp"].append((None, self._waits("sp", toks), None))
        nc = self.nc
        ops = self.ops
        me = self

        def run(engobj, lst):
            for fn, waits, inc in lst:
                for (s, v) in waits:
                    engobj.wait_ge(s, v)
                if fn is None:
                    continue
                ins = fn(engobj)
                ins.then_inc(inc[0], inc[1])
                me.ninst += 1

        with nc.Block() as block:
            @block.sync
            def _(e):
                run(e, ops["sp"])

            @block.scalar
            def _(e):
                run(e, ops["act"])

            @block.vector
            def _(e):
                run(e, ops["dve"])

            @block.gpsimd
            def _(e):
                run(e, ops["pool"])

            @block.tensor
            def _(e):
                run(e, ops["pe"])
        self.ops = {e: [] for e in ENGS}
        self.last_write = {}
        self.readers = {}
        for q in self.dcnt:
            if self.dcnt[q] // DMA_RING > 2500:
                self.dsem[q] = [self.free_sems.pop() for _ in range(DMA_RING)]
                self.dcnt[q] = 0


def round_robin(gens, width):
    gens = list(gens)
    active = []
    while gens or active:
        while gens and len(active) < width:
            active.append(gens.pop(0))
        for g in list(active):
            try:
                next(g)
            except StopIteration:
                active.remove(g)


class Rot:
    def __init__(self, tiles, name):
        self.tiles = tiles
        self.name = name
        self.i = -1

    def next(self):
        self.i += 1
        j = self.i % len(self.tiles)
        return self.tiles[j], f"{self.name}{j}"


class KB:
    def __init__(self, TC, TL, dbg=()):
        self.TC, self.TL = TC, TL
        self.TB = TC + TL
        self.dbg = set(dbg)
        self.nc = bass.Bass("TRN2", target_bir_lowering=False)
        self.es = ExitStack()
        self.P = Prog(self.nc, self.es)
        self.inputs = {}
        self.drams = {}
        nc = self.nc
        self.ident = self.es.enter_context(nc.sbuf_tensor("ident", [128, 128], F32))
        self.bo = self.es.enter_context(nc.sbuf_tensor("bo", [128, 128], F32))
        self.blocks = []
        for b in range(BL):
            self.blocks.append((b, 0, 0, TC))
            for t0 in range(0, TL, 512):
                self.blocks.append((b, 1, TC + t0, min(512, TL - t0)))

    def inp(self, name, shape):
        if name not in self.inputs:
            self.inputs[name] = self.nc.dram_tensor(name, list(shape), F32, kind="ExternalInput").ap()
        return self.inputs[name]

    def dram(self, name, shape, dt=F32):
        if name not in self.drams:
            kind = "ExternalOutput" if name in self.dbg else "Internal"
            self.drams[name] = self.nc.dram_tensor(name, list(shape), dt, kind=kind).ap()
        return self.drams[name]

    def uname(self, name):
        self.uid = getattr(self, "uid", 0) + 1
        return f"{name}_u{self.uid}"

    def sb(self, ph, name, shape, dt=F32):
        return ph.enter_context(self.nc.sbuf_tensor(self.uname(name), list(shape), dt))

    def rot(self, ph, name, shape, n, dt=F32, psum=False):
        if psum:
            tiles = [ph.enter_context(self.nc.psum_tensor(self.uname(f"{name}{i}"), list(shape), dt)) for i in range(n)]
        else:
            tiles = [ph.enter_context(self.nc.sbuf_tensor(self.uname(f"{name}{i}"), list(shape), dt)) for i in range(n)]
        return Rot(tiles, name)

    def dma(self, out, in_, R, W, q="sp", **kw):
        self.P.dma(q, lambda e: e.dma_start(out=out, in_=in_, **kw), reads=R, writes=W)

    def mm(self, out, lhsT, rhs, start, stop, R, W):
        self.P.op("pe", lambda e: e.matmul(out, lhsT=lhsT, rhs=rhs, start=start, stop=stop), reads=R, writes=W)

    def tr(self, out, in_, R, W, ident=None):
        idn = self.ident[0:in_.shape[0], 0:in_.shape[0]] if ident is None else ident
        self.P.op("pe", lambda e: e.transpose(out, in_, idn), reads=list(R) + ["ident"], writes=W)

    def act(self, out, in_, func, R, W, bias=None, scale=1.0):
        kw = {}
        if bias is not None:
            kw["bias"] = bias
        self.P.op("act", lambda e: e.activation(out=out, in_=in_, func=func, scale=scale, **kw), reads=R, writes=W)

    def ts(self, out, in0, s1, s2, op0, op1, R, W, eng="dve"):
        self.P.op(eng, lambda e: e.tensor_scalar(out=out, in0=in0, scalar1=s1, scalar2=s2, op0=op0, op1=op1) if op1 is not None
                  else e.tensor_scalar(out=out, in0=in0, scalar1=s1, scalar2=None, op0=op0), reads=R, writes=W)

    def tt(self, out, in0, in1, op, R, W, eng="dve"):
        self.P.op(eng, lambda e: e.tensor_tensor(out=out, in0=in0, in1=in1, op=op), reads=R, writes=W)

    def stt(self, out, in0, scalar, in1, op0, op1, R, W):
        self.P.op("dve", lambda e: e.scalar_tensor_tensor(out=out, in0=in0, scalar=scalar, in1=in1, op0=op0, op1=op1), reads=R, writes=W)

    def cp(self, out, in_, R, W, eng="dve"):
        if eng == "act":
            self.P.op("act", lambda e: e.copy(out=out, in_=in_), reads=R, writes=W)
        else:
            self.P.op(eng, lambda e: e.tensor_copy(out=out, in_=in_), reads=R, writes=W)

    def memset(self, ap, val, W, eng="dve"):
        self.P.op(eng, lambda e: e.memset(ap, val), reads=(), writes=W)

    def phase_consts(self):
        nc, P = self.nc, self.P
        with ExitStack() as ph:
            ones = self.sb(ph, "ones_i", [128, 128])
            self.memset(ones[:], 1.0, ["ones_i"])
            self.memset(self.ident[:], 0.0, ["ident"])
            self.memset(self.bo[:], 0.0, ["bo"])
            P.op("pool", lambda e: e.affine_select(out=self.ident[:], in_=ones[:], pattern=[[-1, 128]], compare_op=ALU.is_equal,
                                                   fill=0.0, base=0, channel_multiplier=1),
                 reads=["ones_i", "ident"], writes=["ident"])
            self.memset(self.bo[0:64, 0:64], 1.0, ["bo"])
            self.memset(self.bo[64:128, 64:128], 1.0, ["bo"])
            P.end_phase()

    def phase_init(self):
        nc, P = self.nc, self.P
        TC, TL, TB = self.TC, self.TL, self.TB
        x = self.inp("x", [BL, TL, D])
        ctx = self.inp("ctx", [BL, TC, D])
        xres = self.dram("xres", [BL, TB, D])
        self.phase_consts()
        for b in range(BL):
            self.dma(xres[b, 0:TC, :], ctx[b], [], [])
            self.dma(xres[b, TC:TB, :], x[b], [], [])
        P.end_phase()

    def phase_mod(self, l):
        nc, P = self.nc, self.P
        c = self.inp("c", [BL, D])
        cc = self.inp("c_ctx", [1, D])
        ada_w = self.inp("ada_w", [DEPTH, D, 6 * D])
        ada_b = self.inp("ada_b", [DEPTH, 6 * D])
        modrow = self.dram(f"modrow{l}", [3, 6 * D])
        with ExitStack() as ph, nc.allow_non_contiguous_dma(reason="small param transposes"):
            cT = self.sb(ph, "cT", [128, 8, 3])
            scT = self.sb(ph, "scT", [128, 8, 3])
            abt = self.sb(ph, "abt", [3, 6 * D])
            mrow = self.sb(ph, "mrow", [3, 6 * D])
            wr = self.rot(ph, "adaw", [128, 8, 512], 3)
            pr = self.rot(ph, "pmod", [3, 512], 2, psum=True)
            self.dma(cT[:, :, 0], cc[0].rearrange("(c p) -> p c", p=128), [], ["cT"])
            for b in range(BL):
                self.dma(cT[:, :, 1 + b], c[b].rearrange("(c p) -> p c", p=128), [], ["cT"])
            self.dma(abt[:], ada_b[l].partition_broadcast(3), [], ["abt"])
            self.act(scT[:], cT[:], AF.Silu, ["cT"], ["scT"])
            for nb in range(12):
                w, wk = wr.next()
                self.dma(w[:], ada_w[l, :, nb * 512:(nb + 1) * 512].rearrange("(c p) n -> p c n", p=128), [], [wk])
                pt, pk = pr.next()
                for dc in range(8):
                    self.mm(pt[:], scT[:, dc, :], w[:, dc, :], dc == 0, dc == 7, ["scT", wk], [pk])
                self.tt(mrow[:, nb * 512:(nb + 1) * 512], pt[:], abt[:, nb * 512:(nb + 1) * 512], ALU.add, [pk, "abt"], ["mrow"])
            self.dma(modrow[:, :], mrow[:], ["mrow"], [])
            P.end_phase()

    def ln_tile(self, xt, xk, xn, xnk, st, stk, eps=LN_EPS):
        P = self.P
        for h in range(2):
            P.op("dve", lambda e, h=h: e.bn_stats(out=st[:, 6 * h:6 * h + 6], in_=xt[:, 512 * h:512 * h + 512]), reads=[xk], writes=[stk])
        P.op("dve", lambda e: e.bn_aggr(out=st[:, 12:14], in_=st[:, 0:12]), reads=[stk], writes=[stk])
        self.ts(st[:, 14:15], st[:, 13:14], eps, None, ALU.add, None, [stk], [stk])
        self.act(st[:, 14:15], st[:, 14:15], AF.Sqrt, [stk], [stk])
        P.op("dve", lambda e: e.reciprocal(out=st[:, 14:15], in_=st[:, 14:15]), reads=[stk], writes=[stk])
        self.ts(xn, xt[:], st[:, 12:13], st[:, 14:15], ALU.subtract, ALU.mult, [xk, stk], [xnk])

    def load_modfm(self, ph, l):
        modrow = self.dram(f"modrow{l}", [3, 6 * D])
        modfm = self.sb(ph, "modfm", [128, 3, 6, 8])
        for j in range(3):
            self.dma(modfm[:, j], modrow[j].rearrange("(s c p) -> p s c", p=128, c=8), [], ["modfm"])
        return modfm

    def phase_proj(self, l):
        nc, P = self.nc, self.P
        TC, TL, TB = self.TC, self.TL, self.TB
        w_in = self.inp("w_in", [DEPTH, D, N_IN])
        xres = self.dram("xres", [BL, TB, D])
        ZR = self.dram("ZR", [27, 128, BL, TB + 2])
        UT = self.dram("UT", [8, 128, BL, TB])
        GT = self.dram("GT", [16, 128, BL, TB])
        VS = self.dram("VS", [BL, TB, D])
        cblocks = [(i * 512, 512) for i in range(6)] + [(3072, 384)] + [(3456 + i * 512, 512) for i in range(8)]
        with ExitStack() as ph, nc.allow_non_contiguous_dma(reason="small param transposes"):
            modfm = self.load_modfm(ph, l)
            ops1 = self.sb(ph, "ops1", [128, 3, 8])
            self.ts(ops1[:], modfm[:, :, 1, :], 1.0, None, ALU.add, None, ["modfm"], ["ops1"])
            xr = self.rot(ph, "xt", [128, D], 2)
            xnb = self.sb(ph, "xnb", [128, 4, D])
            st = self.rot(ph, "st", [128, 16], 2)
            hT = self.sb(ph, "hT", [128, 8, BL * TB], BF16)
            wr = self.rot(ph, "wblk", [128, 8, 512], 2, dt=BF16)
            wst = self.rot(ph, "wstg", [128, 8, 512], 3)
            ptr = self.rot(ph, "ptr", [128, 512], 2, psum=True)
            pmm = self.rot(ph, "pmm", [128, 512], 4, psum=True)
            stg = self.rot(ph, "stg", [128, 512], 6)
            for (b, kind, t0, L) in self.blocks:
                j = 0 if kind == 0 else 1 + b
                nt = L // 128
                off = b * TB + t0
                for i in range(nt):
                    xt, xk = xr.next()
                    s_, sk = st.next()
                    self.dma(xt[:], xres[b, t0 + i * 128:t0 + (i + 1) * 128, :], [], [xk])
                    self.ln_tile(xt, xk, xnb[:, i, :], f"xnb{i}", s_, sk)
                for dc in range(8):
                    pt, pk = ptr.next()
                    for i in range(nt):
                        self.tr(pt[:, i * 128:(i + 1) * 128], xnb[:, i, dc * 128:(dc + 1) * 128], [f"xnb{i}"], [pk])
                    self.act(hT[:, dc, off:off + L], pt[:, 0:L], AF.Identity, [pk, "ops1", "modfm"], ["hT"],
                             bias=modfm[:, j, 0, dc:dc + 1], scale=ops1[:, j, dc:dc + 1])
            for (c0, cw) in cblocks:
                w, wk = wr.next()
                ws, wsk = wst.next()
                self.dma(ws[:, :, 0:cw], w_in[l, :, c0:c0 + cw].rearrange("(c p) n -> p c n", p=128), [], [wsk])
                self.cp(w[:, :, 0:cw], ws[:, :, 0:cw], [wsk], [wk], eng="pool")
                for (b, kind, t0, L) in self.blocks:
                    nt = L // 128
                    off = b * TB + t0
                    if 4480 <= c0 < 5504:
                        for i in range(nt):
                            pm, pmk = pmm.next()
                            for dc in range(8):
                                self.mm(pm[:, 0:cw], hT[:, dc, off + i * 128:off + (i + 1) * 128], w[:, dc, 0:cw],
                                        dc == 0, dc == 7, ["hT", wk], [pmk])
                            sg, sgk = stg.next()
                            self.act(sg[:, 0:cw], pm[:, 0:cw], AF.Gelu, [pmk], [sgk])
                            self.dma(VS[b, t0 + i * 128:t0 + (i + 1) * 128, c0 - 4480:c0 - 4480 + cw], sg[:, 0:cw], [sgk], [])
                        continue
                    for cc in range(cw // 128):
                        col = c0 + cc * 128
                        pm, pmk = pmm.next()
                        for dc in range(8):
                            self.mm(pm[:, 0:L], w[:, dc, cc * 128:(cc + 1) * 128], hT[:, dc, off:off + L],
                                    dc == 0, dc == 7, ["hT", wk], [pmk])
                        sg, sgk = stg.next()
                        if col < N_RW:
                            self.cp(sg[:, 0:L], pm[:, 0:L], [pmk], [sgk])
                            self.dma(ZR[col // 128, :, b, 1 + t0:1 + t0 + L], sg[:, 0:L], [sgk], [])
                        elif col < 4480:
                            self.act(sg[:, 0:L], pm[:, 0:L], AF.Gelu, [pmk], [sgk])
                            self.dma(UT[(col - N_RW) // 128, :, b, t0:t0 + L], sg[:, 0:L], [sgk], [])
                        else:
                            self.act(sg[:, 0:L], pm[:, 0:L], AF.Sigmoid, [pmk], [sgk])
                            self.dma(GT[(col - 5504) // 128, :, b, t0:t0 + L], sg[:, 0:L], [sgk], [])
            P.end_phase()

    def phase_prep(self, l):
        nc, P = self.nc, self.P
        TC, TL, TB = self.TC, self.TL, self.TB
        shc = self.inp("shift_conv", [DEPTH, 3, N_RW])
        w0 = self.inp("w0", [DEPTH, 2, D]); w2 = self.inp("w2", [DEPTH, 2, 64, D])
        a0 = self.inp("a0", [DEPTH, 2, D]); a2 = self.inp("a2", [DEPTH, 2, 64, D])
        g2 = self.inp("g2", [DEPTH, 128, D])
        k_k = self.inp("k_k", [DEPTH, D]); k_a = self.inp("k_a", [DEPTH, D]); r_k = self.inp("r_k", [DEPTH, H_A, 64])
        ZR = self.dram("ZR", [27, 128, BL, TB + 2])
        SC = self.dram("SC", [9, 16, 128, TB])
        BON = self.dram("BON", [8, 128, BL, TB])
        GG = self.dram("GG", [8, 128, BL, TB])
        TM = self.dram("TM", [5, BL, TB, D], BF16)
        GE = self.dram("GE", [2, 16, 128, TB // 16])
        RL = self.dram("RL", [16, 128, TB // 16])
        with ExitStack() as ph, nc.allow_non_contiguous_dma(reason="small param transposes"):
            ptm = self.rot(ph, "ptm", [128, 512], 2, psum=True)
            stm = self.rot(ph, "stm", [128, 512], 3, dt=BF16)

            def to_tm(op, src, srck, b, p, t0, L):
                pt, pk = ptm.next()
                nt = L // 128
                for i in range(nt):
                    self.tr(pt[:, i * 128:(i + 1) * 128], src[:, i * 128:(i + 1) * 128], [srck], [pk])
                st_, stk_ = stm.next()
                self.cp(st_[:, 0:L], pt[:, 0:L], [pk], [stk_], eng="act")
                self.dma(TM[op, b, t0:t0 + L, p * 128:(p + 1) * 128].rearrange("(i t) c -> t i c", t=128),
                         st_[:, 0:L].rearrange("t (i c) -> t i c", c=128), [stk_], [], q="act")
            scv = self.sb(ph, "scv", [128, 3, 27])
            w0n = self.sb(ph, "w0n", [128, 2, 8]); a0t = self.sb(ph, "a0t", [128, 2, 8])
            kkw = self.sb(ph, "kkw", [128, 8]); kaw = self.sb(ph, "kaw", [128, 8]); rkw = self.sb(ph, "rkw", [128, 8])
            w2t = self.sb(ph, "w2t", [128, D]); a2t = self.sb(ph, "a2t", [128, D]); g2t = self.sb(ph, "g2t", [128, D])
            self.dma(scv[:], shc[l].rearrange("j (c p) -> p j c", p=128), [], ["scv"])
            self.dma(w0n[:], w0[l].rearrange("j (c p) -> p j c", p=128), [], ["w0n"])
            self.ts(w0n[:], w0n[:], -1.0, None, ALU.mult, None, ["w0n"], ["w0n"])
            self.dma(a0t[:], a0[l].rearrange("j (c p) -> p j c", p=128), [], ["a0t"])
            self.dma(kkw[:], k_k[l].rearrange("(c p) -> p c", p=128), [], ["kkw"])
            self.dma(kaw[:], k_a[l].rearrange("(c p) -> p c", p=128), [], ["kaw"])
            self.dma(rkw[:], r_k[l].rearrange("h k -> (h k)").rearrange("(c p) -> p c", p=128), [], ["rkw"])
            for j in range(2):
                self.dma(w2t[64 * j:64 * j + 64, :].bitcast(F32R), w2[l, j], [], ["w2t"], q="pool")
                self.dma(a2t[64 * j:64 * j + 64, :].bitcast(F32R), a2[l, j], [], ["a2t"], q="pool")
            self.dma(g2t[:].bitcast(F32R), g2[l], [], ["g2t"], q="pool")
            pools = {}

            ONE = {"zdw", "zda", "zdg", "dw", "da", "dg", "tdw", "dar", "sgg", "ge0", "ge1", "rsf"}

            def T(name, w=512):
                if name not in pools:
                    pools[name] = self.rot(ph, "p_" + name, [128, w], 1 if name in ONE else 2)
                return pools[name].next()
            psr = self.rot(ph, "pps", [128, 512], 5, psum=True)
            zer5 = self.sb(ph, "zer5", [128, 512])
            self.memset(zer5[:], 0.0, ["zer5"])

            def load_shift(c, b, t0, L, lz, rz, name, f32r=False):
                zt, zk = T("z" + name, 514)
                self.dma(zt[:, 0:L + 2], ZR[c, :, b, t0:t0 + L + 2], [], [zk])
                if lz:
                    self.memset(zt[:, 0:1], 0.0, [zk])
                if rz:
                    self.memset(zt[:, L + 1:L + 2], 0.0, [zk])
                o, ok = T(name)
                self.ts(o[:, 0:L], zt[:, 1:L + 1], scv[:, 1, c:c + 1], None, ALU.mult, None, [zk, "scv"], [ok])
                self.stt(o[:, 0:L], zt[:, 0:L], scv[:, 0, c:c + 1], o[:, 0:L], ALU.mult, ALU.add, [zk, "scv", ok], [ok])
                self.stt(o[:, 0:L], zt[:, 2:L + 2], scv[:, 2, c:c + 1], o[:, 0:L], ALU.mult, ALU.add, [zk, "scv", ok], [ok])
                return o, ok

            for (b, kind, t0, L) in self.blocks:
                lz = (kind == 0) or (t0 == TC)
                rz = (kind == 0) or (t0 + L == TB)
                dw, dwk = load_shift(24, b, t0, L, lz, rz, "dw")
                da, dak = load_shift(25, b, t0, L, lz, rz, "da")
                dg, dgk = load_shift(26, b, t0, L, lz, rz, "dg")
                tdw, tdwk = T("tdw"); dar, dark = T("dar"); sgg, sggk = T("sgg")
                self.act(tdw[:, 0:L].bitcast(F32R), dw[:, 0:L], AF.Tanh, [dwk], [tdwk])
                self.cp(dar[:, 0:L].bitcast(F32R), da[:, 0:L], [dak], [dark], eng="pool")
                self.act(sgg[:, 0:L].bitcast(F32R), dg[:, 0:L], AF.Sigmoid, [dgk], [sggk])
                def pair_gen(p, b=b, t0=t0, L=L, lz=lz, rz=rz, tdw=tdw, tdwk=tdwk, dar=dar, dark=dark, sgg=sgg, sggk=sggk):
                    g = b * 8 + p
                    cs = slice(p * 128, (p + 1) * 128)
                    r, rk_ = load_shift(p, b, t0, L, lz, rz, "r")
                    k, kk_ = load_shift(8 + p, b, t0, L, lz, rz, "k")
                    v, vk_ = load_shift(16 + p, b, t0, L, lz, rz, "v")
                    self.dma(RL[g, :, t0 // 16:(t0 + L) // 16], r[:, 0:L].rearrange("p (n j) -> p n j", j=16)[:, :, 15], [rk_], [])
                    to_tm(4, v, vk_, b, p, t0, L)
                    kkr, kkrk = T("kkr"); sq, sqk = T("sq")
                    self.ts(kkr[:, 0:L], k[:, 0:L], kkw[:, p:p + 1], None, ALU.mult, None, [kk_, "kkw"], [kkrk])
                    self.act(sq[:, 0:L], kkr[:, 0:L], AF.Square, [kkrk], [sqk])
                    yield
                    ps, psk = psr.next()
                    self.mm(ps[:, 0:L], self.bo[:], sq[:, 0:L], True, True, ["bo", sqk], [psk])
                    rn, rnk = T("rn")
                    yield
                    self.ts(rn[:, 0:L], ps[:, 0:L], 1e-12, None, ALU.max, None, [psk], [rnk])
                    yield
                    self.act(rn[:, 0:L], rn[:, 0:L], AF.Ln, [rnk], [rnk])
                    self.act(rn[:, 0:L], rn[:, 0:L], AF.Exp, [rnk], [rnk], scale=-0.5)
                    yield
                    kap, kapk = T("kap")
                    self.tt(kap[:, 0:L], kkr[:, 0:L], rn[:, 0:L], ALU.mult, [kkrk, rnk], [kapk])
                    kA, kAk = T("kA")
                    self.ts(kA[:, 0:L], k[:, 0:L], kaw[:, p:p + 1], None, ALU.mult, None, [kk_, "kaw"], [kAk])
                    kf = None
                    for dr in range(2):
                        rows = slice(64 * dr, 64 * dr + 64)
                        ps, psk = psr.next()
                        self.mm(ps[:, 0:L], w2t[rows, cs].bitcast(F32R), tdw[rows, 0:L].bitcast(F32R), True, True, ["w2t", tdwk], [psk])
                        yield
                        e1, e1k = T(f"e1{dr}")
                        self.act(e1[:, 0:L], ps[:, 0:L], AF.Exp, [psk, "w0n"], [e1k], bias=w0n[:, dr, p:p + 1], scale=-1.0)
                        self.ts(e1[:, 0:L], e1[:, 0:L], 1.0, None, ALU.add, None, [e1k], [e1k])
                        P.op("dve", lambda e, o=e1[:, 0:L]: e.reciprocal(out=o, in_=o), reads=[e1k], writes=[e1k])
                        yield
                        nb = L // 16
                        ld, ldk = T(f"ld{dr}")
                        self.ts(ld[:, 0:L], e1[:, 0:L], -float(np.exp(-0.5)), None, ALU.mult, None, [e1k], [ldk])
                        cs_, csk = T(f"cs{dr}")
                        P.op("dve", lambda e, o=cs_[:, 0:L], i0=ld[:, 0:L], i1=zer5[:, 0:L]: e.tensor_tensor_scan(
                            out=o, data0=i0, data1=i1, initial=0.0, op0=ALU.add, op1=ALU.add), reads=[ldk, "zer5"], writes=[csk])
                        cl, clk = T(f"cl{dr}")
                        c3 = cs_[:, 0:L].rearrange("p (n j) -> p n j", j=16)
                        l3 = cl[:, 0:L].rearrange("p (n j) -> p n j", j=16)
                        self.cp(l3[:, 0, :], c3[:, 0, :], [csk], [clk])
                        if nb > 1:
                            self.tt(l3[:, 1:nb, :], c3[:, 1:nb, :], c3[:, 0:nb - 1, 15:16].broadcast_to([128, nb - 1, 16]), ALU.subtract, [csk], [clk])
                        gx, gxk = T(f"gx{dr}"); gi, gik = T(f"gi{dr}")
                        gev, gevk = T(f"ge{dr}")
                        if dr == 0:
                            self.tt(gx[:, 0:L], cl[:, 0:L], ld[:, 0:L], ALU.subtract, [clk, ldk], [gxk])
                            self.act(gx[:, 0:L], gx[:, 0:L], AF.Exp, [gxk], [gxk])
                            self.act(gi[:, 0:L], cl[:, 0:L], AF.Exp, [clk], [gik], scale=-1.0)
                            rs_, rsk_ = T("rsf")
                            self.act(rs_[:, 0:L], cl[:, 0:L], AF.Exp, [clk], [rsk_])
                            self.cp(gev[:, 0:nb], rs_[:, 0:L].rearrange("p (n j) -> p n j", j=16)[:, :, 15], [rsk_], [gevk])
                        else:
                            g3 = gx[:, 0:L].rearrange("p (n j) -> p n j", j=16)
                            self.tt(g3, l3[:, :, 15:16].broadcast_to([128, nb, 16]), l3, ALU.subtract, [clk], [gxk])
                            self.tt(gi[:, 0:L], gx[:, 0:L], ld[:, 0:L], ALU.add, [gxk, ldk], [gik])
                            self.act(gx[:, 0:L], gx[:, 0:L], AF.Exp, [gxk], [gxk])
                            self.act(gi[:, 0:L], gi[:, 0:L], AF.Exp, [gik], [gik], scale=-1.0)
                            rs_, rsk_ = gx, gxk
                            self.act(gev[:, 0:nb], l3[:, :, 15], AF.Exp, [clk], [gevk])
                        self.dma(GE[dr, g, :, t0 // 16:(t0 + L) // 16], gev[:, 0:nb], [gevk], [])
                        kt_, ktk_ = T(f"kt{dr}"); rt_, rtk_ = T(f"rt{dr}")
                        self.tt(kt_[:, 0:L], kap[:, 0:L], gx[:, 0:L], ALU.mult, [kapk, gxk], [ktk_], eng="pool")
                        self.tt(rt_[:, 0:L], r[:, 0:L], rs_[:, 0:L], ALU.mult, [rk_, rsk_], [rtk_], eng="pool")
                        self.dma(SC[2 * dr, g, :, t0:t0 + L], kt_[:, 0:L], [ktk_], [], q="pool")
                        self.dma(SC[2 * dr + 1, g, :, t0:t0 + L], rt_[:, 0:L], [rtk_], [], q="pool")
                        yield
                        ps, psk = psr.next()
                        self.mm(ps[:, 0:L], a2t[rows, cs].bitcast(F32R), dar[rows, 0:L].bitcast(F32R), True, True, ["a2t", dark], [psk])
                        yield
                        aa, aak = T(f"aa{dr}")
                        self.act(aa[:, 0:L], ps[:, 0:L], AF.Sigmoid, [psk, "a0t"], [aak], bias=a0t[:, dr, p:p + 1])
                        yield
                        kd, kdk = T(f"kd{dr}")
                        self.stt(kd[:, 0:L], aa[:, 0:L], -1.0, kA[:, 0:L], ALU.add, ALU.mult, [aak, kAk], [kdk])
                        self.tt(kd[:, 0:L], kd[:, 0:L], k[:, 0:L], ALU.add, [kdk, kk_], [kdk])
                        kdsc, kdsck = T(f"kdsc{dr}")
                        self.tt(kdsc[:, 0:L], kd[:, 0:L], gi[:, 0:L], ALU.mult, [kdk, gik], [kdsck], eng="pool")
                        to_tm(1 + 2 * dr, kdsc, kdsck, b, p, t0, L)
                        na, nak = T(f"na{dr}")
                        self.stt(na[:, 0:L], kap[:, 0:L], -1.0, aa[:, 0:L], ALU.mult, ALU.mult, [kapk, aak], [nak])
                        self.tt(na[:, 0:L], na[:, 0:L], gi[:, 0:L], ALU.mult, [nak, gik], [nak])
                        to_tm(2 * dr, na, nak, b, p, t0, L)
                        if dr == 0:
                            kf, kfk = kd, kdk
                    yield
                    t1, t1k = T("t1")
                    self.stt(t1[:, 0:L], r[:, 0:L], rkw[:, p:p + 1], kf[:, 0:L], ALU.mult, ALU.mult, [rk_, "rkw", kfk], [t1k])
                    ps, psk = psr.next()
                    self.mm(ps[:, 0:L], self.bo[:], t1[:, 0:L], True, True, ["bo", t1k], [psk])
                    yield
                    bn, bnk = T("bn")
                    self.tt(bn[:, 0:L], ps[:, 0:L], v[:, 0:L], ALU.mult, [psk, vk_], [bnk])
                    self.dma(BON[p, :, b, t0:t0 + L], bn[:, 0:L], [bnk], [])
                    ps, psk = psr.next()
                    self.mm(ps[:, 0:L], g2t[:, cs].bitcast(F32R), sgg[:, 0:L].bitcast(F32R), True, True, ["g2t", sggk], [psk])
                    gt, gtk = T("gt")
                    self.cp(gt[:, 0:L], ps[:, 0:L], [psk], [gtk], eng="act")
                    self.dma(GG[p, :, b, t0:t0 + L], gt[:, 0:L], [gtk], [], q="act")
                round_robin([pair_gen(p) for p in range(8)], 2)
            P.end_phase()

    def phase_scan(self, l, TBs=16, max_steps=None, skip=()):
        nc, P = self.nc, self.P
        BF = mybir.dt.bfloat16
        TC, TL, TB = self.TC, self.TL, self.TB
        SC = self.dram("SC", [9, 16, 128, TB])
        TM = self.dram("TM", [5, BL, TB, D], BF16)
        YD = self.dram("YD", [2, 2, TB, D])
        GE = self.dram("GE", [2, 16, 128, TB // 16]); RL = self.dram("RL", [16, 128, TB // 16])
        assert TC % TBs == 0 and TL % TBs == 0 and TBs == 16
        NBK = TB // 16
        with ExitStack() as ph:
            mask = self.sb(ph, "mask64", [64, 16, 64])
            m16 = self.sb(ph, "m16", [64, 16])
            sel0 = self.sb(ph, "sel0", [96, 32])
            sel = self.sb(ph, "sel", [96, 32], BF)
            mask96 = self.sb(ph, "mask96", [128, 16, 64])
            self.tt(m16[:], self.ident[0:64, 0:16], self.ident[0:64, 16:32], ALU.add, ["ident"], ["m16"])
            self.tt(m16[:], m16[:], self.ident[0:64, 32:48], ALU.add, ["ident", "m16"], ["m16"])
            self.tt(m16[:], m16[:], self.ident[0:64, 48:64], ALU.add, ["ident", "m16"], ["m16"])
            self.cp(mask[:], m16[:].unsqueeze(2).broadcast_to([64, 16, 64]), ["m16"], ["mask"])
            m16b = self.sb(ph, "m16b", [128, 16])
            self.tt(m16b[64:128], self.ident[64:128, 64:80], self.ident[64:128, 80:96], ALU.add, ["ident"], ["m16b"])
            self.tt(m16b[64:128], m16b[64:128], self.ident[64:128, 96:112], ALU.add, ["ident", "m16b"], ["m16b"])
            self.tt(m16b[64:128], m16b[64:128], self.ident[64:128, 112:128], ALU.add, ["ident", "m16b"], ["m16b"])
            self.cp(mask96[64:128], m16b[64:128].unsqueeze(2).broadcast_to([64, 16, 64]), ["m16b"], ["mask96"])
            self.memset(sel0[:], 0.0, ["sel0"])
            for m in range(2):
                P.op("dve", lambda e, m=m: e.tensor_reduce(out=sel0[64:96, m:m + 1], in_=self.ident[64:96, 64 + 16 * m:80 + 16 * m],
                                                          axis=AX.X, op=ALU.add), reads=["ident", "sel0"], writes=["sel0"])
            self.cp(sel[:], sel0[:], ["sel0"], ["sel"])
            get = self.sb(ph, "get", [128, 2, 16, NBK]); rlt = self.sb(ph, "rlt", [128, 16, NBK])
            for d in range(2):
                self.dma(get[:, d], GE[d].rearrange("g p n -> p g n"), [], ["get"])
            self.dma(rlt[:], RL.rearrange("g p n -> p g n"), [], ["rlt"])
            dirs = []
            for d in range(2):
                t = {}
                t["S"] = [self.sb(ph, f"S{d}{i}", [128, 16, 64]) for i in range(1)]
                t["Sb"] = self.sb(ph, f"Sb{d}", [128, 16, 64], BF)
                t["kst"] = self.rot(ph, f"kst{d}", [128, 16, TBs], 2)
                t["rst"] = self.rot(ph, f"rst{d}", [128, 16, TBs], 2)
                t["L1"] = [self.sb(ph, f"L1{d}{i}", [128, TBs, 128], BF) for i in range(2)]
                t["L2"] = [self.sb(ph, f"L2{d}{i}", [128, TBs, 128], BF) for i in range(2)]
                t["Vs"] = self.rot(ph, f"Vs{d}", [32, TBs, 64], 2, dt=BF)
                t["R1"] = self.rot(ph, f"R1{d}", [128, 16, 64], 2, dt=BF)
                for tl_ in t["R1"].tiles:
                    self.memset(tl_[:], 0.0, [], eng="pool")
                t["yb"] = self.rot(ph, f"yb{d}", [2, 1, D], 3)
                t["P12"] = ph.enter_context(nc.psum_tensor(self.uname(f"P12{d}"), [128, 1024], F32))
                t["Py"] = ph.enter_context(nc.psum_tensor(self.uname(f"Py{d}"), [32, 1024], F32))
                t["L1x"] = self.sb(ph, f"L1x{d}", [128, 128], BF)
                for i in range(2):
                    self.memset(t["L1"][i][:], 0.0, [f"L1{d}{i}"], eng="pool")
                    self.memset(t["L2"][i][:], 0.0, [f"L2{d}{i}"], eng="pool")
                self.memset(t["S"][0][:], 0.0, [f"S{d}0"])
                self.memset(t["Sb"][:], 0.0, [f"Sb{d}"])
                self.memset(t["L1x"][:], 0.0, [f"L1x{d}"])
                t["cur"] = 0
                t["blk"] = -1
                dirs.append(t)

            def load_dma(d, tb0):
                t = dirs[d]
                t["blk"] += 1
                i = t["blk"] % 2
                kst, kk = t["kst"].next(); rst, rk = t["rst"].next()
                self.dma(kst[:], SC[2 * d, :, :, tb0:tb0 + TBs].rearrange("g p t -> p g t"), [], [kk])
                self.dma(rst[:], SC[2 * d + 1, :, :, tb0:tb0 + TBs].rearrange("g p t -> p g t"), [], [rk])
                L1, L1k = t["L1"][i], f"L1{d}{i}"
                L2, L2k = t["L2"][i], f"L2{d}{i}"
                Vs, Vsk = t["Vs"].next()
                for m in range(2):
                    for b in range(BL):
                        r0 = 16 * m + 8 * b
                        cs = slice(64 * m, 64 * m + 64)
                        for wi, op in ((3, 2 * d), (0, 2 * d + 1)):
                            self.dma(L2[32 * wi + r0:32 * wi + r0 + 8, :, cs],
                                     TM[op, b, tb0:tb0 + TBs, :].rearrange("t (pp c) -> pp t c", c=128)[:, :, cs], [], [L2k])
                        self.dma(Vs[r0:r0 + 8, :, :],
                                 TM[4, b, tb0:tb0 + TBs, :].rearrange("t (pp c) -> pp t c", c=128)[:, :, cs], [], [Vsk])
                return dict(L1=L1, L1k=L1k, L2=L2, L2k=L2k, Vs=Vs, Vsk=Vsk, tb0=tb0, kst=kst, kk=kk, rst=rst, rk=rk)

            def load_fill(d, blk):
                L1, L1k, kst, kk, rst, rk, tb0 = blk["L1"], blk["L1k"], blk["kst"], blk["kk"], blk["rst"], blk["rk"], blk["tb0"]
                for m in range(2):
                    rows = slice(64 * m, 64 * m + 64)
                    self.cp(L1[rows, :, 96 + 16 * m:112 + 16 * m], kst[rows].rearrange("p g t -> p t g"), [kk], [L1k], eng="pool")
                    rc = slice(64 + 16 * m, 80 + 16 * m)
                    if d == 0:
                        if tb0 > 0:
                            self.cp(L1[rows, 0, rc], rlt[rows, :, tb0 // 16 - 1], ["rlt"], [L1k], eng="pool")
                        self.cp(L1[rows, 1:TBs, rc], rst[rows, :, 0:TBs - 1].rearrange("p g t -> p t g"), [rk], [L1k], eng="pool")
                    else:
                        self.cp(L1[rows, :, rc], rst[rows].rearrange("p g t -> p t g"), [rk], [L1k], eng="pool")
                return blk

            def stage_vx(d, blk, tc):
                t = dirs[d]
                R1, R1k = t["R1"].next()
                self.tt(R1[0:32], blk["Vs"][:, tc, :].unsqueeze(1).broadcast_to([32, 16, 64]), mask[0:32],
                        ALU.mult, [blk["Vsk"], "mask"], [R1k + "v"], eng="pool")
                return R1, R1k

            def stage_m1(d, L1ap, L1keys, R1=None, R1k=None):
                t = dirs[d]
                Sb, Sbk = t["Sb"], f"Sb{d}"
                P12, Pk = t["P12"], f"P12{d}"
                for h in range(2):
                    self.mm(P12[:, 512 * h:512 * h + 512], L1ap, Sb[:, 8 * h:8 * h + 8, :], True, True, L1keys + [Sbk], [Pk])
                if R1 is None:
                    R1, R1k = t["R1"].next()
                self.tt(R1[64:128], P12[64:128, :].rearrange("p (g v) -> p g v", v=64), mask96[64:128], ALU.mult, [Pk, "mask96"], [R1k + "s"])
                return R1, R1k

            def stage_upd(d, blk, tc, R1, R1k, last):
                t = dirs[d]
                S, Sk = t["S"][0], f"S{d}0"
                P12, Pk = t["P12"], f"P12{d}"
                for h in range(2):
                    cs = slice(512 * h, 512 * h + 512)
                    self.mm(P12[:, cs], blk["L2"][:, tc, :], R1[:, 8 * h:8 * h + 8, :], True, True, [blk["L2k"], R1k + "s", R1k + "v"], [Pk])
                self.tt(S[:], S[:], P12[:, :].rearrange("p (g v) -> p g v", v=64), ALU.add, [Sk, Pk], [Sk])
                if last:
                    bi_ = blk["tb0"] // 16
                    self.tt(S[:], S[:], get[:, d, :, bi_:bi_ + 1].broadcast_to([128, 16, 64]), ALU.mult, [Sk, "get"], [Sk])
                self.cp(t["Sb"][:], S[:], [Sk], [f"Sb{d}"], eng="act")

            def stage_y(d, R1, R1k, ytime):
                t = dirs[d]
                if ytime < 0:
                    return
                Py, Pyk = t["Py"], f"Py{d}"
                for h in range(2):
                    self.mm(Py[:, 512 * h:512 * h + 512], sel[64:96, :], R1[64:96, 8 * h:8 * h + 8, :], True, True, ["sel", R1k + "s"], [Pyk])
                yb, ybk = t["yb"].next()
                self.cp(yb[:, 0, :], Py[0:2, :], [Pyk], [ybk], eng="act")
                self.dma(YD[d, :, ytime, :], yb[:, 0, :], [ybk], [], q="act")

            fw_blocks = list(range(0, TB, TBs))
            bw_blocks = list(range(TC - TBs, -1, -TBs)) + list(range(TB - TBs, TC - 1, -TBs))
            nblk = len(fw_blocks)
            if max_steps is not None:
                nblk = max_steps // TBs
            def dir_gen(d):
                blist = fw_blocks if d == 0 else bw_blocks
                blk = load_fill(d, load_dma(d, blist[0]))
                for bi in range(nblk):
                    nxt = load_dma(d, blist[bi + 1]) if bi + 1 < nblk else None
                    for j in range(TBs):
                        if j == TBs // 2 and nxt is not None:
                            load_fill(d, nxt)
                        tc = j if d == 0 else TBs - 1 - j
                        R1, R1k = stage_vx(d, blk, tc)
                        R1, R1k = stage_m1(d, blk["L1"][:, tc, :], [blk["L1k"]], R1, R1k)
                        yield
                        stage_upd(d, blk, tc, R1, R1k, j == TBs - 1)
                        yield
                        tt_ = blk["tb0"] + tc
                        stage_y(d, R1, R1k, tt_ - 1 if d == 0 else tt_)
                        yield
                    blk = nxt
            g0, g1 = dir_gen(0), dir_gen(1)
            next(g0)
            round_robin([g1, g0], 2)
            t = dirs[0]
            for m in range(2):
                rows = slice(64 * m, 64 * m + 64)
                self.cp(t["L1x"][rows, 64 + 16 * m:80 + 16 * m], rlt[rows, :, fw_blocks[nblk - 1] // 16], ["rlt"], ["L1x0"], eng="pool")
            last_t = fw_blocks[nblk - 1] + TBs - 1
            R1, R1k = stage_m1(0, t["L1x"][:], ["L1x0"])
            stage_y(0, R1, R1k, last_t)
            if "SFIN" in self.dbg:
                SF = self.dram("SFIN", [2, 128, 1024])
                for d in range(2):
                    t = dirs[d]
                    self.dma(SF[d], t["S"][0][:].rearrange("p g v -> p (g v)"), [f"S{d}0"], [])
            P.end_phase()

    def phase_readout(self, l):
        nc, P = self.nc, self.P
        TC, TL, TB = self.TC, self.TL, self.TB
        lnx_g = self.inp("lnx_g", [DEPTH, D]); lnx_b = self.inp("lnx_b", [DEPTH, D])
        YD = self.dram("YD", [2, 2, TB, D])
        BON = self.dram("BON", [8, 128, BL, TB]); GG = self.dram("GG", [8, 128, BL, TB])
        YA = self.dram("YA", [8, 128, BL, TB])
        with ExitStack() as ph, nc.allow_non_contiguous_dma(reason="small param transposes"):
            lg = self.sb(ph, "lg", [128, 8]); lb = self.sb(ph, "lb", [128, 8])
            self.dma(lg[:], lnx_g[l].rearrange("(c p) -> p c", p=128), [], ["lg"])
            self.dma(lb[:], lnx_b[l].rearrange("(c p) -> p c", p=128), [], ["lb"])
            yt = self.rot(ph, "yt", [128, 2, 2, 64], 6)
            ys = self.rot(ph, "ysum", [128, 128], 4)
            pT = self.rot(ph, "pT", [128, 512], 3, psum=True)
            pS = self.rot(ph, "pS", [128, 512], 5, psum=True)
            pools = {}

            def T(name):
                if name not in pools:
                    pools[name] = self.rot(ph, "f_" + name, [128, 512], 3)
                return pools[name].next()
            def ro_gen(b, t0, L, p):
                nt = L // 128
                if True:
                    g = b * 8 + p
                    pt, pk = pT.next()
                    for i in range(nt):
                        y2, y2k = yt.next()
                        for d in range(2):
                            self.dma(y2[:, d], YD[d, :, t0 + i * 128:t0 + (i + 1) * 128, g * 64:(g + 1) * 64].rearrange("m t v -> t m v"), [], [y2k])
                        ysm, ysk = ys.next()
                        self.tt(ysm[:].rearrange("t (m v) -> t m v", v=64), y2[:, 0], y2[:, 1], ALU.add, [y2k], [ysk])
                        self.tr(pt[:, i * 128:(i + 1) * 128], ysm[:], [ysk], [pk])
                    yield
                    bn, bnk = T("bn"); gg, ggk = T("gg")
                    self.dma(bn[:, 0:L], BON[p, :, b, t0:t0 + L], [], [bnk])
                    self.dma(gg[:, 0:L], GG[p, :, b, t0:t0 + L], [], [ggk])
                    y, yk = T("y")
                    self.cp(y[:, 0:L], pt[:, 0:L], [pk], [yk], eng="act")
                    yield
                    pm, pmk = pS.next()
                    self.mm(pm[:, 0:L], self.bo[:], y[:, 0:L], True, True, ["bo", yk], [pmk])
                    yield
                    cen, cenk = T("cen")
                    self.stt(cen[:, 0:L], pm[:, 0:L], -1.0 / 64, y[:, 0:L], ALU.mult, ALU.add, [pmk, yk], [cenk])
                    yield
                    sq, sqk = T("sq")
                    self.act(sq[:, 0:L], cen[:, 0:L], AF.Square, [cenk], [sqk])
                    yield
                    pv, pvk = pS.next()
                    self.mm(pv[:, 0:L], self.bo[:], sq[:, 0:L], True, True, ["bo", sqk], [pvk])
                    yield
                    rs, rsk = T("rs")
                    self.ts(rs[:, 0:L], pv[:, 0:L], 1.0 / 64, GN_EPS, ALU.mult, ALU.add, [pvk], [rsk])
                    yield
                    self.act(rs[:, 0:L], rs[:, 0:L], AF.Sqrt, [rsk], [rsk])
                    yield
                    P.op("dve", lambda e, o=rs[:, 0:L]: e.reciprocal(out=o, in_=o), reads=[rsk], writes=[rsk])
                    self.tt(cen[:, 0:L], cen[:, 0:L], rs[:, 0:L], ALU.mult, [cenk, rsk], [cenk])
                    self.ts(cen[:, 0:L], cen[:, 0:L], lg[:, p:p + 1], lb[:, p:p + 1], ALU.mult, ALU.add, [cenk, "lg", "lb"], [cenk])
                    self.tt(cen[:, 0:L], cen[:, 0:L], bn[:, 0:L], ALU.add, [cenk, bnk], [cenk])
                    self.tt(cen[:, 0:L], cen[:, 0:L], gg[:, 0:L], ALU.mult, [cenk, ggk], [cenk])
                    self.dma(YA[p, :, b, t0:t0 + L], cen[:, 0:L], [cenk], [])
            round_robin([ro_gen(b, t0, L, p) for (b, kind, t0, L) in self.blocks for p in range(8)], 3)
            P.end_phase()

    def phase_sgu(self, l):
        nc, P = self.nc, self.P
        TC, TL, TB = self.TC, self.TL, self.TB
        sg_g = self.inp("sgu_ln_g", [DEPTH, D]); sg_b = self.inp("sgu_ln_b", [DEPTH, D])
        sg_w = self.inp("sgu_w", [DEPTH, 8, 128, 128]); sg_bias = self.inp("sgu_b", [DEPTH, 8, 128])
        VS = self.dram("VS", [BL, TB, D]); UT = self.dram("UT", [8, 128, BL, TB])
        YS = self.dram("YS", [8, 128, BL, TB])
        with ExitStack() as ph:
            grow = self.sb(ph, "grow", [128, D]); brow = self.sb(ph, "brow", [128, D])
            sgb = self.sb(ph, "sgb", [128, 8, 128])
            wsT = self.sb(ph, "wsT", [128, 8, 128], BF16)
            wld = self.rot(ph, "wld", [128, 128], 2)
            self.dma(grow[:], sg_g[l].partition_broadcast(128), [], ["grow"])
            self.dma(brow[:], sg_b[l].partition_broadcast(128), [], ["brow"])
            self.dma(sgb[:].rearrange("p g q -> p (g q)"), sg_bias[l].rearrange("g q -> (g q)").partition_broadcast(128), [], ["sgb"])
            pw = self.rot(ph, "pw", [128, 128], 2, psum=True)
            for g in range(8):
                w, wk = wld.next()
                self.dma(w[:], sg_w[l, g], [], [wk])
                pt, pk = pw.next()
                self.tr(pt[:], w[:], [wk], [pk])
                self.cp(wsT[:, g, :], pt[:], [pk], ["wsT"])
            vt = self.rot(ph, "vt", [128, D], 2)
            vn = self.rot(ph, "vn", [128, D], 2)
            vr = self.rot(ph, "vr", [128, D], 2, dt=BF16)
            st = self.rot(ph, "st", [128, 16], 2)
            ut = self.rot(ph, "ut", [128, 8, 128], 2)
            yo = self.rot(ph, "yo", [128, 8, 128], 2)
            pg = self.rot(ph, "pg", [128, 8, 128], 2, psum=True)
            for b in range(BL):
                for t0 in range(0, TB, 128):
                    v, vk = vt.next(); n, nk = vn.next(); s_, sk = st.next()
                    self.dma(v[:], VS[b, t0:t0 + 128, :], [], [vk])
                    u, uk = ut.next()
                    self.dma(u[:], UT[:, :, b, t0:t0 + 128].rearrange("g c p -> c g p"), [], [uk])
                    self.ln_tile(v, vk, n[:], nk, s_, sk)
                    self.tt(n[:], n[:], grow[:], ALU.mult, [nk, "grow"], [nk])
                    nr, nrk = vr.next()
                    self.tt(nr[:], n[:], brow[:], ALU.add, [nk, "brow"], [nrk])
                    pp, ppk = pg.next()
                    for g in range(8):
                        self.mm(pp[:, g, :], nr[:, g * 128:(g + 1) * 128], wsT[:, g, :], True, True, [nrk, "wsT"], [ppk])
                    o, ok = yo.next()
                    self.tt(o[:], pp[:], sgb[:], ALU.add, [ppk, "sgb"], [ok])
                    self.tt(o[:], o[:], u[:], ALU.mult, [ok, uk], [ok])
                    self.dma(YS[:, :, b, t0:t0 + 128].rearrange("g c p -> c g p"), o[:], [ok], [])
            P.end_phase()

    def phase_merge(self, l):
        nc, P = self.nc, self.P
        TC, TL, TB = self.TC, self.TL, self.TB
        wa = self.inp("w_branch_a", [DEPTH, D, D]); wb = self.inp("w_branch_b", [DEPTH, D, D]); wo = self.inp("w_out", [DEPTH, D, D])
        l1g = self.inp("ln1_g", [DEPTH, D]); l1b = self.inp("ln1_b", [DEPTH, D])
        modrow = self.dram(f"modrow{l}", [3, 6 * D])
        xres = self.dram("xres", [BL, TB, D])
        YA = self.dram("YA", [8, 128, BL, TB]); YS = self.dram("YS", [8, 128, BL, TB]); GT = self.dram("GT", [16, 128, BL, TB])
        with ExitStack() as ph:
            WO = self.sb(ph, "WO", [128, 8, D], BF16)
            for dc in range(8):
                self.dma(WO[:, dc, :], wo[l, dc * 128:(dc + 1) * 128, :], [], ["WO"], q="pool")
            wab = self.rot(ph, "wab", [128, 2, 8, 128], 3, dt=BF16)
            grow = self.sb(ph, "grow", [128, D]); brow = self.sb(ph, "brow", [128, D])
            self.dma(grow[:], l1g[l].partition_broadcast(128), [], ["grow"])
            self.dma(brow[:], l1b[l].partition_broadcast(128), [], ["brow"])
            gtr = self.sb(ph, "gtr", [128, 3, D])
            for j in range(3):
                self.dma(gtr[:, j, :], modrow[j, 2 * D:3 * D].partition_broadcast(128), [], ["gtr"])
            yaT = [self.sb(ph, f"yaT{ic}", [128, 512], BF16) for ic in range(8)]
            ysT = [self.sb(ph, f"ysT{ic}", [128, 512], BF16) for ic in range(8)]
            gin = self.rot(ph, "gin", [128, 512], 4)
            mT = self.sb(ph, "mT", [128, 8, 512], BF16)
            tmp = self.rot(ph, "mtmp", [128, 512], 2)
            pA = self.rot(ph, "pA", [128, 512], 2, psum=True)
            pB = self.rot(ph, "pB", [128, 512], 2, psum=True)
            pO = self.rot(ph, "pO", [128, D], 2, psum=True)
            xt = self.rot(ph, "xt", [128, D], 2); xo = self.rot(ph, "xo", [128, D], 2)
            st = self.rot(ph, "st", [128, 16], 2)
            for (b, kind, t0, L) in self.blocks:
                j = 0 if kind == 0 else 1 + b
                nt = L // 128
                ins_a = []; ins_s = []
                for ic in range(8):
                    a_, ak = yaT[ic], f"yaT{ic}"; s__, sk_ = ysT[ic], f"ysT{ic}"
                    self.dma(a_[:, 0:L], YA[ic, :, b, t0:t0 + L], [], [ak], q="pool")
                    self.dma(s__[:, 0:L], YS[ic, :, b, t0:t0 + L], [], [sk_], q="pool")
                    ins_a.append((a_, ak)); ins_s.append((s__, sk_))
                for oc in range(8):
                    ga, gak = gin.next(); gs, gsk = gin.next()
                    self.dma(ga[:, 0:L], GT[oc, :, b, t0:t0 + L], [], [gak])
                    self.dma(gs[:, 0:L], GT[8 + oc, :, b, t0:t0 + L], [], [gsk])
                    pa, pak = pA.next(); pb, pbk = pB.next()
                    W2_, w2k = wab.next()
                    self.dma(W2_[:, 0], wa[l, :, oc * 128:(oc + 1) * 128].rearrange("(c p) n -> p c n", p=128), [], [w2k], q="pool")
                    self.dma(W2_[:, 1], wb[l, :, oc * 128:(oc + 1) * 128].rearrange("(c p) n -> p c n", p=128), [], [w2k], q="pool")
                    for ic in range(8):
                        self.mm(pa[:, 0:L], W2_[:, 0, ic, :], ins_a[ic][0][:, 0:L],
                                ic == 0, ic == 7, [w2k, ins_a[ic][1]], [pak])
                    for ic in range(8):
                        self.mm(pb[:, 0:L], W2_[:, 1, ic, :], ins_s[ic][0][:, 0:L],
                                ic == 0, ic == 7, [w2k, ins_s[ic][1]], [pbk])
                    tm, tmk = tmp.next()
                    self.tt(tm[:, 0:L], pa[:, 0:L], ga[:, 0:L], ALU.mult, [pak, gak], [tmk])
                    self.tt(gs[:, 0:L], pb[:, 0:L], gs[:, 0:L], ALU.mult, [pbk, gsk], [gsk])
                    self.tt(mT[:, oc, 0:L], tm[:, 0:L], gs[:, 0:L], ALU.add, [tmk, gsk], [f"mT{oc}"])
                mk = [f"mT{oc}" for oc in range(8)]
                for i in range(nt):
                    po, pok = pO.next()
                    for h in range(2):
                        for ic in range(8):
                            self.mm(po[:, 512 * h:512 * h + 512], mT[:, ic, i * 128:(i + 1) * 128],
                                    WO[:, ic, 512 * h:512 * h + 512], ic == 0, ic == 7, mk + ["WO"], [pok])
                    x, xk = xt.next(); o, ok = xo.next(); s_, sk = st.next()
                    rows = slice(t0 + i * 128, t0 + (i + 1) * 128)
                    self.dma(x[:], xres[b, rows, :], [], [xk])
                    self.tt(o[:], po[:], gtr[:, j, :], ALU.mult, [pok, "gtr"], [ok])
                    self.stt(o[:], x[:], ALPHA, o[:], ALU.mult, ALU.add, [xk, ok], [ok])
                    self.ln_tile(o, ok, x[:], xk, s_, sk)
                    self.tt(x[:], x[:], grow[:], ALU.mult, [xk, "grow"], [xk])
                    self.tt(x[:], x[:], brow[:], ALU.add, [xk, "brow"], [xk])
                    self.dma(xres[b, rows, :], x[:], [xk], [])
            P.end_phase()

    def phase_moe_a(self, l):
        nc, P = self.nc, self.P
        TC, TL, TB = self.TC, self.TL, self.TB
        NTB = TB // 128
        rw_in = self.inp("router_w", [DEPTH, D, NEXP])
        modrow = self.dram(f"modrow{l}", [3, 6 * D])
        xres = self.dram("xres", [BL, TB, D])
        HMD = self.dram("HMD", [BL * TB, D + 16])
        MOED = self.dram("MOED", [BL * TB, D])
        IDXD = self.dram("IDXD", [128, NEXP * 5], I32)
        capL, capC = 2 * TL // NEXP, 2 * TC // NEXP
        assert capL == 256 and capC == 32
        with ExitStack() as ph, nc.allow_non_contiguous_dma(reason="small param transposes"):
            shr = self.sb(ph, "shr", [128, 3, D]); scr = self.sb(ph, "scr", [128, 3, D])
            for j in range(3):
                self.dma(shr[:, j, :], modrow[j, 3 * D:4 * D].partition_broadcast(128), [], ["shr"])
                self.dma(scr[:, j, :], modrow[j, 4 * D:5 * D].partition_broadcast(128), [], ["scr"])
            self.ts(scr[:], scr[:], 1.0, None, ALU.add, None, ["scr"], ["scr"])
            rw = self.sb(ph, "rw", [128, 8, NEXP])
            self.dma(rw[:], rw_in[l].rearrange("(c p) e -> p c e", p=128), [], ["rw"])
            zt = self.sb(ph, "zt", [128, D])
            self.memset(zt[:], 0.0, ["zt"])
            for i in range(BL * NTB):
                self.dma(MOED[i * 128:(i + 1) * 128, :], zt[:], ["zt"], [])
            xt = self.rot(ph, "xt", [128, D], 2); st = self.rot(ph, "st", [128, 16], 2)
            hm = self.rot(ph, "hm", [128, D + 16], 2)
            hmT = self.rot(ph, "hmT", [128, 8, 128], 2)
            sm = self.rot(ph, "sm", [128, 4], 2)
            ex = self.rot(ph, "ex", [128, NEXP], 2)
            pT = self.rot(ph, "pT", [128, 8, 128], 1, psum=True)
            pr = self.rot(ph, "pr", [128, 512], 2, psum=True)
            pa = self.rot(ph, "pa", [128, 512], 1, psum=True)
            aftL = [self.sb(ph, f"aftL{b}", [NEXP, TL]) for b in range(BL)]
            aftC = [self.sb(ph, f"aftC{b}", [NEXP, TC]) for b in range(BL)]
            def tile_gen(b, ti):
                if True:
                    t0 = ti * 128
                    j = 0 if t0 < TC else 1 + b
                    x, xk = xt.next(); s_, sk = st.next(); h, hk = hm.next()
                    self.dma(x[:], xres[b, t0:t0 + 128, :], [], [xk])
                    self.ln_tile(x, xk, h[:, 0:D], hk, s_, sk)
                    yield
                    self.tt(h[:, 0:D], h[:, 0:D], scr[:, j, :], ALU.mult, [hk, "scr"], [hk])
                    self.tt(h[:, 0:D], h[:, 0:D], shr[:, j, :], ALU.add, [hk, "shr"], [hk])
                    yield
                    pt, pk = pT.next()
                    for dc in range(8):
                        self.tr(pt[:, dc, :], h[:, dc * 128:(dc + 1) * 128], [hk], [pk])
                    yield
                    hT, hTk = hmT.next()
                    self.cp(hT[:], pt[:], [pk], [hTk], eng="act")
                    yield
                    pq, pqk = pr.next()
                    for dc in range(8):
                        self.mm(pq[:, 0:NEXP], hT[:, dc, :], rw[:, dc, :], dc == 0, dc == 7, [hTk, "rw"], [pqk])
                    yield
                    m_, mk_ = sm.next(); e_, ek_ = ex.next()
                    P.op("dve", lambda e, o=m_[:, 0:1], i_=pq[:, 0:NEXP]: e.tensor_reduce(out=o, in_=i_, axis=AX.X, op=ALU.max), reads=[pqk], writes=[mk_])
                    self.ts(m_[:, 1:2], m_[:, 0:1], -1.0, None, ALU.mult, None, [mk_], [mk_])
                    yield
                    P.op("act", lambda e, o=e_[:], i_=pq[:, 0:NEXP], bb=m_[:, 1:2], ac=m_[:, 2:3]: e.activation(out=o, in_=i_, func=AF.Exp, bias=bb, scale=1.0, accum_out=ac),
                         reads=[pqk, mk_], writes=[ek_, mk_])
                    P.op("dve", lambda e, o=m_[:, 3:4], i_=m_[:, 2:3]: e.reciprocal(out=o, in_=i_), reads=[mk_], writes=[mk_])
                    self.ts(h[:, D:D + NEXP], e_[:], m_[:, 3:4], None, ALU.mult, None, [ek_, mk_], [hk])
                    yield
                    self.dma(HMD[b * TB + t0:b * TB + t0 + 128, :], h[:], [hk], [])
                    pp, ppk = pa.next()
                    self.tr(pp[0:NEXP, 0:128], h[:, D:D + NEXP], [hk], [ppk])
                    if t0 < TC:
                        self.cp(aftC[b][:, t0:t0 + 128], pp[0:NEXP, 0:128], [ppk], [f"aftC{b}"], eng="act")
                    else:
                        self.cp(aftL[b][:, t0 - TC:t0 - TC + 128], pp[0:NEXP, 0:128], [ppk], [f"aftL{b}"], eng="act")
            round_robin([tile_gen(b, ti) for b in range(BL) for ti in range(NTB)], 1)
            postm = self.sb(ph, "postm", [128, BL * NTB, NEXP])
            work = self.sb(ph, "work", [NEXP, TL]); cum = self.sb(ph, "cum", [NEXP, TL]); zer = self.sb(ph, "zer", [NEXP, TL])
            m8 = self.sb(ph, "m8", [NEXP, 8])
            self.memset(zer[:], 0.0, ["zer"])
            pp2 = pa
            for b in range(BL):
                for (aft, ak, T_, cap, tbase) in ((aftC[b], f"aftC{b}", TC, capC, 0), (aftL[b], f"aftL{b}", TL, capL, TC)):
                    self.cp(work[:, 0:T_], aft[:, 0:T_], [ak], ["work"])
                    for r_ in range(cap // 8):
                        P.op("dve", lambda e, T_=T_: e.max(out=m8[:], in_=work[:, 0:T_]), reads=["work"], writes=["m8"])
                        if r_ < cap // 8 - 1:
                            P.op("dve", lambda e, T_=T_: e.match_replace(out=work[:, 0:T_], in_to_replace=m8[:], in_values=work[:, 0:T_], imm_value=-1e30),
                                 reads=["work", "m8"], writes=["work"])
                    self.ts(work[:, 0:T_], aft[:, 0:T_], m8[:, 7:8], None, ALU.is_ge, None, [ak, "m8"], ["work"])
                    P.op("dve", lambda e, T_=T_: e.tensor_tensor_scan(out=cum[:, 0:T_], data0=work[:, 0:T_], data1=zer[:, 0:T_], initial=0.0,
                                                                      op0=ALU.add, op1=ALU.add), reads=["work", "zer"], writes=["cum"])
                    self.tt(cum[:, 0:T_], cum[:, 0:T_], work[:, 0:T_], ALU.mult, ["cum", "work"], ["cum"])
                    self.ts(cum[:, 0:T_], cum[:, 0:T_], -1.0, None, ALU.add, None, ["cum"], ["cum"])
                    for i in range(T_ // 128):
                        gt = b * NTB + (tbase + i * 128) // 128
                        pq, pqk = pp2.next()
                        self.tr(pq[:, 256:256 + NEXP], cum[:, i * 128:(i + 1) * 128], ["cum"], [pqk])
                        self.cp(postm[:, gt, :], pq[:, 256:256 + NEXP], [pqk], ["postm"], eng="act")
            tgi = self.sb(ph, "tgi", [128, BL * NTB], I32); tg = self.sb(ph, "tg", [128, BL * NTB])
            iri = self.sb(ph, "iri", [128, 256], I32); ir = self.sb(ph, "ir", [128, 256])
            P.op("pool", lambda e: e.iota(tgi[:], pattern=[[128, BL * NTB]], base=0, channel_multiplier=1), reads=(), writes=["tgi"])
            P.op("pool", lambda e: e.iota(iri[:], pattern=[[1, 256]], base=0, channel_multiplier=0), reads=(), writes=["iri"])
            self.cp(tg[:], tgi[:], ["tgi"], ["tg"]); self.cp(ir[:], iri[:], ["iri"], ["ir"])
            idx = self.sb(ph, "idx", [128, NEXP, 5], I32)
            self.memset(idx[:], 0, ["idx"])
            Pm = self.rot(ph, "Pm", [128, 256], 3)
            Pmc = [self.sb(ph, f"Pmc{b}", [128, 2, 64]) for b in range(BL)]
            for b in range(BL):
                self.memset(Pmc[b][:], 0.0, [f"Pmc{b}"])
            pix = self.rot(ph, "pix", [128, 512], 3, psum=True)
            ntl = TL // 128
            for e_i in range(NEXP):
                for b in range(BL):
                    p0, p0k = pix.next(); p1, p1k = pix.next()
                    for i in range(ntl):
                        gt = b * NTB + TC // 128 + i
                        pm, pmk = Pm.next()
                        self.ts(pm[:], ir[:], postm[:, gt, e_i:e_i + 1], None, ALU.is_equal, None, ["ir", "postm"], [pmk])
                        self.mm(p0[:, 0:1], pm[:, 0:128], tg[:, gt:gt + 1], i == 0, i == ntl - 1, [pmk, "tg"], [p0k])
                        self.mm(p1[:, 0:1], pm[:, 128:256], tg[:, gt:gt + 1], i == 0, i == ntl - 1, [pmk, "tg"], [p1k])
                    self.cp(idx[:, e_i, 2 * b:2 * b + 1], p0[:, 0:1], [p0k], ["idx"])
                    self.cp(idx[:, e_i, 2 * b + 1:2 * b + 2], p1[:, 0:1], [p1k], ["idx"])
                pc, pck = pix.next()
                n_ = 0
                for b in range(BL):
                    for i in range(TC // 128):
                        gt = b * NTB + i
                        pmc = Pmc[b]; pmck = f"Pmc{b}"
                        self.ts(pmc[:, i, 32 * b:32 * b + 32], ir[:, 0:32], postm[:, gt, e_i:e_i + 1], None, ALU.is_equal, None, ["ir", "postm"], [pmck])
                        self.mm(pc[0:64, 0:1], pmc[:, i, :], tg[:, gt:gt + 1], n_ == 0, n_ == BL * (TC // 128) - 1, [pmck, "tg"], [pck])
                        n_ += 1
                self.cp(idx[0:64, e_i, 4:5], pc[0:64, 0:1], [pck], ["idx"])
            self.dma(IDXD[:, :], idx[:].rearrange("p e k -> p (e k)"), ["idx"], [])
            P.end_phase()

    def phase_moe_b(self, l):
        nc, P = self.nc, self.P
        TC, TL, TB = self.TC, self.TL, self.TB
        w1 = self.inp("exp_w1", [DEPTH, NEXP, D, DFF]); w3 = self.inp("exp_w3", [DEPTH, NEXP, D, DFF]); w2 = self.inp("exp_w2", [DEPTH, NEXP, DFF, D])
        HMD = self.dram("HMD", [BL * TB, D + 16]); MOED = self.dram("MOED", [BL * TB, D]); IDXD = self.dram("IDXD", [128, NEXP * 5], I32)
        NS = 640
        blks = [(0, 128), (128, 128), (256, 128), (384, 128), (512, 64)]
        with ExitStack() as ph:
            idx = self.sb(ph, "idx", [128, NEXP, 5], I32)
            self.dma(idx[:].rearrange("p e k -> p (e k)"), IDXD[:, :], [], ["idx"])
            xe = self.rot(ph, "xe", [128, D + 16], 3)
            gates = self.rot(ph, "gates", [128, 8], 2)
            xeTr = self.rot(ph, "xeT", [128, 8, NS], 2, dt=BF16); hidTr = self.rot(ph, "hidT", [128, 16, NS], 2, dt=BF16)
            for tl_ in xeTr.tiles:
                self.memset(tl_[:], 0.0, [])
            stg = self.rot(ph, "wstg", [128, 8, 512], 3)
            wr = self.rot(ph, "wr", [128, 8, 512], 4, dt=BF16)
            sl = self.rot(ph, "sl", [128, 512], 2)
            pg = self.rot(ph, "pg", [128, 8, 128], 1, psum=True)
            pmm = self.rot(ph, "pmm", [128, 512], 6, psum=True)

            def load_w(src_ap):
                s_, sk_ = stg.next(); w_, wk_ = wr.next()
                self.dma(s_[:], src_ap, [], [sk_])
                self.cp(w_[:], s_[:], [sk_], [wk_], eng="pool")
                return w_, wk_
            def load_w2(src_ap):
                s_, sk_ = stg.next(); w_, wk_ = wr.next()
                self.dma(s_[:, 0:4, :], src_ap, [], [sk_])
                self.cp(w_[:, 0:4, :], s_[:, 0:4, :], [sk_], [wk_], eng="pool")
                return w_, wk_
            for e_i in range(NEXP):
                gt_, gk_ = gates.next()
                xeT, xeTk = xeTr.next(); hidT, hidTk = hidTr.next()
                for bi, (c0, n) in enumerate(blks):
                    x_, xk_ = xe.next()
                    P.dma("pool", lambda e, x_=x_, n=n, bi=bi, e_i=e_i: e.indirect_dma_start(
                        out=x_[0:n, :], out_offset=None, in_=HMD[:, :],
                        in_offset=bass.IndirectOffsetOnAxis(ap=idx[0:n, e_i, bi:bi + 1], axis=0)), reads=["idx"], writes=[xk_])
                    self.cp(gt_[0:n, bi:bi + 1], x_[0:n, D + e_i:D + e_i + 1], [xk_], [gk_], eng="act")
                    pt, pk = pg.next()
                    for dc in range(8):
                        self.tr(pt[:, dc, 0:n], x_[0:n, dc * 128:(dc + 1) * 128], [xk_], [pk])
                    self.cp(xeT[:, :, c0:c0 + n], pt[:, :, 0:n], [pk], [xeTk])
                for fb in range(4):
                    W1, W1k = load_w(w1[l, e_i, :, fb * 512:(fb + 1) * 512].rearrange("(c p) n -> p c n", p=128))
                    W3, W3k = load_w(w3[l, e_i, :, fb * 512:(fb + 1) * 512].rearrange("(c p) n -> p c n", p=128))
                    for fc in range(4):
                        fcc = fb * 4 + fc
                        for (n0, nn) in ((0, 512), (512, 128)):
                            p1, p1k = pmm.next(); p3, p3k = pmm.next()
                            for dc in range(8):
                                self.mm(p1[:, 0:nn], W1[:, dc, fc * 128:(fc + 1) * 128], xeT[:, dc, n0:n0 + nn],
                                        dc == 0, dc == 7, [W1k, xeTk], [p1k])
                            for dc in range(8):
                                self.mm(p3[:, 0:nn], W3[:, dc, fc * 128:(fc + 1) * 128], xeT[:, dc, n0:n0 + nn],
                                        dc == 0, dc == 7, [W3k, xeTk], [p3k])
                            s_, sk_ = sl.next()
                            self.act(s_[:, 0:nn], p1[:, 0:nn], AF.Silu, [p1k], [sk_])
                            self.tt(hidT[:, fcc, n0:n0 + nn], s_[:, 0:nn], p3[:, 0:nn], ALU.mult, [sk_, p3k], [hidTk])
                yts = {}
                for half in range(2):
                    accs = [pmm.next() for _ in blks]
                    for pc in range(4):
                        W2, W2k = load_w2(w2[l, e_i, pc * 512:(pc + 1) * 512, half * 512:(half + 1) * 512].rearrange("(c p) n -> p c n", p=128))
                        for bi, (c0, n) in enumerate(blks):
                            po, pok = accs[bi]
                            for fc in range(4):
                                fcc = pc * 4 + fc
                                self.mm(po[:, :], hidT[:, fcc, c0:c0 + 128], W2[:, fc, :],
                                        fcc == 0, fcc == 15, [hidTk, W2k], [pok])
                    for bi, (c0, n) in enumerate(blks):
                        po, pok = accs[bi]
                        if bi not in yts:
                            yts[bi] = self.yes_tile(ph, bi)
                        yt_, ytk_ = yts[bi]
                        self.ts(yt_[0:n, half * 512:(half + 1) * 512], po[0:n, :], gt_[0:n, bi:bi + 1], None, ALU.mult, None, [pok, gk_], [ytk_])
                for bi, (c0, n) in enumerate(blks):
                    yt_, ytk_ = yts[bi]
                    P.dma("pool", lambda e, yt_=yt_, n=n, bi=bi, e_i=e_i: e.indirect_dma_start(
                        out=MOED[:, :], out_offset=bass.IndirectOffsetOnAxis(ap=idx[0:n, e_i, bi:bi + 1], axis=0),
                        in_=yt_[0:n, :], in_offset=None, compute_op=ALU.add), reads=["idx", ytk_], writes=["MOED"])
            P.end_phase()

    def yes_tile(self, ph, bi):
        if not hasattr(self, "_yes") or self._yes_ph is not ph:
            self._yes = [self.rot(ph, f"yesb{k}", [128, D], 1) for k in range(5)]
            self._yes_ph = ph
        return self._yes[bi].next()

    def phase_ln2(self, l, last):
        nc, P = self.nc, self.P
        TC, TL, TB = self.TC, self.TL, self.TB
        l2g = self.inp("ln2_g", [DEPTH, D]); l2b = self.inp("ln2_b", [DEPTH, D])
        modrow = self.dram(f"modrow{l}", [3, 6 * D])
        xres = self.dram("xres", [BL, TB, D]); MOED = self.dram("MOED", [BL * TB, D])
        if last:
            if "out" not in self.drams:
                self.drams["out"] = nc.dram_tensor("out", [BL, TL, D], F32, kind="ExternalOutput").ap()
            outp = self.drams["out"]
        with ExitStack() as ph:
            grow = self.sb(ph, "grow", [128, D]); brow = self.sb(ph, "brow", [128, D])
            self.dma(grow[:], l2g[l].partition_broadcast(128), [], ["grow"])
            self.dma(brow[:], l2b[l].partition_broadcast(128), [], ["brow"])
            gtr = self.sb(ph, "gtr", [128, 3, D])
            for j in range(3):
                self.dma(gtr[:, j, :], modrow[j, 5 * D:6 * D].partition_broadcast(128), [], ["gtr"])
            xt = self.rot(ph, "xt", [128, D], 3); mt = self.rot(ph, "mt", [128, D], 3); st = self.rot(ph, "st", [128, 16], 2)
            for b in range(BL):
                for t0 in range(0, TB, 128):
                    if last and t0 < TC:
                        continue
                    j = 0 if t0 < TC else 1 + b
                    x, xk = xt.next(); m, mk = mt.next(); s_, sk = st.next()
                    self.dma(x[:], xres[b, t0:t0 + 128, :], [], [xk])
                    self.dma(m[:], MOED[b * TB + t0:b * TB + t0 + 128, :], [], [mk])
                    self.tt(m[:], m[:], gtr[:, j, :], ALU.mult, [mk, "gtr"], [mk])
                    self.stt(m[:], x[:], ALPHA, m[:], ALU.mult, ALU.add, [xk, mk], [mk])
                    self.ln_tile(m, mk, x[:], xk, s_, sk)
                    self.tt(x[:], x[:], grow[:], ALU.mult, [xk, "grow"], [xk])
                    self.tt(x[:], x[:], brow[:], ALU.add, [xk, "brow"], [xk])
                    if last:
                        self.dma(outp[b, t0 - TC:t0 - TC + 128, :], x[:], [xk], [])
                    else:
                        self.dma(xres[b, t0:t0 + 128, :], x[:], [xk], [])
            P.end_phase()

    def build_all(self):
        self.phase_init()
        for l in range(DEPTH):
            self.phase_mod(l)
            self.phase_proj(l)
            self.phase_prep(l)
            self.phase_scan(l)
            self.phase_readout(l)
            self.phase_sgu(l)
            self.phase_merge(l)
            self.phase_moe_a(l)
            self.phase_moe_b(l)
            self.phase_ln2(l, l == DEPTH - 1)
        return self.finish()

    def finish(self):
        self.es.close()
        return self.nc


_CACHE = {}

WEIGHTS = ["ada_w", "ada_b", "w_in", "shift_conv", "w0", "w2", "a0", "a2", "g2", "k_k", "k_a", "r_k", "lnx_g", "lnx_b",
           "sgu_ln_g", "sgu_ln_b", "sgu_w", "sgu_b", "w_branch_a", "w_branch_b", "w_out", "ln1_g", "ln1_b", "router_w",
           "exp_w1", "exp_w3", "exp_w2", "ln2_g", "ln2_b"]


def kernel(**inputs):
    x = np.asarray(inputs["x"], dtype=np.float32)
    B, TL, _ = x.shape
    TC = inputs["ctx"].shape[1]
    if "nc" not in _CACHE:
        kb = KB(TC, TL)
        _CACHE["nc"] = kb.build_all()
        _CACHE["names"] = list(kb.inputs.keys())
    nc = _CACHE["nc"]
    names = _CACHE["names"]
    shared = {k: np.ascontiguousarray(np.asarray(inputs[k], dtype=np.float32)) for k in WEIGHTS if k in names}
    cctx = np.ascontiguousarray(np.asarray(inputs["c_ctx"], dtype=np.float32)[None, :])
    in_maps = []
    for c in range(NCORE):
        m = dict(shared)
        m["x"] = np.ascontiguousarray(x[BL * c:BL * c + BL])
        m["ctx"] = np.ascontiguousarray(np.asarray(inputs["ctx"], dtype=np.float32)[BL * c:BL * c + BL])
        m["c"] = np.ascontiguousarray(np.asarray(inputs["c"], dtype=np.float32)[BL * c:BL * c + BL])
        m["c_ctx"] = cctx
        in_maps.append({k: m[k] for k in names})
    res = run_bass_kernel_spmd(nc, in_maps, core_ids=list(range(NCORE)))
    return np.concatenate([res.results[c]["out"] for c in range(NCORE)], axis=0).astype(np.float32)
```
